# Optimizing a Trainium2 kernel written in Bass

```python
import jax, jax.numpy as jnp
from jax import lax
import numpy as np

D_MODEL = 2048
BATCH = 1
SEQ = 8192
DEPTH = 4

N_MIXERS = 4
GROUP_WIDTH = D_MODEL // N_MIXERS
HEAD_DIM = 128
HEADS_PER_GROUP = GROUP_WIDTH // HEAD_DIM
D_FF = 5632
CONV_WIDTH = 3
POOL_WINDOWS = (2, 4, 8, 16)
POOL_GROUPS = len(POOL_WINDOWS)
POOL_GROUP_WIDTH = GROUP_WIDTH // POOL_GROUPS
MLSTM_CHUNK = 128
SB_BLOCK = 128
NORM_EPS = 1e-6

_COL_SIZES = (GROUP_WIDTH,) * 3 + (GROUP_WIDTH,) * 4 + (HEADS_PER_GROUP,) * 2 + (GROUP_WIDTH,) + (GROUP_WIDTH,) * 3
D_IN_PROJ = sum(_COL_SIZES)
SPLIT_POINTS = tuple(int(s) for s in np.cumsum(_COL_SIZES)[:-1])

kernel_name = "hymba_style_parallel_hybrid_macaron"


def rms_norm(x, g):
    xf = x.astype(jnp.float32)
    y = xf * lax.rsqrt(jnp.mean(xf * xf, axis=-1, keepdims=True) + NORM_EPS)
    return (y * g.astype(jnp.float32)).astype(x.dtype)


def swiglu(x, w_gate, w_up, w_down):
    return (jax.nn.silu(x @ w_gate) * (x @ w_up)) @ w_down


def short_conv_mixer(b, c, u, conv_w):
    z = c * u
    y = lax.conv_general_dilated(
        z, conv_w[:, None, :].astype(z.dtype), window_strides=(1,),
        padding=((CONV_WIDTH - 1, 0),), dimension_numbers=('NWC', 'WIO', 'NWC'),
        feature_group_count=z.shape[-1])
    return b * y


def mlstm_mixer(q, k, v, i_pre, f_pre, o_pre, head_gain):
    B_, S, H, dh = q.shape
    L = MLSTM_CHUNK
    NC = S // L
    f32 = jnp.float32
    qf = q.astype(f32)
    kf = k.astype(f32) * (dh ** -0.5)
    vf = v.astype(f32)
    log_f = jax.nn.log_sigmoid(f_pre.astype(f32))
    i_g = i_pre.astype(f32)

    def to_chunks(a):
        return a.reshape(B_, NC, L, H, -1).transpose(1, 0, 3, 2, 4)

    def gate_chunks(a):
        return a.reshape(B_, NC, L, H).transpose(1, 0, 3, 2)

    causal = jnp.tril(jnp.ones((L, L), dtype=bool))

    def step(carry, inp):
        C, n, m = carry
        qb, kb, vb, lf, ig = inp
        b = jnp.cumsum(lf, axis=-1)
        D = jnp.where(causal, b[..., :, None] - b[..., None, :] + ig[..., None, :], -jnp.inf)
        inter = b + m[..., None]
        m_t = jnp.maximum(inter, jnp.max(D, axis=-1))
        w_intra = jnp.exp(D - m_t[..., None])
        w_inter = jnp.exp(inter - m_t)
        s = jnp.einsum('bhtd,bhsd->bhts', qb, kb) * w_intra
        num = jnp.einsum('bhts,bhsd->bhtd', s, vb) + w_inter[..., None] * jnp.einsum('bhtd,bhde->bhte', qb, C)
        den = jnp.sum(s, axis=-1) + w_inter * jnp.einsum('bhtd,bhd->bht', qb, n)
        h = num / jnp.maximum(jnp.abs(den), jnp.exp(-m_t))[..., None]
        b_last = b[..., -1]
        g = b_last[..., None] - b + ig
        m_new = jnp.maximum(b_last + m, jnp.max(g, axis=-1))
        decay = jnp.exp(b_last + m - m_new)
        wk = jnp.exp(g - m_new[..., None])
        C_new = decay[..., None, None] * C + jnp.einsum('bhs,bhsd,bhse->bhde', wk, kb, vb)
        n_new = decay[..., None] * n + jnp.einsum('bhs,bhsd->bhd', wk, kb)
        return (C_new, n_new, m_new), h

    init = (jnp.zeros((B_, H, dh, dh), f32), jnp.zeros((B_, H, dh), f32), jnp.zeros((B_, H), f32))
    _, hs = lax.scan(step, init, (to_chunks(qf), to_chunks(kf), to_chunks(vf), gate_chunks(log_f), gate_chunks(i_g)))
    h = hs.transpose(1, 0, 3, 2, 4).reshape(B_, S, H, dh)
    h = h * lax.rsqrt(jnp.mean(h * h, axis=-1, keepdims=True) + NORM_EPS)
    h = h.reshape(B_, S, H * dh) * head_gain.astype(f32)
    return (jax.nn.sigmoid(o_pre.astype(f32)) * h).astype(q.dtype)


def pool_mixer(u, pool_w, pool_scale):
    B_, S, _ = u.shape
    f32 = jnp.float32
    uf = u.astype(f32).reshape(B_, S, POOL_GROUPS, POOL_GROUP_WIDTH)
    cs = jnp.pad(jnp.cumsum(uf, axis=1), ((0, 0), (1, 0), (0, 0), (0, 0)))
    t = jnp.arange(S)
    win = jnp.array(POOL_WINDOWS, dtype=jnp.int32)
    lo = jnp.maximum(t[:, None] + 1 - win[None, :], 0)
    grp = jnp.arange(POOL_GROUPS)[None, :]
    window_sum = cs[:, 1:] - cs[:, lo, grp]
    count = (t[:, None] + 1 - lo).astype(f32)
    pooled = window_sum / count[None, :, :, None] - uf
    y = jnp.einsum('bsgc,gcd->bsgd', pooled, pool_w.astype(f32)).reshape(B_, S, GROUP_WIDTH)
    return (y * pool_scale.astype(f32)).astype(u.dtype)


def stick_breaking_mixer(q, k, v):
    B_, S, H, dh = q.shape
    NB = S // SB_BLOCK
    f32 = jnp.float32
    qf = q.astype(f32) * (dh ** -0.5)
    kf = k.astype(f32)
    vf = v.astype(f32)
    q_blocks = qf.reshape(B_, NB, SB_BLOCK, H, dh).transpose(1, 0, 3, 2, 4)
    key_pos = jnp.arange(S)

    def block(args):
        q_blk, blk = args
        q_pos = blk * SB_BLOCK + jnp.arange(SB_BLOCK)
        z = jnp.einsum('bhqd,bshd->bhqs', q_blk, kf)
        past = key_pos[None, :] < q_pos[:, None]
        log_1m_beta = jnp.where(past, jax.nn.log_sigmoid(-z), 0.0)
        rest = lax.cumsum(log_1m_beta, axis=3, reverse=True) - log_1m_beta
        A = jnp.where(past, jnp.exp(jax.nn.log_sigmoid(z) + rest), 0.0)
        return jnp.einsum('bhqs,bshd->bqhd', A, vf)

    out = lax.map(block, (q_blocks, jnp.arange(NB)))
    return out.transpose(1, 0, 2, 3, 4).reshape(B_, S, H * dh).astype(q.dtype)


def token_mix(h, w_in, w_out, conv_w, pool_w, pool_scale, i_bias, f_bias, head_gain):
    B_, S, _ = h.shape
    proj = h @ w_in
    (cb, cc, cu, mq, mk, mv, mo, mi, mf, pu, sq, sk, sv) = jnp.split(proj, SPLIT_POINTS, axis=-1)

    def heads(a):
        return a.reshape(B_, S, HEADS_PER_GROUP, HEAD_DIM)

    y_conv = short_conv_mixer(cb, cc, cu, conv_w)
    y_mlstm = mlstm_mixer(heads(mq), heads(mk), heads(mv), mi + i_bias, mf + f_bias, mo, head_gain)
    y_pool = pool_mixer(pu, pool_w, pool_scale)
    y_sb = stick_breaking_mixer(heads(sq), heads(sk), heads(sv))
    y = jnp.concatenate([y_conv.astype(h.dtype), y_mlstm.astype(h.dtype),
                         y_pool.astype(h.dtype), y_sb.astype(h.dtype)], axis=-1)
    return y @ w_out


def setup_inputs(seed: int = 0) -> dict:
    key = jax.random.key(seed)
    ks = jax.random.split(key, 17)
    f32 = jnp.float32
    G, H = GROUP_WIDTH, HEADS_PER_GROUP
    nrm = lambda k, shape, scale: jax.random.normal(k, shape, f32) * scale
    return {
        "x": nrm(ks[0], (BATCH, SEQ, D_MODEL), 1.0),
        "w_in": nrm(ks[1], (DEPTH, D_MODEL, D_IN_PROJ), D_MODEL ** -0.5),
        "w_out": nrm(ks[2], (DEPTH, D_MODEL, D_MODEL), D_MODEL ** -0.5),
        "conv_w": nrm(ks[3], (DEPTH, CONV_WIDTH, G), CONV_WIDTH ** -0.5),
        "pool_w": nrm(ks[4], (DEPTH, POOL_GROUPS, POOL_GROUP_WIDTH, POOL_GROUP_WIDTH), POOL_GROUP_WIDTH ** -0.5),
        "pool_scale": 1.0 + nrm(ks[5], (DEPTH, G), 0.02),
        "mlstm_i_bias": nrm(ks[6], (DEPTH, H), 0.1),
        "mlstm_f_bias": 3.0 + 3.0 * jax.random.uniform(ks[7], (DEPTH, H), f32),
        "mlstm_head_gain": 1.0 + nrm(ks[8], (DEPTH, G), 0.02),
        "ffn1_w_gate": nrm(ks[9], (DEPTH, D_MODEL, D_FF), D_MODEL ** -0.5),
        "ffn1_w_up": nrm(ks[10], (DEPTH, D_MODEL, D_FF), D_MODEL ** -0.5),
        "ffn1_w_down": nrm(ks[11], (DEPTH, D_FF, D_MODEL), D_FF ** -0.5),
        "ffn2_w_gate": nrm(ks[12], (DEPTH, D_MODEL, D_FF), D_MODEL ** -0.5),
        "ffn2_w_up": nrm(ks[13], (DEPTH, D_MODEL, D_FF), D_MODEL ** -0.5),
        "ffn2_w_down": nrm(ks[14], (DEPTH, D_FF, D_MODEL), D_FF ** -0.5),
        "norm_gains": 1.0 + nrm(ks[15], (DEPTH, 6, D_MODEL), 0.02),
    }


def reference(x, w_in, w_out, conv_w, pool_w, pool_scale, mlstm_i_bias, mlstm_f_bias, mlstm_head_gain,
              ffn1_w_gate, ffn1_w_up, ffn1_w_down, ffn2_w_gate, ffn2_w_up, ffn2_w_down, norm_gains):
    for l in range(DEPTH):
        g = norm_gains[l]
        h = rms_norm(x, g[0])
        x = x + 0.5 * rms_norm(swiglu(h, ffn1_w_gate[l], ffn1_w_up[l], ffn1_w_down[l]), g[1])
        h = rms_norm(x, g[2])
        y = token_mix(h, w_in[l], w_out[l], conv_w[l], pool_w[l], pool_scale[l],
                      mlstm_i_bias[l], mlstm_f_bias[l], mlstm_head_gain[l])
        x = x + rms_norm(y, g[3])
        h = rms_norm(x, g[4])
        x = x + 0.5 * rms_norm(swiglu(h, ffn2_w_gate[l], ffn2_w_up[l], ffn2_w_down[l]), g[5])
    return x
```

```python
import os
import numpy as np
import ml_dtypes
from contextlib import ExitStack
import concourse.bass as bass
import concourse.mybir as mybir
from concourse.bass_utils import run_bass_kernel_spmd

F32 = mybir.dt.float32
BF16 = mybir.dt.bfloat16
AF = mybir.ActivationFunctionType
ALU = mybir.AluOpType

NCORES = 8
D = 2048
DC = D // 128
S = 8192
T = S // NCORES
DFF = 5632
FC = DFF // 128
DEPTH = 4
EPS = 1e-6
G = 512
DIN = 5640


class Buf:
    def __init__(self, t, name=""):
        self.t = t
        self.name = name
        self.w = {}
        self.r = {}
        self.dsem = None
        self.dcount = 0

    def __getitem__(self, idx):
        return self.t[idx]


class Eng:
    def __init__(self, K, name, h):
        self.K = K
        self.name = name
        self.h = h
        self.sem = K.es.enter_context(K.nc.semaphore("e_" + name))
        self.n = 0
        self.seen = {}

    def wait(self, tok):
        if tok is None:
            return
        sem, val = tok
        if sem is self.sem and self.name == "pe":
            return
        k = id(sem)
        if self.seen.get(k, 0) >= val:
            return
        self.h.wait_ge(sem, val)
        self.seen[k] = val


class Kern:
    def __init__(self, nc, es):
        self.nc = nc
        self.es = es
        self.pe = Eng(self, "pe", nc.tensor)
        self.act = Eng(self, "act", nc.scalar)
        self.dve = Eng(self, "dve", nc.vector)
        self.pool = Eng(self, "pool", nc.gpsimd)
        self.sp = Eng(self, "sp", nc.sync)
        self.engs = [self.pe, self.act, self.dve, self.pool, self.sp]
        self.free_sems = []
        self.semcnt = {}

    def op(self, eng, fn, reads=(), writes=()):
        for b in reads:
            for tok in b.w.values():
                eng.wait(tok)
        for b in writes:
            for tok in b.w.values():
                eng.wait(tok)
            for tok in b.r.values():
                eng.wait(tok)
        ins = fn(eng.h)
        eng.n += 1
        ins.then_inc(eng.sem, 1)
        tok = (eng.sem, eng.n)
        for b in reads:
            b.r[id(eng.sem)] = tok
        for b in writes:
            b.w = {id(eng.sem): tok}
            b.r = {}
        return ins

    def get_dsem(self, name):
        if self.free_sems:
            return self.free_sems.pop()
        return self.es.enter_context(self.nc.semaphore(self.uniq("d_" + name)))

    def dma(self, q, ob, out_ap, ib, in_ap, partial=False, owner=None, **kw):
        own = owner or ob
        for tok in ib.w.values():
            q.wait(tok)
        if not partial:
            for tok in ob.w.values():
                q.wait(tok)
        for tok in ob.r.values():
            q.wait(tok)
        if own.dsem is None:
            own.dsem = self.get_dsem(own.name)
        cnt = self.semcnt.get(id(own.dsem), 0) + 1
        self.semcnt[id(own.dsem)] = cnt
        ins = q.h.dma_start(out=out_ap, in_=in_ap, **kw)
        ins.then_inc(own.dsem, 16)
        tok = (own.dsem, 16 * cnt)
        ib.r[id(own.dsem)] = tok
        if partial:
            ob.w[id(own.dsem)] = tok
        else:
            ob.w = {id(own.dsem): tok}
        ob.r = {}
        return ins

    def sync_bufs(self, bufs, engs=None):
        for e in (engs or self.engs):
            for b in bufs:
                for tok in b.w.values():
                    e.wait(tok)
                for tok in b.r.values():
                    e.wait(tok)

    def uniq(self, name):
        self.uid = getattr(self, "uid", 0) + 1
        return f"{name}_{self.uid}"

    def sbuf(self, st, name, shape, dt):
        t = st.enter_context(self.nc.sbuf_tensor(self.uniq(name), shape, dt))
        return Buf(t, name)

    def psum(self, st, name, shape, dt=F32):
        t = st.enter_context(self.nc.psum_tensor(self.uniq(name), shape, dt))
        return Buf(t, name)

    def dram(self, name, shape, dt, kind="Internal"):
        return Buf(self.nc.dram_tensor(name, shape, dt, kind=kind).ap(), name)


class Phase:
    def __init__(self, K):
        self.K = K
        self.st = ExitStack()
        self.bufs = []

    def __enter__(self):
        self.st.__enter__()
        return self

    def sbuf(self, name, shape, dt):
        b = self.K.sbuf(self.st, name, shape, dt)
        self.bufs.append(b)
        return b

    def psum(self, name, shape, dt=F32):
        b = self.K.psum(self.st, name, shape, dt)
        self.bufs.append(b)
        return b

    def __exit__(self, *a):
        self.K.sync_bufs(self.bufs)
        for b in self.bufs:
            if b.dsem is not None:
                self.K.free_sems.append(b.dsem)
        return self.st.__exit__(*a)


def norm_in_phase(K, C, x_src, gcol, hT):
    with Phase(K) as ph:
        xs = ph.sbuf("n_xs", [128, DC, T], F32)
        sq = [ph.sbuf(f"n_sq{i}", [128, T], BF16) for i in range(2)]
        ss = ph.psum("n_ss", [128, T])
        rstd = ph.sbuf("n_rstd", [128, T], F32)
        for c in range(DC):
            K.dma(K.sp, xs, xs[:, c, :], x_src, x_src[c], partial=True)
        for c in range(DC):
            s = sq[c % 2]
            K.op(K.act, lambda e: e.activation(out=s[:, :], in_=xs[:, c, :], func=AF.Square),
                 reads=[xs], writes=[s])
            for h in range(T // 512):
                K.op(K.pe, lambda e: e.matmul(ss[:, h * 512:(h + 1) * 512], lhsT=C["ones"][:, :],
                                              rhs=s[:, h * 512:(h + 1) * 512],
                                              start=(c == 0), stop=(c == DC - 1)),
                     reads=[C["ones"], s], writes=[ss])
        rstd_from_ss(K, C, ph, ss, rstd)
        for c in range(DC):
            K.op(K.dve, lambda e: e.scalar_tensor_tensor(out=hT[:, c, :], in0=xs[:, c, :],
                                                         scalar=gcol(c), in1=rstd[:, :],
                                                         op0=ALU.mult, op1=ALU.mult),
                 reads=[xs, rstd, C["gains"]], writes=[hT])


def rstd_from_ss(K, C, ph, ss, rstd):
    rt = ph.sbuf("r_rt", [128, T], F32)
    K.op(K.act, lambda e: e.activation(out=rt[:, :], in_=ss[:, :], func=AF.Sqrt,
                                       scale=1.0 / D, bias=C["eps"][:, 0:1]),
         reads=[ss, C["eps"]], writes=[rt])
    K.op(K.dve, lambda e: e.reciprocal(out=rstd[:, :], in_=rt[:, :]), reads=[rt], writes=[rstd])


def epilogue_phase(K, C, x_src, y_src, rstd, gcol, x_dst, half):
    with Phase(K) as ph:
        xb = [ph.sbuf(f"e_x{i}", [128, T], F32) for i in range(2)]
        yb = [ph.sbuf(f"e_y{i}", [128, T], F32) for i in range(2)]
        for c in range(DC):
            x_, y_ = xb[c % 2], yb[c % 2]
            K.dma(K.sp, x_, x_[:, :], x_src, x_src[c])
            K.dma(K.sp, y_, y_[:, :], y_src, y_src[c])
            K.op(K.dve, lambda e: e.scalar_tensor_tensor(out=y_[:, :], in0=y_[:, :], scalar=gcol(c),
                                                         in1=rstd[:, :], op0=ALU.mult, op1=ALU.mult),
                 reads=[y_, rstd, C["gains"]], writes=[y_])
            if half:
                K.op(K.dve, lambda e: e.scalar_tensor_tensor(out=x_[:, :], in0=y_[:, :], scalar=0.5,
                                                             in1=x_[:, :], op0=ALU.mult, op1=ALU.add),
                     reads=[y_, x_], writes=[x_])
            else:
                K.op(K.dve, lambda e: e.tensor_tensor(out=x_[:, :], in0=y_[:, :], in1=x_[:, :], op=ALU.add),
                     reads=[y_, x_], writes=[x_])
            K.dma(K.sp, x_dst, x_dst[c], x_, x_[:, :], partial=True, owner=x_)


def down_phase(K, C, ph_outer, rhsT, nk, wd, yT, rstd):
    with Phase(K) as p4:
        ss = p4.psum("f_ss", [128, T])
        db = [p4.sbuf(f"f_db{i}", [128, nk, 128], BF16) for i in range(2)]
        po = [p4.psum(f"f_po{i}", [128, T]) for i in range(2)]
        ysb = [p4.sbuf(f"f_y{i}", [128, T], F32) for i in range(2)]
        sq = [p4.sbuf(f"f_sq{i}", [128, T], BF16) for i in range(2)]
        for i in range(DC):
            w = db[i % 2]
            p = po[i % 2]
            y_ = ysb[i % 2]
            s_ = sq[i % 2]
            for f0 in range(0, nk, 16):
                f1 = min(nk, f0 + 16)
                K.dma(K.pool, w, w[:, f0:f1, :], wd, wd[i][:, f0:f1, :], partial=(f0 > 0))
            for fc in range(nk):
                for h in range(T // 512):
                    K.op(K.pe, lambda e: e.matmul(p[:, h * 512:(h + 1) * 512], lhsT=w[:, fc, :],
                                                  rhs=rhsT[:, fc, h * 512:(h + 1) * 512],
                                                  start=(fc == 0), stop=(fc == nk - 1)),
                         reads=[w, rhsT], writes=[p])
            K.op(K.act, lambda e: e.activation(out=s_[:, :], in_=p[:, :], func=AF.Square),
                 reads=[p], writes=[s_])
            K.op(K.act, lambda e: e.activation(out=y_[:, :], in_=p[:, :], func=AF.Copy), reads=[p], writes=[y_])
            K.dma(K.sp, yT, yT[i], y_, y_[:, :], partial=True, owner=y_)
            for h in range(T // 512):
                K.op(K.pe, lambda e: e.matmul(ss[:, h * 512:(h + 1) * 512], lhsT=C["ones"][:, :],
                                              rhs=s_[:, h * 512:(h + 1) * 512],
                                              start=(i == 0), stop=(i == DC - 1)),
                     reads=[C["ones"], s_], writes=[ss])
        rstd_from_ss(K, C, p4, ss, rstd)


def ffn_stage(K, C, x_src, x_dst, yT, wgu, wd, gi):
    gains = C["gains"]
    with Phase(K) as p1:
        hT = p1.sbuf("f_hT", [128, DC, T], BF16)
        rstd2 = p1.sbuf("f_rstd2", [128, T], F32)
        norm_in_phase(K, C, x_src, lambda c: gains[:, gi * DC + c:gi * DC + c + 1], hT)
        with Phase(K) as big:
            hid = big.sbuf("f_hid", [128, FC, T], BF16)
            with Phase(K) as p2:
                wb = [p2.sbuf(f"f_wb{i}", [128, 2, DC, 128], BF16) for i in range(3)]
                ps = [p2.psum(f"f_ps{i}", [128, 2, T]) for i in range(2)]
                sg = [p2.sbuf(f"f_sg{i}", [128, T], BF16) for i in range(2)]
                for j in range(FC):
                    w = wb[j % 3]
                    p = ps[j % 2]
                    g_ = sg[j % 2]
                    for m in range(2):
                        K.dma(K.pool, w, w[:, m, :, :], wgu, wgu[j][:, m, :, :], partial=(m > 0))
                    for m in range(2):
                        for kc in range(DC):
                            for h in range(T // 512):
                                K.op(K.pe, lambda e: e.matmul(p[:, m, h * 512:(h + 1) * 512],
                                                              lhsT=w[:, m, kc, :],
                                                              rhs=hT[:, kc, h * 512:(h + 1) * 512],
                                                              start=(kc == 0), stop=(kc == DC - 1)),
                                     reads=[w, hT], writes=[p])
                    K.op(K.act, lambda e: e.activation(out=g_[:, :], in_=p[:, 0, :], func=AF.Silu),
                         reads=[p], writes=[g_])
                    K.op(K.dve, lambda e: e.tensor_tensor(out=hid[:, j, :], in0=p[:, 1, :], in1=g_[:, :],
                                                          op=ALU.mult),
                         reads=[p, g_], writes=[hid])
            down_phase(K, C, big, hid, FC, wd, yT, rstd2)
        epilogue_phase(K, C, x_src, yT, rstd2,
                       lambda c: gains[:, (gi + 1) * DC + c:(gi + 1) * DC + c + 1], x_dst, half=True)


def load_consts(K, st, gains_dram):
    C = {}
    C["ones"] = K.sbuf(st, "c_ones", [128, 128], BF16)
    C["eps"] = K.sbuf(st, "c_eps", [128, 1], F32)
    C["gains"] = K.sbuf(st, "c_gains", [128, 6 * DC], F32)
    K.op(K.dve, lambda e: e.memset(C["ones"][:, :], 1.0), writes=[C["ones"]])
    K.op(K.dve, lambda e: e.memset(C["eps"][:, :], EPS), writes=[C["eps"]])
    K.dma(K.sp, C["gains"], C["gains"][:, :], gains_dram, gains_dram[:, :])
    return C


def finish(K, out_bufs):
    K.sync_bufs(out_bufs, engs=[K.sp])


def tile_gu(wg, wu):
    a = wg.reshape(DC, 128, FC, 128).transpose(2, 1, 0, 3)
    b = wu.reshape(DC, 128, FC, 128).transpose(2, 1, 0, 3)
    return np.ascontiguousarray(np.stack([a, b], axis=2))


def tile_down(wd):
    return np.ascontiguousarray(wd.reshape(FC, 128, DC, 128).transpose(2, 1, 0, 3))


def gains_cols(g):
    return np.ascontiguousarray(g.reshape(6, DC, 128).transpose(2, 0, 1).reshape(128, 6 * DC))


def to_fm(xc):
    return np.ascontiguousarray(xc.T.reshape(DC, 128, T))


def from_fm(xt):
    return np.ascontiguousarray(xt.reshape(D, T).T)


NCH = S // 128
QTL = 8
LNSC = float(np.log(128.0 ** -0.5))


def cast_load_cols(K, buf, src, ncols, lead=None):
    for c0 in range(0, ncols, 2048):
        c1 = min(ncols, c0 + 2048)
        K.dma(K.pool, buf, buf[:, c0:c1], src, src[:, c0:c1], partial=(c0 > 0))


def cast_load_3d(K, buf, src, n, inner):
    step = max(1, 2048 // inner)
    for c0 in range(0, n, step):
        c1 = min(n, c0 + step)
        K.dma(K.pool, buf, buf[:, c0:c1, :], src, src[:, c0:c1, :], partial=(c0 > 0))


def conv_pool_stage(K, C, I, O):
    with Phase(K) as ph:
        cw = ph.sbuf("cw", [128, 4, 3], F32)
        psc = ph.sbuf("psc", [128, 4], F32)
        pw = ph.sbuf("pw", [128, 4, 128], BF16)
        K.dma(K.sp, cw, cw[:, :, :], I["conv_w"], I["conv_w"][:, :, :])
        K.dma(K.sp, psc, psc[:, :], I["pool_scale"], I["pool_scale"][:, :])
        K.dma(K.pool, pw, pw[:, :, :], I["pool_w"], I["pool_w"][:, :, :])
        for ch in range(4):
            cc = ph.sbuf("cc", [128, T + 2], F32)
            cu = ph.sbuf("cu", [128, T + 2], F32)
            cb = ph.sbuf("cb", [128, T], F32)
            acc = ph.sbuf("acc", [128, T], F32)
            K.dma(K.sp, cc, cc[:, :], I["cc"], I["cc"][ch])
            K.dma(K.sp, cu, cu[:, :], I["cu"], I["cu"][ch])
            K.dma(K.sp, cb, cb[:, :], I["cb"], I["cb"][ch])
            K.op(K.dve, lambda e: e.tensor_tensor(out=cc[:, :], in0=cc[:, :], in1=cu[:, :], op=ALU.mult),
                 reads=[cu, cc], writes=[cc])
            K.op(K.dve, lambda e: e.tensor_scalar(out=acc[:, :], in0=cc[:, 2:T + 2], scalar1=cw[:, ch, 2:3],
                                                  scalar2=None, op0=ALU.mult), reads=[cc, cw], writes=[acc])
            for j in (1, 0):
                K.op(K.dve, lambda e: e.scalar_tensor_tensor(out=acc[:, :], in0=cc[:, j:T + j],
                                                             scalar=cw[:, ch, j:j + 1], in1=acc[:, :],
                                                             op0=ALU.mult, op1=ALU.add),
                     reads=[cc, cw, acc], writes=[acc])
            K.op(K.dve, lambda e: e.tensor_tensor(out=acc[:, :], in0=acc[:, :], in1=cb[:, :], op=ALU.mult),
                 reads=[acc, cb], writes=[acc])
            K.dma(K.sp, O["y_conv"], O["y_conv"][ch], acc, acc[:, :], partial=True, owner=acc)
            u = ph.sbuf("pu", [128, T + 15], F32)
            sa = ph.sbuf("psa", [128, T + 15], F32)
            sb = ph.sbuf("psb", [128, T + 15], F32)
            ic = ph.sbuf("pic", [128, T], F32)
            pl = ph.sbuf("ppl", [128, T], BF16)
            yo = ph.sbuf("pyo", [128, T], F32)
            pps = ph.psum("pps", [128, T])
            K.dma(K.sp, u, u[:, :], I["pu"], I["pu"][ch])
            K.dma(K.sp, ic, ic[:, :], I["invcnt"], I["invcnt"][ch])
            cur, nxt = u, sa
            sh = 1
            for lvl in range(ch + 1):
                c_, n_ = cur, nxt
                K.op(K.dve, lambda e: e.tensor_tensor(out=n_[:, sh:T + 15], in0=c_[:, sh:T + 15],
                                                      in1=c_[:, 0:T + 15 - sh], op=ALU.add),
                     reads=[c_], writes=[n_])
                cur = nxt
                nxt = sb if cur is sa else sa
                sh *= 2
            ws = cur
            K.op(K.dve, lambda e: e.tensor_tensor(out=ws[:, 15:T + 15], in0=ws[:, 15:T + 15], in1=ic[:, :],
                                                  op=ALU.mult), reads=[ws, ic], writes=[ws])
            K.op(K.dve, lambda e: e.tensor_tensor(out=pl[:, :], in0=ws[:, 15:T + 15], in1=u[:, 15:T + 15],
                                                  op=ALU.subtract), reads=[ws, u], writes=[pl])
            for h in range(T // 512):
                K.op(K.pe, lambda e: e.matmul(pps[:, h * 512:(h + 1) * 512], lhsT=pw[:, ch, :],
                                              rhs=pl[:, h * 512:(h + 1) * 512], start=True, stop=True),
                     reads=[pw, pl], writes=[pps])
            K.op(K.act, lambda e: e.activation(out=yo[:, :], in_=pps[:, :], func=AF.Copy,
                                               scale=psc[:, ch:ch + 1]), reads=[pps, psc], writes=[yo])
            K.dma(K.sp, O["y_pool"], O["y_pool"][ch], yo, yo[:, :], partial=True, owner=yo)


def sb_stage(K, C, I, O):
    with Phase(K) as ph:
        qT = ph.sbuf("sqT", [128, QTL * 512], BF16)
        kT = ph.sbuf("skT", [128, S], BF16)
        v = ph.sbuf("sv", [128, NCH, 128], BF16)
        qpos = ph.sbuf("qpos", [128, QTL * 512], F32)
        kpos = ph.sbuf("kpos", [128, NCH], F32)
        qf = ph.sbuf("sqf", [128, QTL * 512], F32)
        K.dma(K.sp, qf, qf[:, :], I["sq"], I["sq"][:, :])
        K.op(K.dve, lambda e: e.tensor_scalar(out=qT[:, :], in0=qf[:, :], scalar1=float(128.0 ** -0.5),
                                              scalar2=None, op0=ALU.mult), reads=[qf], writes=[qT])
        cast_load_cols(K, kT, I["skT"], S)
        cast_load_3d(K, v, I["sv"], NCH, 128)
        K.dma(K.sp, qpos, qpos[:, :], I["qpos"], I["qpos"][:, :])
        K.dma(K.sp, kpos, kpos[:, :], I["kpos"], I["kpos"][:, :])
        zp = [ph.psum(f"szp{i}", [128, 512]) for i in range(2)]
        R = ph.psum("sR", [128, 512])
        Op = ph.psum("sO", [128, 512])
        e_ = [ph.sbuf(f"se{i}", [128, 512], F32) for i in range(2)]
        sp = [ph.sbuf(f"ssp{i}", [128, 512], F32) for i in range(2)]
        spb = [ph.sbuf(f"sspb{i}", [128, 512], BF16) for i in range(2)]
        tmp = [ph.sbuf(f"stmp{i}", [128, 512], F32) for i in range(2)]
        A = [ph.sbuf(f"sA{i}", [128, 512], BF16) for i in range(2)]
        Am = [ph.sbuf(f"sAm{i}", [128, 512], BF16) for i in range(2)]
        msk = [ph.sbuf(f"smk{i}", [128, 512], F32) for i in range(2)]
        osb = [ph.sbuf(f"sos{i}", [128, 512], F32) for i in range(2)]
        step = 0
        for i in range(QTL):
            nb = 8 * i + 8
            q_ = qT[:, i * 512:(i + 1) * 512]
            for bi, kb in enumerate(range(nb - 1, -1, -1)):
                masked = bi < 8
                z = zp[step % 2]; ee = e_[step % 2]; s_ = sp[step % 2]; sb_ = spb[step % 2]
                t_ = tmp[step % 2]; a_ = A[step % 2]; am_ = Am[step % 2]; m_ = msk[step % 2]
                step += 1
                K.op(K.pe, lambda e: e.matmul(z[:, :], lhsT=kT[:, kb * 128:(kb + 1) * 128], rhs=q_,
                                              start=True, stop=True), reads=[kT, qT], writes=[z])
                K.op(K.act, lambda e: e.activation(out=ee[:, :], in_=z[:, :], func=AF.Exp), reads=[z], writes=[ee])
                K.op(K.act, lambda e: e.activation(out=s_[:, :], in_=ee[:, :], func=AF.Ln, bias=C["one"][:, 0:1]),
                     reads=[ee, C["one"]], writes=[s_])
                if masked:
                    K.op(K.pool, lambda e: e.tensor_scalar(out=m_[:, :], in0=qpos[:, i * 512:(i + 1) * 512],
                                                           scalar1=kpos[:, kb:kb + 1], scalar2=None,
                                                           op0=ALU.is_gt), reads=[qpos, kpos], writes=[m_])
                    K.op(K.pool, lambda e: e.tensor_tensor(out=sb_[:, :], in0=s_[:, :], in1=m_[:, :], op=ALU.mult),
                         reads=[s_, m_], writes=[sb_])
                else:
                    K.op(K.pool, lambda e: e.tensor_copy(out=sb_[:, :], in_=s_[:, :]), reads=[s_], writes=[sb_])
                K.op(K.dve, lambda e: e.tensor_tensor(out=t_[:, :], in0=z[:, :], in1=s_[:, :], op=ALU.subtract),
                     reads=[z, s_], writes=[t_])
                K.op(K.pe, lambda e: e.matmul(R[:, :], lhsT=C["Ustrict"][:, :], rhs=sb_[:, :],
                                              start=(bi == 0), stop=False, skip_group_check=True),
                     reads=[C["Ustrict"], sb_], writes=[R])
                K.op(K.dve, lambda e: e.tensor_tensor(out=t_[:, :], in0=t_[:, :], in1=R[:, :], op=ALU.subtract),
                     reads=[t_, R], writes=[t_])
                K.op(K.pe, lambda e: e.matmul(R[:, :], lhsT=C["Lincl"][:, :], rhs=sb_[:, :],
                                              start=False, stop=(kb == 0), skip_group_check=True),
                     reads=[C["Lincl"], sb_], writes=[R])
                K.op(K.act, lambda e: e.activation(out=a_[:, :], in_=t_[:, :], func=AF.Exp), reads=[t_], writes=[a_])
                if masked:
                    K.op(K.pool, lambda e: e.tensor_tensor(out=am_[:, :], in0=a_[:, :], in1=m_[:, :], op=ALU.mult),
                         reads=[a_, m_], writes=[am_])
                    ause = am_
                else:
                    ause = a_
                K.op(K.pe, lambda e: e.matmul(Op[:, :], lhsT=v[:, kb, :], rhs=ause[:, :],
                                              start=(bi == 0), stop=(kb == 0), skip_group_check=True),
                     reads=[v, ause], writes=[Op])
            o_ = osb[i % 2]
            K.op(K.act, lambda e: e.activation(out=o_[:, :], in_=Op[:, :], func=AF.Copy), reads=[Op], writes=[o_])
            K.dma(K.sp, O["y_sb"], O["y_sb"][:, i * 512:(i + 1) * 512], o_, o_[:, :], partial=True, owner=o_)


def mlstm_stage(K, C, I, O):
    with Phase(K) as ph:
        qT = ph.sbuf("mqT", [128, S], BF16)
        kT = ph.sbuf("mkT", [128, S], BF16)
        kt = ph.sbuf("mk", [128, NCH, 128], F32)
        va = ph.sbuf("mva", [128, NCH, 129], BF16)
        gi = ph.sbuf("mgi", [128, NCH], F32)
        gf = ph.sbuf("mgf", [128, NCH], F32)
        bi_ = ph.sbuf("mbi", [128, 1], F32)
        bf_ = ph.sbuf("mbf", [128, 1], F32)
        hg = ph.sbuf("mhg", [128, 1], F32)
        cast_load_cols(K, qT, I["mqT"], S)
        cast_load_cols(K, kT, I["mkT"], S)
        for c0 in range(0, NCH, 16):
            K.dma(K.sp, kt, kt[:, c0:c0 + 16, :], I["mk"], I["mk"][:, c0:c0 + 16, :], partial=(c0 > 0))
        cast_load_3d(K, va, I["mva"], NCH, 129)
        K.dma(K.sp, gi, gi[:, :], I["mgi"], I["mgi"][:, :])
        K.dma(K.sp, gf, gf[:, :], I["mgf"], I["mgf"][:, :])
        K.dma(K.sp, bi_, bi_[:, :], I["mbi"], I["mbi"][:, :])
        K.dma(K.sp, bf_, bf_[:, :], I["mbf"], I["mbf"][:, :])
        K.dma(K.sp, hg, hg[:, :], I["mhg"], I["mhg"][:, :])
        lf = ph.sbuf("mlf", [128, NCH], F32)
        nbf = ph.sbuf("mnbf", [128, 1], F32)
        K.op(K.dve, lambda e: e.tensor_scalar(out=nbf[:, :], in0=bf_[:, :], scalar1=-1.0, scalar2=None,
                                              op0=ALU.mult), reads=[bf_], writes=[nbf])
        K.op(K.act, lambda e: e.activation(out=lf[:, :], in_=gf[:, :], func=AF.Exp, scale=-1.0,
                                           bias=nbf[:, 0:1]), reads=[gf, nbf], writes=[lf])
        K.op(K.act, lambda e: e.activation(out=lf[:, :], in_=lf[:, :], func=AF.Ln, bias=C["one"][:, 0:1]),
             reads=[lf, C["one"]], writes=[lf])
        K.op(K.dve, lambda e: e.tensor_scalar(out=lf[:, :], in0=lf[:, :], scalar1=-1.0, scalar2=None,
                                              op0=ALU.mult), reads=[lf], writes=[lf])
        gps = ph.psum("mgps", [128, 2, NCH])
        K.op(K.pe, lambda e: e.matmul(gps[:, 0, :], lhsT=C["TriF"][:, :], rhs=lf[:, :], start=True, stop=True),
             reads=[C["TriF"], lf], writes=[gps])
        K.op(K.pe, lambda e: e.matmul(gps[:, 1, :], lhsT=C["onesF"][:, :], rhs=lf[:, :], start=True, stop=True),
             reads=[C["onesF"], lf], writes=[gps])
        ig = ph.sbuf("mig", [128, NCH], F32)
        K.op(K.dve, lambda e: e.tensor_scalar(out=ig[:, :], in0=gi[:, :], scalar1=bi_[:, 0:1], scalar2=None,
                                              op0=ALU.add), reads=[gi, bi_], writes=[ig])
        imb = ph.sbuf("mimb", [128, NCH], F32)
        K.op(K.dve, lambda e: e.tensor_tensor(out=imb[:, :], in0=ig[:, :], in1=gps[:, 0, :], op=ALU.subtract),
             reads=[ig, gps], writes=[imb])
        ek = ph.sbuf("mek", [128, NCH], F32)
        K.op(K.act, lambda e: e.activation(out=ek[:, :], in_=imb[:, :], func=AF.Exp, bias=C["lnsc"][:, 0:1]),
             reads=[imb, C["lnsc"]], writes=[ek])
        wk = ph.sbuf("mwk", [128, NCH], F32)
        K.op(K.dve, lambda e: e.tensor_tensor(out=wk[:, :], in0=imb[:, :], in1=gps[:, 1, :], op=ALU.add),
             reads=[imb, gps], writes=[wk])
        K.op(K.act, lambda e: e.activation(out=wk[:, :], in_=wk[:, :], func=AF.Exp, bias=C["lnsc"][:, 0:1]),
             reads=[wk, C["lnsc"]], writes=[wk])
        dec = ph.sbuf("mdec", [128, NCH], F32)
        K.op(K.act, lambda e: e.activation(out=dec[:, :], in_=gps[:, 1, :], func=AF.Exp), reads=[gps], writes=[dec])
        Cst = ph.sbuf("mC", [128, 129], F32)
        Cbf = ph.sbuf("mCbf", [128, 128], BF16)
        nbc = ph.sbuf("mnbc", [128, 128], BF16)
        K.op(K.dve, lambda e: e.memset(Cst[:, :], 0.0), writes=[Cst])
        K.op(K.dve, lambda e: e.memset(Cbf[:, :], 0.0), writes=[Cbf])
        K.op(K.dve, lambda e: e.memset(nbc[:, :], 0.0), writes=[nbc])
        sps = ph.psum("msps", [128, 128])
        bps = ph.psum("mbps", [128, 128])
        nps = ph.psum("mnps", [128, 128])
        dps = ph.psum("mdps", [128, 128])
        kvps = ph.psum("mkvps", [128, 129])
        mps = ph.psum("mmps", [128, 128])
        lfb = ph.sbuf("mlfb", [128, 128], F32)
        embt = ph.sbuf("membt", [128, 128], F32)
        PT = ph.sbuf("mPT", [128, 128], BF16)
        k2 = ph.sbuf("mk2", [128, 128], BF16)
        dm = ph.sbuf("mdm", [128, 128], F32)
        hh = ph.sbuf("mhh", [128, 128], F32)
        sq = ph.sbuf("msq", [128, 128], BF16)
        rt = ph.sbuf("mrt", [128, 128], F32)
        og = ph.sbuf("mog", [128, 128], F32)
        osg = ph.sbuf("mosg", [128, 128], F32)
        yo = [ph.sbuf(f"myo{i}", [128, 128], F32) for i in range(2)]
        for k in range(NCH):
            ts_ = slice(k * 128, (k + 1) * 128)
            K.dma(K.sp, og, og[:, :], I["moT"], I["moT"][:, ts_])
            K.op(K.pe, lambda e: e.matmul(sps[:, :], lhsT=kT[:, ts_], rhs=qT[:, ts_], start=True, stop=True),
                 reads=[kT, qT], writes=[sps])
            K.op(K.dve, lambda e: e.scalar_tensor_tensor(out=PT[:, :], in0=sps[:, :], scalar=ek[:, k:k + 1],
                                                         in1=C["causal"][:, :], op0=ALU.mult, op1=ALU.mult),
                 reads=[sps, ek, C["causal"]], writes=[PT])
            K.op(K.dve, lambda e: e.tensor_scalar(out=lfb[:, :], in0=C["onesF"][:, :], scalar1=lf[:, k:k + 1],
                                                  scalar2=None, op0=ALU.mult), reads=[C["onesF"], lf], writes=[lfb])
            K.op(K.pe, lambda e: e.matmul(bps[:, :], lhsT=lfb[:, :], rhs=C["TriF"][:, :], start=True, stop=True),
                 reads=[lfb, C["TriF"]], writes=[bps])
            K.op(K.act, lambda e: e.activation(out=embt[:, :], in_=bps[:, :], func=AF.Exp, scale=-1.0),
                 reads=[bps], writes=[embt])
            K.op(K.pe, lambda e: e.matmul(nps[:, :], lhsT=va[:, k, 0:128], rhs=PT[:, :], start=True, stop=False),
                 reads=[va, PT], writes=[nps])
            K.op(K.pe, lambda e: e.matmul(nps[:, :], lhsT=Cbf[:, :], rhs=qT[:, ts_], start=False, stop=True),
                 reads=[Cbf, qT], writes=[nps])
            K.op(K.pe, lambda e: e.matmul(dps[:, :], lhsT=C["ones"][:, :], rhs=PT[:, :], start=True, stop=False),
                 reads=[C["ones"], PT], writes=[dps])
            K.op(K.pe, lambda e: e.matmul(dps[:, :], lhsT=nbc[:, :], rhs=qT[:, ts_], start=False, stop=True),
                 reads=[nbc, qT], writes=[dps])
            K.op(K.act, lambda e: e.activation(out=dm[:, :], in_=dps[:, :], func=AF.Abs), reads=[dps], writes=[dm])
            K.op(K.dve, lambda e: e.tensor_tensor(out=dm[:, :], in0=dm[:, :], in1=embt[:, :], op=ALU.max),
                 reads=[dm, embt], writes=[dm])
            K.op(K.dve, lambda e: e.reciprocal(out=dm[:, :], in_=dm[:, :]), reads=[dm], writes=[dm])
            K.op(K.dve, lambda e: e.tensor_tensor(out=hh[:, :], in0=nps[:, :], in1=dm[:, :], op=ALU.mult),
                 reads=[nps, dm], writes=[hh])
            K.op(K.dve, lambda e: e.tensor_scalar(out=k2[:, :], in0=kt[:, k, :], scalar1=wk[:, k:k + 1],
                                                  scalar2=None, op0=ALU.mult), reads=[kt, wk], writes=[k2])
            K.op(K.pe, lambda e: e.matmul(kvps[:, :], lhsT=k2[:, :], rhs=va[:, k, :], start=True, stop=True),
                 reads=[k2, va], writes=[kvps])
            K.op(K.dve, lambda e: e.scalar_tensor_tensor(out=Cst[:, :], in0=Cst[:, :], scalar=dec[:, k:k + 1],
                                                         in1=kvps[:, :], op0=ALU.mult, op1=ALU.add),
                 reads=[Cst, dec, kvps], writes=[Cst])
            K.op(K.act, lambda e: e.activation(out=Cbf[:, :], in_=Cst[:, 0:128], func=AF.Copy),
                 reads=[Cst], writes=[Cbf])
            K.op(K.pool, lambda e: e.tensor_scalar(out=nbc[:, :], in0=C["onesF"][:, :], scalar1=Cst[:, 128:129],
                                                   scalar2=None, op0=ALU.mult), reads=[C["onesF"], Cst], writes=[nbc])
            K.op(K.act, lambda e: e.activation(out=sq[:, :], in_=hh[:, :], func=AF.Square), reads=[hh], writes=[sq])
            K.op(K.pe, lambda e: e.matmul(mps[:, :], lhsT=C["ones"][:, :], rhs=sq[:, :], start=True, stop=True),
                 reads=[C["ones"], sq], writes=[mps])
            K.op(K.act, lambda e: e.activation(out=rt[:, :], in_=mps[:, :], func=AF.Sqrt, scale=1.0 / 128,
                                               bias=C["eps"][:, 0:1]), reads=[mps, C["eps"]], writes=[rt])
            K.op(K.dve, lambda e: e.reciprocal(out=rt[:, :], in_=rt[:, :]), reads=[rt], writes=[rt])
            K.op(K.dve, lambda e: e.scalar_tensor_tensor(out=hh[:, :], in0=hh[:, :], scalar=hg[:, 0:1],
                                                         in1=rt[:, :], op0=ALU.mult, op1=ALU.mult),
                 reads=[hh, hg, rt], writes=[hh])
            K.op(K.act, lambda e: e.activation(out=osg[:, :], in_=og[:, :], func=AF.Sigmoid), reads=[og], writes=[osg])
            y_ = yo[k % 2]
            K.op(K.dve, lambda e: e.tensor_tensor(out=y_[:, :], in0=hh[:, :], in1=osg[:, :], op=ALU.mult),
                 reads=[hh, osg], writes=[y_])
            K.dma(K.sp, O["y_ml"], O["y_ml"][:, ts_], y_, y_[:, :], partial=True, owner=y_)


def load_mixer_consts(K, st, C, I):
    for name, dt in (("Ustrict", BF16), ("Lincl", BF16), ("causal", F32), ("TriF", F32), ("onesF", F32)):
        C[name] = K.sbuf(st, "c_" + name, [128, 128], dt)
        q = K.pool if dt == BF16 else K.sp
        K.dma(q, C[name], C[name][:, :], I["k_" + name], I["k_" + name][:, :])
    C["one"] = K.sbuf(st, "c_one", [128, 1], F32)
    C["lnsc"] = K.sbuf(st, "c_lnsc", [128, 1], F32)
    K.op(K.dve, lambda e: e.memset(C["one"][:, :], 1.0), writes=[C["one"]])
    K.op(K.dve, lambda e: e.memset(C["lnsc"][:, :], LNSC), writes=[C["lnsc"]])


def mixer_consts_host():
    j = np.arange(128)[:, None]
    s = np.arange(128)[None, :]
    return {
        "k_Ustrict": (j > s).astype(np.float32),
        "k_Lincl": (j <= s).astype(np.float32),
        "k_causal": (j <= s).astype(np.float32),
        "k_TriF": (j <= s).astype(np.float32),
        "k_onesF": np.ones((128, 128), np.float32),
    }


OFF = dict(cb=0, cc=512, cu=1024, mq=1536, mk=2048, mv=2560, mo=3072, mi=3584, mf=3588, pu=3592,
           sq=4104, sk=4616, sv=5128)


def _fm_halo(a, c, halo):
    lo = c * T - halo
    if lo < 0:
        blk = np.concatenate([np.zeros((-lo, a.shape[1]), a.dtype), a[0:(c + 1) * T]], axis=0)
    else:
        blk = a[lo:(c + 1) * T]
    return np.ascontiguousarray(blk.T.reshape(4, 128, halo + T))


def sb_tiles(c):
    r = c // 4
    return [2 * i + r for i in range(QTL)]


def mixer_inputs(proj, conv_w, pool_w, pool_scale, i_bias, f_bias, head_gain, c):
    h = c % 4
    hs = slice(h * 128, (h + 1) * 128)
    g = lambda name: proj[:, OFF[name]:OFF[name] + 512]
    I = {}
    I["cb"] = _fm_halo(g("cb"), c, 0)
    I["cc"] = _fm_halo(g("cc"), c, 2)
    I["cu"] = _fm_halo(g("cu"), c, 2)
    I["pu"] = _fm_halo(g("pu"), c, 15)
    I["conv_w"] = np.ascontiguousarray(conv_w.reshape(3, 4, 128).transpose(2, 1, 0))
    I["pool_w"] = np.ascontiguousarray(pool_w.transpose(1, 0, 2))
    I["pool_scale"] = np.ascontiguousarray(pool_scale.reshape(4, 128).T)
    t = np.arange(c * T, (c + 1) * T)
    cnt = np.stack([np.minimum(t + 1, w) for w in (2, 4, 8, 16)], axis=0).astype(np.float32)
    I["invcnt"] = np.ascontiguousarray(np.broadcast_to((1.0 / cnt)[:, None, :], (4, 128, T))).astype(np.float32)
    mq, mk, mv, mo = g("mq")[:, hs], g("mk")[:, hs], g("mv")[:, hs], g("mo")[:, hs]
    I["mqT"] = np.ascontiguousarray(mq.T)
    I["mkT"] = np.ascontiguousarray(mk.T)
    I["mk"] = np.ascontiguousarray(mk.reshape(NCH, 128, 128).transpose(1, 0, 2))
    va = np.concatenate([mv, np.ones((S, 1), np.float32)], axis=1)
    I["mva"] = np.ascontiguousarray(va.reshape(NCH, 128, 129).transpose(1, 0, 2))
    I["moT"] = np.ascontiguousarray(mo.T)
    I["mgi"] = np.ascontiguousarray(proj[:, OFF["mi"] + h].reshape(NCH, 128).T)
    I["mgf"] = np.ascontiguousarray(proj[:, OFF["mf"] + h].reshape(NCH, 128).T)
    I["mbi"] = np.full((128, 1), i_bias[h], np.float32)
    I["mbf"] = np.full((128, 1), f_bias[h], np.float32)
    I["mhg"] = np.ascontiguousarray(head_gain[hs].reshape(128, 1))
    sq, sk, sv = g("sq")[:, hs], g("sk")[:, hs], g("sv")[:, hs]
    tiles = sb_tiles(c)
    qsel = np.concatenate([sq[ti * 512:(ti + 1) * 512] for ti in tiles], axis=0)
    I["sq"] = np.ascontiguousarray(qsel.T)
    I["skT"] = np.ascontiguousarray(sk.T)
    I["sv"] = np.ascontiguousarray(sv.reshape(NCH, 128, 128).transpose(1, 0, 2))
    qp = np.concatenate([np.arange(ti * 512, (ti + 1) * 512) for ti in tiles]).astype(np.float32)
    I["qpos"] = np.ascontiguousarray(np.broadcast_to(qp[None, :], (128, QTL * 512)))
    I["kpos"] = np.ascontiguousarray((np.arange(NCH)[None, :] * 128 + np.arange(128)[:, None]).astype(np.float32))
    I.update(mixer_consts_host())
    return I


MIX_IN_SHAPES = dict(cb=[4, 128, T], cc=[4, 128, T + 2], cu=[4, 128, T + 2], pu=[4, 128, T + 15],
                     conv_w=[128, 4, 3], pool_w=[128, 4, 128], pool_scale=[128, 4], invcnt=[4, 128, T],
                     mqT=[128, S], mkT=[128, S], mk=[128, NCH, 128], mva=[128, NCH, 129], moT=[128, S],
                     mgi=[128, NCH], mgf=[128, NCH], mbi=[128, 1], mbf=[128, 1], mhg=[128, 1],
                     sq=[128, QTL * 512], skT=[128, S], sv=[128, NCH, 128], qpos=[128, QTL * 512],
                     kpos=[128, NCH], k_Ustrict=[128, 128], k_Lincl=[128, 128], k_causal=[128, 128],
                     k_TriF=[128, 128], k_onesF=[128, 128])
MIX_OUT_SHAPES = dict(y_conv=[4, 128, T], y_pool=[4, 128, T], y_ml=[128, S], y_sb=[128, QTL * 512])


def build_mixer_prog(parts=("cp", "ml", "sb")):
    nc = bass.Bass("TRN2", target_bir_lowering=False)
    with ExitStack() as es:
        K = Kern(nc, es)
        I = {k: K.dram(k, v, F32, "ExternalInput") for k, v in MIX_IN_SHAPES.items()}
        O = {k: K.dram(k, v, F32, "ExternalOutput") for k, v in MIX_OUT_SHAPES.items()}
        C = {}
        C["ones"] = K.sbuf(es, "c_ones", [128, 128], BF16)
        C["eps"] = K.sbuf(es, "c_eps", [128, 1], F32)
        K.op(K.dve, lambda e: e.memset(C["ones"][:, :], 1.0), writes=[C["ones"]])
        K.op(K.dve, lambda e: e.memset(C["eps"][:, :], EPS), writes=[C["eps"]])
        load_mixer_consts(K, es, C, I)
        if "cp" in parts:
            conv_pool_stage(K, C, I, O)
        if "ml" in parts:
            mlstm_stage(K, C, I, O)
        if "sb" in parts:
            sb_stage(K, C, I, O)
        finish(K, list(O.values()))
    return nc


def assemble_mix(results):
    y = np.zeros((S, D), np.float32)
    for c, r in enumerate(results):
        ts = slice(c * T, (c + 1) * T)
        y[ts, 0:512] = r["y_conv"].reshape(512, T).T
        y[ts, 1024:1536] = r["y_pool"].reshape(512, T).T
        h = c % 4
        if c < 4:
            y[:, 512 + h * 128:512 + (h + 1) * 128] = r["y_ml"].T
        for i, ti in enumerate(sb_tiles(c)):
            y[ti * 512:(ti + 1) * 512, 1536 + h * 128:1536 + (h + 1) * 128] = r["y_sb"][:, i * 512:(i + 1) * 512].T
    return y


NPJ = 45


def inproj_stage(K, C, x_src, projT, win, gi):
    gains = C["gains"]
    with Phase(K) as p1:
        hT = p1.sbuf("i_hT", [128, DC, T], BF16)
        norm_in_phase(K, C, x_src, lambda c: gains[:, gi * DC + c:gi * DC + c + 1], hT)
        with Phase(K) as p2:
            wb = [p2.sbuf(f"i_wb{i}", [128, DC, 128], BF16) for i in range(3)]
            ps = [p2.psum(f"i_ps{i}", [128, T]) for i in range(2)]
            ob = [p2.sbuf(f"i_ob{i}", [128, T], F32) for i in range(2)]
            for j in range(NPJ):
                w = wb[j % 3]; p = ps[j % 2]; o_ = ob[j % 2]
                K.dma(K.pool, w, w[:, :, :], win, win[j])
                for kc in range(DC):
                    for h in range(T // 512):
                        K.op(K.pe, lambda e: e.matmul(p[:, h * 512:(h + 1) * 512], lhsT=w[:, kc, :],
                                                      rhs=hT[:, kc, h * 512:(h + 1) * 512],
                                                      start=(kc == 0), stop=(kc == DC - 1)),
                             reads=[w, hT], writes=[p])
                K.op(K.act, lambda e: e.activation(out=o_[:, :], in_=p[:, :], func=AF.Copy), reads=[p], writes=[o_])
                K.dma(K.sp, projT, projT[j], o_, o_[:, :], partial=True, owner=o_)


def outproj_stage(K, C, x_src, ymix, wout, yT, x_dst, gi):
    gains = C["gains"]
    with Phase(K) as p1:
        yb = p1.sbuf("o_yb", [128, DC, T], BF16)
        rstd = p1.sbuf("o_rstd", [128, T], F32)
        for c in range(DC):
            K.dma(K.pool, yb, yb[:, c, :], ymix, ymix[c], partial=(c > 0))
        down_phase(K, C, p1, yb, DC, wout, yT, rstd)
        epilogue_phase(K, C, x_src, yT, rstd, lambda c: gains[:, gi * DC + c:gi * DC + c + 1], x_dst, half=False)


def build_pa():
    nc = bass.Bass("TRN2", target_bir_lowering=False)
    with ExitStack() as es:
        K = Kern(nc, es)
        x_in = K.dram("x_in", [DC, 128, T], F32, "ExternalInput")
        wgu = K.dram("wgu", [FC, 128, 2, DC, 128], F32, "ExternalInput")
        wd = K.dram("wd", [DC, 128, FC, 128], F32, "ExternalInput")
        win = K.dram("win", [NPJ, 128, DC, 128], F32, "ExternalInput")
        gains = K.dram("gains", [128, 6 * DC], F32, "ExternalInput")
        x_out = K.dram("x_out", [DC, 128, T], F32, "ExternalOutput")
        projT = K.dram("projT", [NPJ, 128, T], F32, "ExternalOutput")
        yT = K.dram("yT", [DC, 128, T], F32, "Internal")
        C = load_consts(K, es, gains)
        ffn_stage(K, C, x_in, x_out, yT, wgu, wd, 0)
        inproj_stage(K, C, x_out, projT, win, 2)
        finish(K, [x_out, projT])
    return nc


def build_pc():
    nc = bass.Bass("TRN2", target_bir_lowering=False)
    with ExitStack() as es:
        K = Kern(nc, es)
        x_in = K.dram("x_in", [DC, 128, T], F32, "ExternalInput")
        ymix = K.dram("ymix", [DC, 128, T], F32, "ExternalInput")
        wout = K.dram("wout", [DC, 128, DC, 128], F32, "ExternalInput")
        wgu = K.dram("wgu", [FC, 128, 2, DC, 128], F32, "ExternalInput")
        wd = K.dram("wd", [DC, 128, FC, 128], F32, "ExternalInput")
        gains = K.dram("gains", [128, 6 * DC], F32, "ExternalInput")
        x_out = K.dram("x_out", [DC, 128, T], F32, "ExternalOutput")
        x_mid = K.dram("x_mid", [DC, 128, T], F32, "Internal")
        yT = K.dram("yT", [DC, 128, T], F32, "Internal")
        C = load_consts(K, es, gains)
        outproj_stage(K, C, x_in, ymix, wout, yT, x_mid, 3)
        ffn_stage(K, C, x_mid, x_out, yT, wgu, wd, 4)
        finish(K, [x_out])
    return nc


def tile_kn(w, nk, nn):
    return np.ascontiguousarray(w.reshape(nk, 128, nn, 128).transpose(2, 1, 0, 3))


_PROGS = {}


def _prog(name, fn):
    if name not in _PROGS:
        _PROGS[name] = fn()
    return _PROGS[name]


def kernel(x, w_in, w_out, conv_w, pool_w, pool_scale, mlstm_i_bias, mlstm_f_bias, mlstm_head_gain,
           ffn1_w_gate, ffn1_w_up, ffn1_w_down, ffn2_w_gate, ffn2_w_up, ffn2_w_down, norm_gains):
    f = lambda a: np.asarray(a, dtype=np.float32)
    x = f(x)
    cores = list(range(NCORES))
    xs = [to_fm(x[0, c * T:(c + 1) * T]) for c in cores]
    pa = _prog("pa", build_pa)
    pb = _prog("pb", build_mixer_prog)
    pc = _prog("pc", build_pc)
    for l in range(DEPTH):
        gc = gains_cols(f(norm_gains[l]))
        wgu1 = tile_gu(f(ffn1_w_gate[l]), f(ffn1_w_up[l]))
        wd1 = tile_down(f(ffn1_w_down[l]))
        winp = np.zeros((D, NPJ * 128), np.float32)
        winp[:, :DIN] = f(w_in[l])
        win_t = tile_kn(winp, DC, NPJ)
        res = run_bass_kernel_spmd(pa, [{"x_in": xs[c], "wgu": wgu1, "wd": wd1, "win": win_t, "gains": gc}
                                        for c in cores], core_ids=cores)
        xs = [r["x_out"] for r in res.results]
        proj = np.concatenate([r["projT"].reshape(NPJ * 128, T).T[:, :DIN] for r in res.results], axis=0)
        del res, wgu1, wd1, win_t, winp
        proj = np.ascontiguousarray(proj)
        ims = [mixer_inputs(proj, f(conv_w[l]), f(pool_w[l]), f(pool_scale[l]), f(mlstm_i_bias[l]),
                            f(mlstm_f_bias[l]), f(mlstm_head_gain[l]), c) for c in cores]
        res = run_bass_kernel_spmd(pb, ims, core_ids=cores)
        y = assemble_mix(res.results)
        del res, ims, proj
        wgu2 = tile_gu(f(ffn2_w_gate[l]), f(ffn2_w_up[l]))
        wd2 = tile_down(f(ffn2_w_down[l]))
        wo_t = tile_kn(f(w_out[l]), DC, DC)
        res = run_bass_kernel_spmd(pc, [{"x_in": xs[c], "ymix": to_fm(y[c * T:(c + 1) * T]), "wout": wo_t,
                                         "wgu": wgu2, "wd": wd2, "gains": gc} for c in cores], core_ids=cores)
        xs = [r["x_out"] for r in res.results]
        del res, wgu2, wd2, wo_t
    out = np.concatenate([from_fm(xc) for xc in xs], axis=0)[None]
    return np.ascontiguousarray(out.astype(np.float32))
```

```python
import os
import numpy as np
import ml_dtypes
from contextlib import ExitStack
import concourse.bass as bass
import concourse.mybir as mybir
from concourse.bass_utils import run_bass_kernel_spmd

F32 = mybir.dt.float32
BF16 = mybir.dt.bfloat16
AF = mybir.ActivationFunctionType
ALU = mybir.AluOpType

NCORES = 8
D = 2048
DC = D // 128
S = 8192
T = S // NCORES
DFF = 5632
FC = DFF // 128
DEPTH = 4
EPS = 1e-6
G = 512
DIN = 5640


class Buf:
    def __init__(self, t, name=""):
        self.t = t
        self.name = name
        self.w = {}
        self.r = {}
        self.dsem = None
        self.dcount = 0

    def __getitem__(self, idx):
        return self.t[idx]


class Eng:
    def __init__(self, K, name, h):
        self.K = K
        self.name = name
        self.h = h
        self.sem = K.es.enter_context(K.nc.semaphore("e_" + name))
        self.n = 0
        self.seen = {}

    def wait(self, tok):
        if tok is None:
            return
        sem, val = tok
        if sem is self.sem and self.name == "pe":
            return
        k = id(sem)
        if self.seen.get(k, 0) >= val:
            return
        self.h.wait_ge(sem, val)
        self.seen[k] = val


class Kern:
    def __init__(self, nc, es):
        self.nc = nc
        self.es = es
        self.pe = Eng(self, "pe", nc.tensor)
        self.act = Eng(self, "act", nc.scalar)
        self.dve = Eng(self, "dve", nc.vector)
        self.pool = Eng(self, "pool", nc.gpsimd)
        self.sp = Eng(self, "sp", nc.sync)
        self.engs = [self.pe, self.act, self.dve, self.pool, self.sp]
        self.free_sems = []
        self.semcnt = {}

    def op(self, eng, fn, reads=(), writes=()):
        for b in reads:
            for tok in b.w.values():
                eng.wait(tok)
        for b in writes:
            for tok in b.w.values():
                eng.wait(tok)
            for tok in b.r.values():
                eng.wait(tok)
        ins = fn(eng.h)
        eng.n += 1
        ins.then_inc(eng.sem, 1)
        tok = (eng.sem, eng.n)
        for b in reads:
            b.r[id(eng.sem)] = tok
        for b in writes:
            b.w = {id(eng.sem): tok}
            b.r = {}
        return ins

    def get_dsem(self, name):
        if self.free_sems:
            return self.free_sems.pop()
        return self.es.enter_context(self.nc.semaphore(self.uniq("d_" + name)))

    def dma(self, q, ob, out_ap, ib, in_ap, partial=False, owner=None, **kw):
        own = owner or ob
        for tok in ib.w.values():
            q.wait(tok)
        if not partial:
            for tok in ob.w.values():
                q.wait(tok)
        for tok in ob.r.values():
            q.wait(tok)
        if own.dsem is None:
            own.dsem = self.get_dsem(own.name)
        cnt = self.semcnt.get(id(own.dsem), 0) + 1
        self.semcnt[id(own.dsem)] = cnt
        ins = q.h.dma_start(out=out_ap, in_=in_ap, **kw)
        ins.then_inc(own.dsem, 16)
        tok = (own.dsem, 16 * cnt)
        ib.r[id(own.dsem)] = tok
        if partial:
            ob.w[id(own.dsem)] = tok
        else:
            ob.w = {id(own.dsem): tok}
        ob.r = {}
        return ins

    def collective(self, kind, ib, in_ap, ob, out_ap, first=True):
        q = self.pool
        for tok in ib.w.values():
            q.wait(tok)
        if first:
            for tok in ob.w.values():
                q.wait(tok)
            for tok in ob.r.values():
                q.wait(tok)
        if not hasattr(self, "ccsem"):
            self.ccsem = self.es.enter_context(self.nc.semaphore("ccsem"))
            self.cccnt = 0
        self.cccnt += 1
        ins = q.h.collective_compute(kind, ALU.bypass, replica_groups=[list(range(NCORES))],
                                     ins=[in_ap], outs=[out_ap])
        ins.then_inc(self.ccsem, 1)
        tok = (self.ccsem, self.cccnt)
        ib.r[id(self.ccsem)] = tok
        if first:
            ob.w = {id(self.ccsem): tok}
        else:
            ob.w[id(self.ccsem)] = tok
        ob.r = {}

    def dma_sel(self, ob, ib, variants_fn, owner=None, partial=False, nvar=NCORES):
        q = self.pool
        own = owner or ob
        for tok in ib.w.values():
            q.wait(tok)
        if not partial:
            for tok in ob.w.values():
                q.wait(tok)
        for tok in ob.r.values():
            q.wait(tok)
        if own.dsem is None:
            own.dsem = self.get_dsem(own.name)
        lists = [variants_fn(v) for v in range(nvar)]
        n = len(lists[0])
        assert all(len(l) == n for l in lists)
        if not hasattr(self, "pid"):
            self.pid = q.h.partition_id()
        for v in range(nvar):
            with q.h.If(self.pid == v):
                for (oa, ia) in lists[v]:
                    q.h.dma_start(out=oa, in_=ia).then_inc(own.dsem, 16)
        cnt = self.semcnt.get(id(own.dsem), 0) + n
        self.semcnt[id(own.dsem)] = cnt
        tok = (own.dsem, 16 * cnt)
        ib.r[id(own.dsem)] = tok
        if partial:
            ob.w[id(own.dsem)] = tok
        else:
            ob.w = {id(own.dsem): tok}
        ob.r = {}

    def sync_bufs(self, bufs, engs=None):
        for e in (engs or self.engs):
            for b in bufs:
                for tok in b.w.values():
                    e.wait(tok)
                for tok in b.r.values():
                    e.wait(tok)

    def uniq(self, name):
        self.uid = getattr(self, "uid", 0) + 1
        return f"{name}_{self.uid}"

    def sbuf(self, st, name, shape, dt):
        t = st.enter_context(self.nc.sbuf_tensor(self.uniq(name), shape, dt))
        return Buf(t, name)

    def psum(self, st, name, shape, dt=F32):
        t = st.enter_context(self.nc.psum_tensor(self.uniq(name), shape, dt))
        return Buf(t, name)

    def dram(self, name, shape, dt, kind="Internal"):
        return Buf(self.nc.dram_tensor(name, shape, dt, kind=kind).ap(), name)


class Phase:
    def __init__(self, K):
        self.K = K
        self.st = ExitStack()
        self.bufs = []

    def __enter__(self):
        self.st.__enter__()
        return self

    def sbuf(self, name, shape, dt):
        b = self.K.sbuf(self.st, name, shape, dt)
        self.bufs.append(b)
        return b

    def psum(self, name, shape, dt=F32):
        b = self.K.psum(self.st, name, shape, dt)
        self.bufs.append(b)
        return b

    def __exit__(self, *a):
        self.K.sync_bufs(self.bufs)
        for b in self.bufs:
            if b.dsem is not None:
                self.K.free_sems.append(b.dsem)
        return self.st.__exit__(*a)


def norm_in_phase(K, C, x_src, gcol, hT):
    with Phase(K) as ph:
        xs = ph.sbuf("n_xs", [128, DC, T], F32)
        sq = [ph.sbuf(f"n_sq{i}", [128, T], BF16) for i in range(2)]
        ss = ph.psum("n_ss", [128, T])
        rstd = ph.sbuf("n_rstd", [128, T], F32)
        for c in range(DC):
            K.dma(K.sp, xs, xs[:, c, :], x_src, x_src[c], partial=True)
        for c in range(DC):
            s = sq[c % 2]
            K.op(K.act, lambda e: e.activation(out=s[:, :], in_=xs[:, c, :], func=AF.Square),
                 reads=[xs], writes=[s])
            for h in range(T // 512):
                K.op(K.pe, lambda e: e.matmul(ss[:, h * 512:(h + 1) * 512], lhsT=C["ones"][:, :],
                                              rhs=s[:, h * 512:(h + 1) * 512],
                                              start=(c == 0), stop=(c == DC - 1)),
                     reads=[C["ones"], s], writes=[ss])
        rstd_from_ss(K, C, ph, ss, rstd)
        for c in range(DC):
            K.op(K.dve, lambda e: e.scalar_tensor_tensor(out=hT[:, c, :], in0=xs[:, c, :],
                                                         scalar=gcol(c), in1=rstd[:, :],
                                                         op0=ALU.mult, op1=ALU.mult),
                 reads=[xs, rstd, C["gains"]], writes=[hT])


def rstd_from_ss(K, C, ph, ss, rstd):
    rt = ph.sbuf("r_rt", [128, T], F32)
    K.op(K.act, lambda e: e.activation(out=rt[:, :], in_=ss[:, :], func=AF.Sqrt,
                                       scale=1.0 / D, bias=C["eps"][:, 0:1]),
         reads=[ss, C["eps"]], writes=[rt])
    K.op(K.dve, lambda e: e.reciprocal(out=rstd[:, :], in_=rt[:, :]), reads=[rt], writes=[rstd])


def epilogue_phase(K, C, x_src, y_src, rstd, gcol, x_dst, half):
    with Phase(K) as ph:
        xb = [ph.sbuf(f"e_x{i}", [128, T], F32) for i in range(2)]
        yb = [ph.sbuf(f"e_y{i}", [128, T], F32) for i in range(2)]
        for c in range(DC):
            x_, y_ = xb[c % 2], yb[c % 2]
            K.dma(K.sp, x_, x_[:, :], x_src, x_src[c])
            K.dma(K.sp, y_, y_[:, :], y_src, y_src[c])
            K.op(K.dve, lambda e: e.scalar_tensor_tensor(out=y_[:, :], in0=y_[:, :], scalar=gcol(c),
                                                         in1=rstd[:, :], op0=ALU.mult, op1=ALU.mult),
                 reads=[y_, rstd, C["gains"]], writes=[y_])
            if half:
                K.op(K.dve, lambda e: e.scalar_tensor_tensor(out=x_[:, :], in0=y_[:, :], scalar=0.5,
                                                             in1=x_[:, :], op0=ALU.mult, op1=ALU.add),
                     reads=[y_, x_], writes=[x_])
            else:
                K.op(K.dve, lambda e: e.tensor_tensor(out=x_[:, :], in0=y_[:, :], in1=x_[:, :], op=ALU.add),
                     reads=[y_, x_], writes=[x_])
            K.dma(K.sp, x_dst, x_dst[c], x_, x_[:, :], partial=True, owner=x_)


def down_phase(K, C, ph_outer, rhsT, nk, wd, yT, rstd):
    with Phase(K) as p4:
        ss = p4.psum("f_ss", [128, T])
        db = [p4.sbuf(f"f_db{i}", [128, nk, 128], BF16) for i in range(2)]
        po = [p4.psum(f"f_po{i}", [128, T]) for i in range(2)]
        ysb = [p4.sbuf(f"f_y{i}", [128, T], F32) for i in range(2)]
        sq = [p4.sbuf(f"f_sq{i}", [128, T], BF16) for i in range(2)]
        for i in range(DC):
            w = db[i % 2]
            p = po[i % 2]
            y_ = ysb[i % 2]
            s_ = sq[i % 2]
            for f0 in range(0, nk, 16):
                f1 = min(nk, f0 + 16)
                K.dma(K.pool, w, w[:, f0:f1, :], wd, wd[i][:, f0:f1, :], partial=(f0 > 0))
            for fc in range(nk):
                for h in range(T // 512):
                    K.op(K.pe, lambda e: e.matmul(p[:, h * 512:(h + 1) * 512], lhsT=w[:, fc, :],
                                                  rhs=rhsT[:, fc, h * 512:(h + 1) * 512],
                                                  start=(fc == 0), stop=(fc == nk - 1)),
                         reads=[w, rhsT], writes=[p])
            K.op(K.act, lambda e: e.activation(out=s_[:, :], in_=p[:, :], func=AF.Square),
                 reads=[p], writes=[s_])
            K.op(K.act, lambda e: e.activation(out=y_[:, :], in_=p[:, :], func=AF.Copy), reads=[p], writes=[y_])
            K.dma(K.sp, yT, yT[i], y_, y_[:, :], partial=True, owner=y_)
            for h in range(T // 512):
                K.op(K.pe, lambda e: e.matmul(ss[:, h * 512:(h + 1) * 512], lhsT=C["ones"][:, :],
                                              rhs=s_[:, h * 512:(h + 1) * 512],
                                              start=(i == 0), stop=(i == DC - 1)),
                     reads=[C["ones"], s_], writes=[ss])
        rstd_from_ss(K, C, p4, ss, rstd)


def ffn_stage(K, C, x_src, x_dst, yT, wgu, wd, gi):
    gains = C["gains"]
    with Phase(K) as p1:
        hT = p1.sbuf("f_hT", [128, DC, T], BF16)
        rstd2 = p1.sbuf("f_rstd2", [128, T], F32)
        norm_in_phase(K, C, x_src, lambda c: gains[:, gi * DC + c:gi * DC + c + 1], hT)
        with Phase(K) as big:
            hid = big.sbuf("f_hid", [128, FC, T], BF16)
            with Phase(K) as p2:
                wb = [p2.sbuf(f"f_wb{i}", [128, 2, DC, 128], BF16) for i in range(3)]
                ps = [p2.psum(f"f_ps{i}", [128, 2, T]) for i in range(2)]
                sg = [p2.sbuf(f"f_sg{i}", [128, T], BF16) for i in range(2)]
                for j in range(FC):
                    w = wb[j % 3]
                    p = ps[j % 2]
                    g_ = sg[j % 2]
                    for m in range(2):
                        K.dma(K.pool, w, w[:, m, :, :], wgu, wgu[j][:, m, :, :], partial=(m > 0))
                    for m in range(2):
                        for kc in range(DC):
                            for h in range(T // 512):
                                K.op(K.pe, lambda e: e.matmul(p[:, m, h * 512:(h + 1) * 512],
                                                              lhsT=w[:, m, kc, :],
                                                              rhs=hT[:, kc, h * 512:(h + 1) * 512],
                                                              start=(kc == 0), stop=(kc == DC - 1)),
                                     reads=[w, hT], writes=[p])
                    K.op(K.act, lambda e: e.activation(out=g_[:, :], in_=p[:, 0, :], func=AF.Silu),
                         reads=[p], writes=[g_])
                    K.op(K.dve, lambda e: e.tensor_tensor(out=hid[:, j, :], in0=p[:, 1, :], in1=g_[:, :],
                                                          op=ALU.mult),
                         reads=[p, g_], writes=[hid])
            down_phase(K, C, big, hid, FC, wd, yT, rstd2)
        epilogue_phase(K, C, x_src, yT, rstd2,
                       lambda c: gains[:, (gi + 1) * DC + c:(gi + 1) * DC + c + 1], x_dst, half=True)


def load_consts(K, st, gains_dram):
    C = {}
    C["ones"] = K.sbuf(st, "c_ones", [128, 128], BF16)
    C["eps"] = K.sbuf(st, "c_eps", [128, 1], F32)
    C["gains"] = K.sbuf(st, "c_gains", [128, int(gains_dram.t.shape[1])], F32)
    K.op(K.dve, lambda e: e.memset(C["ones"][:, :], 1.0), writes=[C["ones"]])
    K.op(K.dve, lambda e: e.memset(C["eps"][:, :], EPS), writes=[C["eps"]])
    K.dma(K.sp, C["gains"], C["gains"][:, :], gains_dram, gains_dram[:, :])
    return C


def finish(K, out_bufs):
    K.sync_bufs(out_bufs, engs=[K.sp])


def tile_gu(wg, wu):
    a = wg.reshape(DC, 128, FC, 128).transpose(2, 1, 0, 3)
    b = wu.reshape(DC, 128, FC, 128).transpose(2, 1, 0, 3)
    return np.ascontiguousarray(np.stack([a, b], axis=2))


def tile_down(wd):
    return np.ascontiguousarray(wd.reshape(FC, 128, DC, 128).transpose(2, 1, 0, 3))


def gains_cols(g):
    return np.ascontiguousarray(g.reshape(6, DC, 128).transpose(2, 0, 1).reshape(128, 6 * DC))


def to_fm(xc):
    return np.ascontiguousarray(xc.T.reshape(DC, 128, T))


def from_fm(xt):
    return np.ascontiguousarray(xt.reshape(D, T).T)


NCH = S // 128
QTL = 8
LNSC = float(np.log(128.0 ** -0.5))


def cast_load_cols(K, buf, src, ncols, lead=None):
    for c0 in range(0, ncols, 2048):
        c1 = min(ncols, c0 + 2048)
        K.dma(K.pool, buf, buf[:, c0:c1], src, src[:, c0:c1], partial=(c0 > 0))


def cast_load_3d(K, buf, src, n, inner):
    step = max(1, 2048 // inner)
    for c0 in range(0, n, step):
        c1 = min(n, c0 + step)
        K.dma(K.pool, buf, buf[:, c0:c1, :], src, src[:, c0:c1, :], partial=(c0 > 0))


def conv_pool_stage(K, C, I, O):
    with Phase(K) as ph:
        cw = ph.sbuf("cw", [128, 4, 3], F32)
        psc = ph.sbuf("psc", [128, 4], F32)
        pw = ph.sbuf("pw", [128, 4, 128], BF16)
        K.dma(K.sp, cw, cw[:, :, :], I["conv_w"], I["conv_w"][:, :, :])
        K.dma(K.sp, psc, psc[:, :], I["pool_scale"], I["pool_scale"][:, :])
        K.dma(K.pool, pw, pw[:, :, :], I["pool_w"], I["pool_w"][:, :, :])
        for ch in range(4):
            cc = ph.sbuf("cc", [128, T + 2], F32)
            cu = ph.sbuf("cu", [128, T + 2], F32)
            cb = ph.sbuf("cb", [128, T], F32)
            acc = ph.sbuf("acc", [128, T], F32)
            K.dma(K.sp, cc, cc[:, :], I["cc"], I["cc"][ch])
            K.dma(K.sp, cu, cu[:, :], I["cu"], I["cu"][ch])
            K.dma(K.sp, cb, cb[:, :], I["cb"], I["cb"][ch])
            K.op(K.dve, lambda e: e.tensor_tensor(out=cc[:, :], in0=cc[:, :], in1=cu[:, :], op=ALU.mult),
                 reads=[cu, cc], writes=[cc])
            K.op(K.dve, lambda e: e.tensor_scalar(out=acc[:, :], in0=cc[:, 2:T + 2], scalar1=cw[:, ch, 2:3],
                                                  scalar2=None, op0=ALU.mult), reads=[cc, cw], writes=[acc])
            for j in (1, 0):
                K.op(K.dve, lambda e: e.scalar_tensor_tensor(out=acc[:, :], in0=cc[:, j:T + j],
                                                             scalar=cw[:, ch, j:j + 1], in1=acc[:, :],
                                                             op0=ALU.mult, op1=ALU.add),
                     reads=[cc, cw, acc], writes=[acc])
            K.op(K.dve, lambda e: e.tensor_tensor(out=acc[:, :], in0=acc[:, :], in1=cb[:, :], op=ALU.mult),
                 reads=[acc, cb], writes=[acc])
            K.dma(K.sp, O["y_conv"], O["y_conv"][ch], acc, acc[:, :], partial=True, owner=acc)
            u = ph.sbuf("pu", [128, T + 15], F32)
            sa = ph.sbuf("psa", [128, T + 15], F32)
            sb = ph.sbuf("psb", [128, T + 15], F32)
            ic = ph.sbuf("pic", [128, T], F32)
            pl = ph.sbuf("ppl", [128, T], BF16)
            yo = ph.sbuf("pyo", [128, T], F32)
            pps = ph.psum("pps", [128, T])
            K.dma(K.sp, u, u[:, :], I["pu"], I["pu"][ch])
            K.dma(K.sp, ic, ic[:, :], I["invcnt"], I["invcnt"][ch])
            cur, nxt = u, sa
            sh = 1
            for lvl in range(ch + 1):
                c_, n_ = cur, nxt
                K.op(K.dve, lambda e: e.tensor_tensor(out=n_[:, sh:T + 15], in0=c_[:, sh:T + 15],
                                                      in1=c_[:, 0:T + 15 - sh], op=ALU.add),
                     reads=[c_], writes=[n_])
                cur = nxt
                nxt = sb if cur is sa else sa
                sh *= 2
            ws = cur
            K.op(K.dve, lambda e: e.tensor_tensor(out=ws[:, 15:T + 15], in0=ws[:, 15:T + 15], in1=ic[:, :],
                                                  op=ALU.mult), reads=[ws, ic], writes=[ws])
            K.op(K.dve, lambda e: e.tensor_tensor(out=pl[:, :], in0=ws[:, 15:T + 15], in1=u[:, 15:T + 15],
                                                  op=ALU.subtract), reads=[ws, u], writes=[pl])
            for h in range(T // 512):
                K.op(K.pe, lambda e: e.matmul(pps[:, h * 512:(h + 1) * 512], lhsT=pw[:, ch, :],
                                              rhs=pl[:, h * 512:(h + 1) * 512], start=True, stop=True),
                     reads=[pw, pl], writes=[pps])
            K.op(K.act, lambda e: e.activation(out=yo[:, :], in_=pps[:, :], func=AF.Copy,
                                               scale=psc[:, ch:ch + 1]), reads=[pps, psc], writes=[yo])
            K.dma(K.sp, O["y_pool"], O["y_pool"][ch], yo, yo[:, :], partial=True, owner=yo)


def sb_stage(K, C, I, O):
    with Phase(K) as ph:
        qT = ph.sbuf("sqT", [128, QTL * 512], BF16)
        kT = ph.sbuf("skT", [128, S], BF16)
        v = ph.sbuf("sv", [128, NCH, 128], BF16)
        qpos = ph.sbuf("qpos", [128, QTL * 512], F32)
        kpos = ph.sbuf("kpos", [128, NCH], F32)
        qf = ph.sbuf("sqf", [128, QTL * 512], F32)
        K.dma(K.sp, qf, qf[:, :], I["sq"], I["sq"][:, :])
        K.op(K.dve, lambda e: e.tensor_scalar(out=qT[:, :], in0=qf[:, :], scalar1=float(128.0 ** -0.5),
                                              scalar2=None, op0=ALU.mult), reads=[qf], writes=[qT])
        cast_load_cols(K, kT, I["skT"], S)
        cast_load_3d(K, v, I["sv"], NCH, 128)
        K.dma(K.sp, qpos, qpos[:, :], I["qpos"], I["qpos"][:, :])
        K.dma(K.sp, kpos, kpos[:, :], I["kpos"], I["kpos"][:, :])
        zp = [ph.psum(f"szp{i}", [128, 512]) for i in range(2)]
        R = ph.psum("sR", [128, 512])
        Op = ph.psum("sO", [128, 512])
        e_ = [ph.sbuf(f"se{i}", [128, 512], F32) for i in range(2)]
        sp = [ph.sbuf(f"ssp{i}", [128, 512], F32) for i in range(2)]
        spb = [ph.sbuf(f"sspb{i}", [128, 512], BF16) for i in range(2)]
        tmp = [ph.sbuf(f"stmp{i}", [128, 512], F32) for i in range(2)]
        A = [ph.sbuf(f"sA{i}", [128, 512], BF16) for i in range(2)]
        Am = [ph.sbuf(f"sAm{i}", [128, 512], BF16) for i in range(2)]
        msk = [ph.sbuf(f"smk{i}", [128, 512], F32) for i in range(2)]
        osb = [ph.sbuf(f"sos{i}", [128, 512], F32) for i in range(2)]
        step = 0
        for i in range(QTL):
            nb = 8 * i + 8
            q_ = qT[:, i * 512:(i + 1) * 512]
            for bi, kb in enumerate(range(nb - 1, -1, -1)):
                masked = bi < 8
                z = zp[step % 2]; ee = e_[step % 2]; s_ = sp[step % 2]; sb_ = spb[step % 2]
                t_ = tmp[step % 2]; a_ = A[step % 2]; am_ = Am[step % 2]; m_ = msk[step % 2]
                step += 1
                K.op(K.pe, lambda e: e.matmul(z[:, :], lhsT=kT[:, kb * 128:(kb + 1) * 128], rhs=q_,
                                              start=True, stop=True), reads=[kT, qT], writes=[z])
                K.op(K.act, lambda e: e.activation(out=ee[:, :], in_=z[:, :], func=AF.Exp), reads=[z], writes=[ee])
                K.op(K.act, lambda e: e.activation(out=s_[:, :], in_=ee[:, :], func=AF.Ln, bias=C["one"][:, 0:1]),
                     reads=[ee, C["one"]], writes=[s_])
                if masked:
                    K.op(K.pool, lambda e: e.tensor_scalar(out=m_[:, :], in0=qpos[:, i * 512:(i + 1) * 512],
                                                           scalar1=kpos[:, kb:kb + 1], scalar2=None,
                                                           op0=ALU.is_gt), reads=[qpos, kpos], writes=[m_])
                    K.op(K.pool, lambda e: e.tensor_tensor(out=sb_[:, :], in0=s_[:, :], in1=m_[:, :], op=ALU.mult),
                         reads=[s_, m_], writes=[sb_])
                else:
                    K.op(K.pool, lambda e: e.tensor_copy(out=sb_[:, :], in_=s_[:, :]), reads=[s_], writes=[sb_])
                K.op(K.dve, lambda e: e.tensor_tensor(out=t_[:, :], in0=z[:, :], in1=s_[:, :], op=ALU.subtract),
                     reads=[z, s_], writes=[t_])
                K.op(K.pe, lambda e: e.matmul(R[:, :], lhsT=C["Ustrict"][:, :], rhs=sb_[:, :],
                                              start=(bi == 0), stop=False, skip_group_check=True),
                     reads=[C["Ustrict"], sb_], writes=[R])
                K.op(K.dve, lambda e: e.tensor_tensor(out=t_[:, :], in0=t_[:, :], in1=R[:, :], op=ALU.subtract),
                     reads=[t_, R], writes=[t_])
                K.op(K.pe, lambda e: e.matmul(R[:, :], lhsT=C["Lincl"][:, :], rhs=sb_[:, :],
                                              start=False, stop=(kb == 0), skip_group_check=True),
                     reads=[C["Lincl"], sb_], writes=[R])
                K.op(K.act, lambda e: e.activation(out=a_[:, :], in_=t_[:, :], func=AF.Exp), reads=[t_], writes=[a_])
                if masked:
                    K.op(K.pool, lambda e: e.tensor_tensor(out=am_[:, :], in0=a_[:, :], in1=m_[:, :], op=ALU.mult),
                         reads=[a_, m_], writes=[am_])
                    ause = am_
                else:
                    ause = a_
                K.op(K.pe, lambda e: e.matmul(Op[:, :], lhsT=v[:, kb, :], rhs=ause[:, :],
                                              start=(bi == 0), stop=(kb == 0), skip_group_check=True),
                     reads=[v, ause], writes=[Op])
            o_ = osb[i % 2]
            K.op(K.act, lambda e: e.activation(out=o_[:, :], in_=Op[:, :], func=AF.Copy), reads=[Op], writes=[o_])
            K.dma(K.sp, O["y_sb"], O["y_sb"][:, i * 512:(i + 1) * 512], o_, o_[:, :], partial=True, owner=o_)


def mlstm_stage(K, C, I, O):
    with Phase(K) as ph:
        qT = ph.sbuf("mqT", [128, S], BF16)
        kT = ph.sbuf("mkT", [128, S], BF16)
        kt = ph.sbuf("mk", [128, NCH, 128], F32)
        va = ph.sbuf("mva", [128, NCH, 129], BF16)
        gi = ph.sbuf("mgi", [128, NCH], F32)
        gf = ph.sbuf("mgf", [128, NCH], F32)
        bi_ = ph.sbuf("mbi", [128, 1], F32)
        bf_ = ph.sbuf("mbf", [128, 1], F32)
        hg = ph.sbuf("mhg", [128, 1], F32)
        cast_load_cols(K, qT, I["mqT"], S)
        cast_load_cols(K, kT, I["mkT"], S)
        for c0 in range(0, NCH, 16):
            K.dma(K.sp, kt, kt[:, c0:c0 + 16, :], I["mk"], I["mk"][:, c0:c0 + 16, :], partial=(c0 > 0))
        cast_load_3d(K, va, I["mva"], NCH, 129)
        K.dma(K.sp, gi, gi[:, :], I["mgi"], I["mgi"][:, :])
        K.dma(K.sp, gf, gf[:, :], I["mgf"], I["mgf"][:, :])
        K.dma(K.sp, bi_, bi_[:, :], I["mbi"], I["mbi"][:, :])
        K.dma(K.sp, bf_, bf_[:, :], I["mbf"], I["mbf"][:, :])
        K.dma(K.sp, hg, hg[:, :], I["mhg"], I["mhg"][:, :])
        lf = ph.sbuf("mlf", [128, NCH], F32)
        nbf = ph.sbuf("mnbf", [128, 1], F32)
        K.op(K.dve, lambda e: e.tensor_scalar(out=nbf[:, :], in0=bf_[:, :], scalar1=-1.0, scalar2=None,
                                              op0=ALU.mult), reads=[bf_], writes=[nbf])
        K.op(K.act, lambda e: e.activation(out=lf[:, :], in_=gf[:, :], func=AF.Exp, scale=-1.0,
                                           bias=nbf[:, 0:1]), reads=[gf, nbf], writes=[lf])
        K.op(K.act, lambda e: e.activation(out=lf[:, :], in_=lf[:, :], func=AF.Ln, bias=C["one"][:, 0:1]),
             reads=[lf, C["one"]], writes=[lf])
        K.op(K.dve, lambda e: e.tensor_scalar(out=lf[:, :], in0=lf[:, :], scalar1=-1.0, scalar2=None,
                                              op0=ALU.mult), reads=[lf], writes=[lf])
        gps = ph.psum("mgps", [128, 2, NCH])
        K.op(K.pe, lambda e: e.matmul(gps[:, 0, :], lhsT=C["TriF"][:, :], rhs=lf[:, :], start=True, stop=True),
             reads=[C["TriF"], lf], writes=[gps])
        K.op(K.pe, lambda e: e.matmul(gps[:, 1, :], lhsT=C["onesF"][:, :], rhs=lf[:, :], start=True, stop=True),
             reads=[C["onesF"], lf], writes=[gps])
        ig = ph.sbuf("mig", [128, NCH], F32)
        K.op(K.dve, lambda e: e.tensor_scalar(out=ig[:, :], in0=gi[:, :], scalar1=bi_[:, 0:1], scalar2=None,
                                              op0=ALU.add), reads=[gi, bi_], writes=[ig])
        imb = ph.sbuf("mimb", [128, NCH], F32)
        K.op(K.dve, lambda e: e.tensor_tensor(out=imb[:, :], in0=ig[:, :], in1=gps[:, 0, :], op=ALU.subtract),
             reads=[ig, gps], writes=[imb])
        ek = ph.sbuf("mek", [128, NCH], F32)
        K.op(K.act, lambda e: e.activation(out=ek[:, :], in_=imb[:, :], func=AF.Exp, bias=C["lnsc"][:, 0:1]),
             reads=[imb, C["lnsc"]], writes=[ek])
        wk = ph.sbuf("mwk", [128, NCH], F32)
        K.op(K.dve, lambda e: e.tensor_tensor(out=wk[:, :], in0=imb[:, :], in1=gps[:, 1, :], op=ALU.add),
             reads=[imb, gps], writes=[wk])
        K.op(K.act, lambda e: e.activation(out=wk[:, :], in_=wk[:, :], func=AF.Exp, bias=C["lnsc"][:, 0:1]),
             reads=[wk, C["lnsc"]], writes=[wk])
        dec = ph.sbuf("mdec", [128, NCH], F32)
        K.op(K.act, lambda e: e.activation(out=dec[:, :], in_=gps[:, 1, :], func=AF.Exp), reads=[gps], writes=[dec])
        Cst = ph.sbuf("mC", [128, 129], F32)
        Cbf = ph.sbuf("mCbf", [128, 128], BF16)
        nbc = ph.sbuf("mnbc", [128, 128], BF16)
        K.op(K.dve, lambda e: e.memset(Cst[:, :], 0.0), writes=[Cst])
        K.op(K.dve, lambda e: e.memset(Cbf[:, :], 0.0), writes=[Cbf])
        K.op(K.dve, lambda e: e.memset(nbc[:, :], 0.0), writes=[nbc])
        sps = ph.psum("msps", [128, 128])
        bps = ph.psum("mbps", [128, 128])
        nps = ph.psum("mnps", [128, 128])
        dps = ph.psum("mdps", [128, 128])
        kvps = ph.psum("mkvps", [128, 129])
        mps = ph.psum("mmps", [128, 128])
        lfb = ph.sbuf("mlfb", [128, 128], F32)
        embt = ph.sbuf("membt", [128, 128], F32)
        PT = ph.sbuf("mPT", [128, 128], BF16)
        k2 = ph.sbuf("mk2", [128, 128], BF16)
        dm = ph.sbuf("mdm", [128, 128], F32)
        hh = ph.sbuf("mhh", [128, 128], F32)
        sq = ph.sbuf("msq", [128, 128], BF16)
        rt = ph.sbuf("mrt", [128, 128], F32)
        og = ph.sbuf("mog", [128, 128], F32)
        osg = ph.sbuf("mosg", [128, 128], F32)
        yo = [ph.sbuf(f"myo{i}", [128, 128], F32) for i in range(2)]
        for k in range(NCH):
            ts_ = slice(k * 128, (k + 1) * 128)
            K.dma(K.sp, og, og[:, :], I["moT"], I["moT"][:, ts_])
            K.op(K.pe, lambda e: e.matmul(sps[:, :], lhsT=kT[:, ts_], rhs=qT[:, ts_], start=True, stop=True),
                 reads=[kT, qT], writes=[sps])
            K.op(K.dve, lambda e: e.scalar_tensor_tensor(out=PT[:, :], in0=sps[:, :], scalar=ek[:, k:k + 1],
                                                         in1=C["causal"][:, :], op0=ALU.mult, op1=ALU.mult),
                 reads=[sps, ek, C["causal"]], writes=[PT])
            K.op(K.dve, lambda e: e.tensor_scalar(out=lfb[:, :], in0=C["onesF"][:, :], scalar1=lf[:, k:k + 1],
                                                  scalar2=None, op0=ALU.mult), reads=[C["onesF"], lf], writes=[lfb])
            K.op(K.pe, lambda e: e.matmul(bps[:, :], lhsT=lfb[:, :], rhs=C["TriF"][:, :], start=True, stop=True),
                 reads=[lfb, C["TriF"]], writes=[bps])
            K.op(K.act, lambda e: e.activation(out=embt[:, :], in_=bps[:, :], func=AF.Exp, scale=-1.0),
                 reads=[bps], writes=[embt])
            K.op(K.pe, lambda e: e.matmul(nps[:, :], lhsT=va[:, k, 0:128], rhs=PT[:, :], start=True, stop=False),
                 reads=[va, PT], writes=[nps])
            K.op(K.pe, lambda e: e.matmul(nps[:, :], lhsT=Cbf[:, :], rhs=qT[:, ts_], start=False, stop=True),
                 reads=[Cbf, qT], writes=[nps])
            K.op(K.pe, lambda e: e.matmul(dps[:, :], lhsT=C["ones"][:, :], rhs=PT[:, :], start=True, stop=False),
                 reads=[C["ones"], PT], writes=[dps])
            K.op(K.pe, lambda e: e.matmul(dps[:, :], lhsT=nbc[:, :], rhs=qT[:, ts_], start=False, stop=True),
                 reads=[nbc, qT], writes=[dps])
            K.op(K.act, lambda e: e.activation(out=dm[:, :], in_=dps[:, :], func=AF.Abs), reads=[dps], writes=[dm])
            K.op(K.dve, lambda e: e.tensor_tensor(out=dm[:, :], in0=dm[:, :], in1=embt[:, :], op=ALU.max),
                 reads=[dm, embt], writes=[dm])
            K.op(K.dve, lambda e: e.reciprocal(out=dm[:, :], in_=dm[:, :]), reads=[dm], writes=[dm])
            K.op(K.dve, lambda e: e.tensor_tensor(out=hh[:, :], in0=nps[:, :], in1=dm[:, :], op=ALU.mult),
                 reads=[nps, dm], writes=[hh])
            K.op(K.dve, lambda e: e.tensor_scalar(out=k2[:, :], in0=kt[:, k, :], scalar1=wk[:, k:k + 1],
                                                  scalar2=None, op0=ALU.mult), reads=[kt, wk], writes=[k2])
            K.op(K.pe, lambda e: e.matmul(kvps[:, :], lhsT=k2[:, :], rhs=va[:, k, :], start=True, stop=True),
                 reads=[k2, va], writes=[kvps])
            K.op(K.dve, lambda e: e.scalar_tensor_tensor(out=Cst[:, :], in0=Cst[:, :], scalar=dec[:, k:k + 1],
                                                         in1=kvps[:, :], op0=ALU.mult, op1=ALU.add),
                 reads=[Cst, dec, kvps], writes=[Cst])
            K.op(K.act, lambda e: e.activation(out=Cbf[:, :], in_=Cst[:, 0:128], func=AF.Copy),
                 reads=[Cst], writes=[Cbf])
            K.op(K.pool, lambda e: e.tensor_scalar(out=nbc[:, :], in0=C["onesF"][:, :], scalar1=Cst[:, 128:129],
                                                   scalar2=None, op0=ALU.mult), reads=[C["onesF"], Cst], writes=[nbc])
            K.op(K.act, lambda e: e.activation(out=sq[:, :], in_=hh[:, :], func=AF.Square), reads=[hh], writes=[sq])
            K.op(K.pe, lambda e: e.matmul(mps[:, :], lhsT=C["ones"][:, :], rhs=sq[:, :], start=True, stop=True),
                 reads=[C["ones"], sq], writes=[mps])
            K.op(K.act, lambda e: e.activation(out=rt[:, :], in_=mps[:, :], func=AF.Sqrt, scale=1.0 / 128,
                                               bias=C["eps"][:, 0:1]), reads=[mps, C["eps"]], writes=[rt])
            K.op(K.dve, lambda e: e.reciprocal(out=rt[:, :], in_=rt[:, :]), reads=[rt], writes=[rt])
            K.op(K.dve, lambda e: e.scalar_tensor_tensor(out=hh[:, :], in0=hh[:, :], scalar=hg[:, 0:1],
                                                         in1=rt[:, :], op0=ALU.mult, op1=ALU.mult),
                 reads=[hh, hg, rt], writes=[hh])
            K.op(K.act, lambda e: e.activation(out=osg[:, :], in_=og[:, :], func=AF.Sigmoid), reads=[og], writes=[osg])
            y_ = yo[k % 2]
            K.op(K.dve, lambda e: e.tensor_tensor(out=y_[:, :], in0=hh[:, :], in1=osg[:, :], op=ALU.mult),
                 reads=[hh, osg], writes=[y_])
            K.dma(K.sp, O["y_ml"], O["y_ml"][:, ts_], y_, y_[:, :], partial=True, owner=y_)


def load_mixer_consts(K, st, C, I):
    for name, dt in (("Ustrict", BF16), ("Lincl", BF16), ("causal", F32), ("TriF", F32), ("onesF", F32)):
        C[name] = K.sbuf(st, "c_" + name, [128, 128], dt)
        q = K.pool if dt == BF16 else K.sp
        K.dma(q, C[name], C[name][:, :], I["k_" + name], I["k_" + name][:, :])
    C["one"] = K.sbuf(st, "c_one", [128, 1], F32)
    C["lnsc"] = K.sbuf(st, "c_lnsc", [128, 1], F32)
    K.op(K.dve, lambda e: e.memset(C["one"][:, :], 1.0), writes=[C["one"]])
    K.op(K.dve, lambda e: e.memset(C["lnsc"][:, :], LNSC), writes=[C["lnsc"]])


def mixer_consts_host():
    j = np.arange(128)[:, None]
    s = np.arange(128)[None, :]
    return {
        "k_Ustrict": (j > s).astype(np.float32),
        "k_Lincl": (j <= s).astype(np.float32),
        "k_causal": (j <= s).astype(np.float32),
        "k_TriF": (j <= s).astype(np.float32),
        "k_onesF": np.ones((128, 128), np.float32),
    }


OFF = dict(cb=0, cc=512, cu=1024, mq=1536, mk=2048, mv=2560, mo=3072, mi=3584, mf=3588, pu=3592,
           sq=4104, sk=4616, sv=5128)


def _fm_halo(a, c, halo):
    lo = c * T - halo
    if lo < 0:
        blk = np.concatenate([np.zeros((-lo, a.shape[1]), a.dtype), a[0:(c + 1) * T]], axis=0)
    else:
        blk = a[lo:(c + 1) * T]
    return np.ascontiguousarray(blk.T.reshape(4, 128, halo + T))


def sb_tiles(c):
    r = c // 4
    return [2 * i + r for i in range(QTL)]


def mixer_inputs(proj, conv_w, pool_w, pool_scale, i_bias, f_bias, head_gain, c):
    h = c % 4
    hs = slice(h * 128, (h + 1) * 128)
    g = lambda name: proj[:, OFF[name]:OFF[name] + 512]
    I = {}
    I["cb"] = _fm_halo(g("cb"), c, 0)
    I["cc"] = _fm_halo(g("cc"), c, 2)
    I["cu"] = _fm_halo(g("cu"), c, 2)
    I["pu"] = _fm_halo(g("pu"), c, 15)
    I["conv_w"] = np.ascontiguousarray(conv_w.reshape(3, 4, 128).transpose(2, 1, 0))
    I["pool_w"] = np.ascontiguousarray(pool_w.transpose(1, 0, 2))
    I["pool_scale"] = np.ascontiguousarray(pool_scale.reshape(4, 128).T)
    t = np.arange(c * T, (c + 1) * T)
    cnt = np.stack([np.minimum(t + 1, w) for w in (2, 4, 8, 16)], axis=0).astype(np.float32)
    I["invcnt"] = np.ascontiguousarray(np.broadcast_to((1.0 / cnt)[:, None, :], (4, 128, T))).astype(np.float32)
    mq, mk, mv, mo = g("mq")[:, hs], g("mk")[:, hs], g("mv")[:, hs], g("mo")[:, hs]
    I["mqT"] = np.ascontiguousarray(mq.T)
    I["mkT"] = np.ascontiguousarray(mk.T)
    I["mk"] = np.ascontiguousarray(mk.reshape(NCH, 128, 128).transpose(1, 0, 2))
    va = np.concatenate([mv, np.ones((S, 1), np.float32)], axis=1)
    I["mva"] = np.ascontiguousarray(va.reshape(NCH, 128, 129).transpose(1, 0, 2))
    I["moT"] = np.ascontiguousarray(mo.T)
    I["mgi"] = np.ascontiguousarray(proj[:, OFF["mi"] + h].reshape(NCH, 128).T)
    I["mgf"] = np.ascontiguousarray(proj[:, OFF["mf"] + h].reshape(NCH, 128).T)
    I["mbi"] = np.full((128, 1), i_bias[h], np.float32)
    I["mbf"] = np.full((128, 1), f_bias[h], np.float32)
    I["mhg"] = np.ascontiguousarray(head_gain[hs].reshape(128, 1))
    sq, sk, sv = g("sq")[:, hs], g("sk")[:, hs], g("sv")[:, hs]
    tiles = sb_tiles(c)
    qsel = np.concatenate([sq[ti * 512:(ti + 1) * 512] for ti in tiles], axis=0)
    I["sq"] = np.ascontiguousarray(qsel.T)
    I["skT"] = np.ascontiguousarray(sk.T)
    I["sv"] = np.ascontiguousarray(sv.reshape(NCH, 128, 128).transpose(1, 0, 2))
    qp = np.concatenate([np.arange(ti * 512, (ti + 1) * 512) for ti in tiles]).astype(np.float32)
    I["qpos"] = np.ascontiguousarray(np.broadcast_to(qp[None, :], (128, QTL * 512)))
    I["kpos"] = np.ascontiguousarray((np.arange(NCH)[None, :] * 128 + np.arange(128)[:, None]).astype(np.float32))
    I.update(mixer_consts_host())
    return I


MIX_IN_SHAPES = dict(cb=[4, 128, T], cc=[4, 128, T + 2], cu=[4, 128, T + 2], pu=[4, 128, T + 15],
                     conv_w=[128, 4, 3], pool_w=[128, 4, 128], pool_scale=[128, 4], invcnt=[4, 128, T],
                     mqT=[128, S], mkT=[128, S], mk=[128, NCH, 128], mva=[128, NCH, 129], moT=[128, S],
                     mgi=[128, NCH], mgf=[128, NCH], mbi=[128, 1], mbf=[128, 1], mhg=[128, 1],
                     sq=[128, QTL * 512], skT=[128, S], sv=[128, NCH, 128], qpos=[128, QTL * 512],
                     kpos=[128, NCH], k_Ustrict=[128, 128], k_Lincl=[128, 128], k_causal=[128, 128],
                     k_TriF=[128, 128], k_onesF=[128, 128])
MIX_OUT_SHAPES = dict(y_conv=[4, 128, T], y_pool=[4, 128, T], y_ml=[128, S], y_sb=[128, QTL * 512])


def build_mixer_prog(parts=("cp", "ml", "sb")):
    nc = bass.Bass("TRN2", target_bir_lowering=False)
    with ExitStack() as es:
        K = Kern(nc, es)
        I = {k: K.dram(k, v, F32, "ExternalInput") for k, v in MIX_IN_SHAPES.items()}
        O = {k: K.dram(k, v, F32, "ExternalOutput") for k, v in MIX_OUT_SHAPES.items()}
        C = {}
        C["ones"] = K.sbuf(es, "c_ones", [128, 128], BF16)
        C["eps"] = K.sbuf(es, "c_eps", [128, 1], F32)
        K.op(K.dve, lambda e: e.memset(C["ones"][:, :], 1.0), writes=[C["ones"]])
        K.op(K.dve, lambda e: e.memset(C["eps"][:, :], EPS), writes=[C["eps"]])
        load_mixer_consts(K, es, C, I)
        if "cp" in parts:
            conv_pool_stage(K, C, I, O)
        if "ml" in parts:
            mlstm_stage(K, C, I, O)
        if "sb" in parts:
            sb_stage(K, C, I, O)
        finish(K, list(O.values()))
    return nc


def assemble_mix(results):
    y = np.zeros((S, D), np.float32)
    for c, r in enumerate(results):
        ts = slice(c * T, (c + 1) * T)
        y[ts, 0:512] = r["y_conv"].reshape(512, T).T
        y[ts, 1024:1536] = r["y_pool"].reshape(512, T).T
        h = c % 4
        if c < 4:
            y[:, 512 + h * 128:512 + (h + 1) * 128] = r["y_ml"].T
        for i, ti in enumerate(sb_tiles(c)):
            y[ti * 512:(ti + 1) * 512, 1536 + h * 128:1536 + (h + 1) * 128] = r["y_sb"][:, i * 512:(i + 1) * 512].T
    return y


NPJ = 45


def inproj_stage(K, C, x_src, projT, win, gi):
    gains = C["gains"]
    with Phase(K) as p1:
        hT = p1.sbuf("i_hT", [128, DC, T], BF16)
        norm_in_phase(K, C, x_src, lambda c: gains[:, gi * DC + c:gi * DC + c + 1], hT)
        with Phase(K) as p2:
            wb = [p2.sbuf(f"i_wb{i}", [128, DC, 128], BF16) for i in range(3)]
            ps = [p2.psum(f"i_ps{i}", [128, T]) for i in range(2)]
            ob = [p2.sbuf(f"i_ob{i}", [128, T], F32) for i in range(2)]
            for j in range(NPJ):
                w = wb[j % 3]; p = ps[j % 2]; o_ = ob[j % 2]
                K.dma(K.pool, w, w[:, :, :], win, win[j])
                for kc in range(DC):
                    for h in range(T // 512):
                        K.op(K.pe, lambda e: e.matmul(p[:, h * 512:(h + 1) * 512], lhsT=w[:, kc, :],
                                                      rhs=hT[:, kc, h * 512:(h + 1) * 512],
                                                      start=(kc == 0), stop=(kc == DC - 1)),
                             reads=[w, hT], writes=[p])
                K.op(K.act, lambda e: e.activation(out=o_[:, :], in_=p[:, :], func=AF.Copy), reads=[p], writes=[o_])
                K.dma(K.sp, projT, projT[j], o_, o_[:, :], partial=True, owner=o_)


def outproj_stage(K, C, x_src, ymix, wout, yT, x_dst, gi):
    gains = C["gains"]
    with Phase(K) as p1:
        yb = p1.sbuf("o_yb", [128, DC, T], BF16)
        rstd = p1.sbuf("o_rstd", [128, T], F32)
        for c in range(DC):
            K.dma(K.pool, yb, yb[:, c, :], ymix, ymix[c], partial=(c > 0))
        down_phase(K, C, p1, yb, DC, wout, yT, rstd)
        epilogue_phase(K, C, x_src, yT, rstd, lambda c: gains[:, gi * DC + c:gi * DC + c + 1], x_dst, half=False)


def build_pa():
    nc = bass.Bass("TRN2", target_bir_lowering=False)
    with ExitStack() as es:
        K = Kern(nc, es)
        x_in = K.dram("x_in", [DC, 128, T], F32, "ExternalInput")
        wgu = K.dram("wgu", [FC, 128, 2, DC, 128], F32, "ExternalInput")
        wd = K.dram("wd", [DC, 128, FC, 128], F32, "ExternalInput")
        win = K.dram("win", [NPJ, 128, DC, 128], F32, "ExternalInput")
        gains = K.dram("gains", [128, 6 * DC], F32, "ExternalInput")
        x_out = K.dram("x_out", [DC, 128, T], F32, "ExternalOutput")
        projT = K.dram("projT", [NPJ, 128, T], F32, "ExternalOutput")
        yT = K.dram("yT", [DC, 128, T], F32, "Internal")
        C = load_consts(K, es, gains)
        ffn_stage(K, C, x_in, x_out, yT, wgu, wd, 0)
        inproj_stage(K, C, x_out, projT, win, 2)
        finish(K, [x_out, projT])
    return nc


def build_pc():
    nc = bass.Bass("TRN2", target_bir_lowering=False)
    with ExitStack() as es:
        K = Kern(nc, es)
        x_in = K.dram("x_in", [DC, 128, T], F32, "ExternalInput")
        ymix = K.dram("ymix", [DC, 128, T], F32, "ExternalInput")
        wout = K.dram("wout", [DC, 128, DC, 128], F32, "ExternalInput")
        wgu = K.dram("wgu", [FC, 128, 2, DC, 128], F32, "ExternalInput")
        wd = K.dram("wd", [DC, 128, FC, 128], F32, "ExternalInput")
        gains = K.dram("gains", [128, 6 * DC], F32, "ExternalInput")
        x_out = K.dram("x_out", [DC, 128, T], F32, "ExternalOutput")
        x_mid = K.dram("x_mid", [DC, 128, T], F32, "Internal")
        yT = K.dram("yT", [DC, 128, T], F32, "Internal")
        C = load_consts(K, es, gains)
        outproj_stage(K, C, x_in, ymix, wout, yT, x_mid, 3)
        ffn_stage(K, C, x_mid, x_out, yT, wgu, wd, 4)
        finish(K, [x_out])
    return nc


def tile_kn(w, nk, nn):
    return np.ascontiguousarray(w.reshape(nk, 128, nn, 128).transpose(2, 1, 0, 3))


CH = dict(cb=0, cc=4, cu=8, mq=12, mk=16, mv=20, mo=24, pu=28, sq=32, sk=36, sv=40, gates=44)
NBLK = 38
B_MQ, B_MKT, B_MO, B_SQ, B_SKT, B_MK, B_MV, B_SV, B_GATES, B_HALO = 0, 4, 8, 12, 16, 20, 24, 28, 32, 33
B_SQ1 = 34
NBLK2 = 16


def blk(buf, r, b, nb):
    return buf[b][r * 128:(r + 1) * 128, :]


def inproj_stage_f(K, C, x_src, projT, PAY, win_l, gi):
    gains = C["gains"]
    with Phase(K) as p1:
        hT = p1.sbuf("i_hT", [128, DC, T], BF16)
        norm_in_phase(K, C, x_src, lambda c: gains[:, gi * DC + c:gi * DC + c + 1], hT)
        with Phase(K) as p2:
            wb = [p2.sbuf(f"i_wb{i}", [128, DC, 128], BF16) for i in range(3)]
            ps = [p2.psum(f"i_ps{i}", [128, T]) for i in range(2)]
            ob = [p2.sbuf(f"i_ob{i}", [128, T], F32) for i in range(2)]
            pt = [p2.psum(f"i_pt{i}", [128, T]) for i in range(2)]
            ot = [p2.sbuf(f"i_ot{i}", [128, T], F32) for i in range(2)]
            gsb = p2.sbuf("i_gsb", [128, 8, 8], F32)
            ntm = 0
            for j in range(NPJ):
                w = wb[j % 3]; p = ps[j % 2]; o_ = ob[j % 2]
                K.dma(K.pool, w, w[:, :, :], win_l, win_l[j])
                if j < NPJ - 1:
                    for kc in range(DC):
                        for h in range(T // 512):
                            K.op(K.pe, lambda e: e.matmul(p[:, h * 512:(h + 1) * 512], lhsT=w[:, kc, :],
                                                          rhs=hT[:, kc, h * 512:(h + 1) * 512],
                                                          start=(kc == 0), stop=(kc == DC - 1)),
                                 reads=[w, hT], writes=[p])
                    K.op(K.act, lambda e: e.activation(out=o_[:, :], in_=p[:, :], func=AF.Copy),
                         reads=[p], writes=[o_])
                    K.dma(K.sp, projT, projT[j], o_, o_[:, :], partial=True, owner=o_)
                tmb = None
                for nm, b0 in (("mk", B_MK), ("mv", B_MV), ("sv", B_SV)):
                    if CH[nm] <= j < CH[nm] + 4:
                        tmb = b0 + (j - CH[nm])
                if tmb is not None:
                    q_ = pt[ntm % 2]; t_ = ot[ntm % 2]; ntm += 1
                    for tb in range(T // 128):
                        for kc in range(DC):
                            K.op(K.pe, lambda e: e.matmul(q_[:, tb * 128:(tb + 1) * 128],
                                                          lhsT=hT[:, kc, tb * 128:(tb + 1) * 128], rhs=w[:, kc, :],
                                                          start=(kc == 0), stop=(kc == DC - 1)),
                                 reads=[w, hT], writes=[q_])
                    K.op(K.act, lambda e: e.activation(out=t_[:, :], in_=q_[:, :], func=AF.Copy),
                         reads=[q_], writes=[t_])
                    K.dma(K.sp, PAY, PAY[tmb * 128:(tmb + 1) * 128, :], t_, t_[:, :], partial=True, owner=t_)
                if j == CH["gates"]:
                    q_ = pt[ntm % 2]; ntm += 1
                    for tb in range(T // 128):
                        for kc in range(DC):
                            K.op(K.pe, lambda e: e.matmul(q_[:, tb * 8:(tb + 1) * 8],
                                                          lhsT=hT[:, kc, tb * 128:(tb + 1) * 128], rhs=w[:, kc, 0:8],
                                                          start=(kc == 0), stop=(kc == DC - 1)),
                                 reads=[w, hT], writes=[q_])
                    K.op(K.act, lambda e: e.activation(out=gsb[:, :, :],
                                                       in_=q_[:, 0:64].rearrange("p (tb g) -> p g tb", g=8),
                                                       func=AF.Copy), reads=[q_], writes=[gsb])
                    K.dma(K.sp, PAY, PAY[B_GATES * 128:(B_GATES + 1) * 128, 0:64],
                          gsb, gsb[:, :, :].rearrange("p g tb -> p (g tb)"), partial=True, owner=gsb)
    for nm, b0 in (("mq", B_MQ), ("mk", B_MKT), ("mo", B_MO), ("sk", B_SKT)):
        for h in range(4):
            K.dma(K.sp, PAY, PAY[(b0 + h) * 128:(b0 + h + 1) * 128, :], projT, projT[CH[nm] + h], partial=True)
    for h in range(4):
        for half, b0 in ((0, B_SQ), (1, B_SQ1)):
            K.dma(K.sp, PAY, PAY[(b0 + h) * 128:(b0 + h + 1) * 128, 0:512], projT,
                  projT[CH["sq"] + h][:, half * 512:(half + 1) * 512], partial=True)
    hb = PAY[B_HALO * 128:(B_HALO + 1) * 128, :]
    for ch in range(4):
        K.dma(K.sp, PAY, hb[:, ch * 2:ch * 2 + 2], projT, projT[CH["cc"] + ch][:, T - 2:T], partial=True)
        K.dma(K.sp, PAY, hb[:, 8 + ch * 2:8 + ch * 2 + 2], projT, projT[CH["cu"] + ch][:, T - 2:T], partial=True)
        K.dma(K.sp, PAY, hb[:, 16 + ch * 15:16 + ch * 15 + 15], projT, projT[CH["pu"] + ch][:, T - 15:T],
              partial=True)


def conv_pool_stage_f(K, C, L, projT, GAT, ymixL, zeros):
    def halo_var(dst_ap, c0, n):
        def f(v):
            if v == 0:
                return [(dst_ap, zeros[:, 0:n])]
            return [(dst_ap, blk(GAT, v - 1, B_HALO, NBLK)[:, c0:c0 + n])]
        return f
    with Phase(K) as ph:
        cw = ph.sbuf("cw", [128, 4, 3], F32)
        psc = ph.sbuf("psc", [128, 4], F32)
        pw = ph.sbuf("pw", [128, 4, 128], BF16)
        K.dma(K.sp, cw, cw[:, :, :], L["conv_w"], L["conv_w"][:, :, :])
        K.dma(K.sp, psc, psc[:, :], L["pool_scale"], L["pool_scale"][:, :])
        K.dma(K.pool, pw, pw[:, :, :], L["pool_w"], L["pool_w"][:, :, :])
        for ch in range(4):
            cc = ph.sbuf("cc", [128, T + 2], F32)
            cu = ph.sbuf("cu", [128, T + 2], F32)
            cb = ph.sbuf("cb", [128, T], F32)
            acc = ph.sbuf("acc", [128, T], F32)
            K.dma(K.sp, cc, cc[:, 2:T + 2], projT, projT[CH["cc"] + ch])
            K.dma_sel(cc, GAT, halo_var(cc[:, 0:2], ch * 2, 2), partial=True)
            K.dma(K.sp, cu, cu[:, 2:T + 2], projT, projT[CH["cu"] + ch])
            K.dma_sel(cu, GAT, halo_var(cu[:, 0:2], 8 + ch * 2, 2), partial=True)
            K.dma(K.sp, cb, cb[:, :], projT, projT[CH["cb"] + ch])
            K.op(K.dve, lambda e: e.tensor_tensor(out=cc[:, :], in0=cc[:, :], in1=cu[:, :], op=ALU.mult),
                 reads=[cu, cc], writes=[cc])
            K.op(K.dve, lambda e: e.tensor_scalar(out=acc[:, :], in0=cc[:, 2:T + 2], scalar1=cw[:, ch, 2:3],
                                                  scalar2=None, op0=ALU.mult), reads=[cc, cw], writes=[acc])
            for j in (1, 0):
                K.op(K.dve, lambda e: e.scalar_tensor_tensor(out=acc[:, :], in0=cc[:, j:T + j],
                                                             scalar=cw[:, ch, j:j + 1], in1=acc[:, :],
                                                             op0=ALU.mult, op1=ALU.add),
                     reads=[cc, cw, acc], writes=[acc])
            K.op(K.dve, lambda e: e.tensor_tensor(out=acc[:, :], in0=acc[:, :], in1=cb[:, :], op=ALU.mult),
                 reads=[acc, cb], writes=[acc])
            K.dma(K.sp, ymixL, ymixL[ch], acc, acc[:, :], partial=True, owner=acc)
            u = ph.sbuf("pu", [128, T + 15], F32)
            sa = ph.sbuf("psa", [128, T + 15], F32)
            sb = ph.sbuf("psb", [128, T + 15], F32)
            ic = ph.sbuf("pic", [128, T], F32)
            pl = ph.sbuf("ppl", [128, T], BF16)
            yo = ph.sbuf("pyo", [128, T], F32)
            pps = ph.psum("pps", [128, T])
            K.dma(K.sp, u, u[:, 15:T + 15], projT, projT[CH["pu"] + ch])
            K.dma_sel(u, GAT, halo_var(u[:, 0:15], 16 + ch * 15, 15), partial=True)
            K.dma(K.sp, ic, ic[:, :], L["invcnt"], L["invcnt"][ch])
            cur, nxt = u, sa
            sh = 1
            for lvl in range(ch + 1):
                c_, n_ = cur, nxt
                K.op(K.dve, lambda e: e.tensor_tensor(out=n_[:, sh:T + 15], in0=c_[:, sh:T + 15],
                                                      in1=c_[:, 0:T + 15 - sh], op=ALU.add),
                     reads=[c_], writes=[n_])
                cur = nxt
                nxt = sb if cur is sa else sa
                sh *= 2
            ws = cur
            K.op(K.dve, lambda e: e.tensor_tensor(out=ws[:, 15:T + 15], in0=ws[:, 15:T + 15], in1=ic[:, :],
                                                  op=ALU.mult), reads=[ws, ic], writes=[ws])
            K.op(K.dve, lambda e: e.tensor_tensor(out=pl[:, :], in0=ws[:, 15:T + 15], in1=u[:, 15:T + 15],
                                                  op=ALU.subtract), reads=[ws, u], writes=[pl])
            for h in range(T // 512):
                K.op(K.pe, lambda e: e.matmul(pps[:, h * 512:(h + 1) * 512], lhsT=pw[:, ch, :],
                                              rhs=pl[:, h * 512:(h + 1) * 512], start=True, stop=True),
                     reads=[pw, pl], writes=[pps])
            K.op(K.act, lambda e: e.activation(out=yo[:, :], in_=pps[:, :], func=AF.Copy,
                                               scale=psc[:, ch:ch + 1]), reads=[pps, psc], writes=[yo])
            K.dma(K.sp, ymixL, ymixL[4 + ch], yo, yo[:, :], partial=True, owner=yo)


class GatIn:
    def __init__(self, K, GAT):
        self.K = K
        self.GAT = GAT

    def cols(self, buf, b0, per_rank_cols=T, coff=lambda v: 0):
        n = per_rank_cols
        self.K.dma_sel(buf, self.GAT, lambda v: [(buf[:, r * n:(r + 1) * n],
                                                  blk(self.GAT, r, b0 + v % 4, NBLK)[:, coff(v):coff(v) + n])
                                                 for r in range(NCORES)])

    def tm(self, buf, b0, width=128):
        self.K.dma_sel(buf, self.GAT, lambda v: [(buf[:, 8 * r:8 * r + 8, 0:128],
                                                  blk(self.GAT, r, b0 + v % 4, NBLK).rearrange("p (a b) -> p a b", b=128))
                                                 for r in range(NCORES)], partial=True)

    def gate(self, buf, g0):
        self.K.dma_sel(buf, self.GAT, lambda v: [(buf[:, 8 * r:8 * r + 8],
                                                  blk(self.GAT, r, B_GATES, NBLK)[:, (g0 + v % 4) * 8:(g0 + v % 4) * 8 + 8])
                                                 for r in range(NCORES)])


def sb_stage_f(K, C, L, GI, PAY2):
    with Phase(K) as ph:
        qT = ph.sbuf("sqT", [128, QTL * 512], BF16)
        kT = ph.sbuf("skT", [128, S], BF16)
        v = ph.sbuf("sv", [128, NCH, 128], BF16)
        qpos = ph.sbuf("qpos", [128, QTL * 512], F32)
        kpos = ph.sbuf("kpos", [128, NCH], F32)
        qf = ph.sbuf("sqf", [128, QTL * 512], F32)
        K.dma_sel(qf, GI.GAT, lambda c_: [(qf[:, r * 512:(r + 1) * 512],
                                           blk(GI.GAT, r, (B_SQ1 if c_ // 4 else B_SQ) + c_ % 4, NBLK)[:, 0:512])
                                          for r in range(NCORES)])
        K.op(K.dve, lambda e: e.tensor_scalar(out=qT[:, :], in0=qf[:, :], scalar1=float(128.0 ** -0.5),
                                              scalar2=None, op0=ALU.mult), reads=[qf], writes=[qT])
        GI.cols(kT, B_SKT)
        GI.tm(v, B_SV)
        K.dma(K.sp, qpos, qpos[:, :], L["qpos"], L["qpos"][:, :])
        K.dma(K.sp, kpos, kpos[:, :], L["kpos"], L["kpos"][:, :])
        def chain_bufs(cid):
            B = {}
            B["zp"] = [ph.psum(f"szp{cid}{i}", [128, 512]) for i in range(2)]
            B["R"] = ph.psum(f"sR{cid}", [128, 512])
            B["O"] = ph.psum(f"sO{cid}", [128, 512])
            for nm, dt in (("e", F32), ("sp", F32), ("spb", BF16), ("tmp", F32), ("A", BF16), ("Am", BF16),
                           ("msk", F32), ("os", F32)):
                B[nm] = [ph.sbuf(f"s{nm}{cid}{i}", [128, 512], dt) for i in range(2)]
            return B

        def tile_steps(i, B):
            R = B["R"]; Op = B["O"]
            nb = 8 * i + 8
            q_ = qT[:, i * 512:(i + 1) * 512]
            for bi, kb in enumerate(range(nb - 1, -1, -1)):
                masked = bi < 8
                k2_ = bi % 2
                z = B["zp"][k2_]; ee = B["e"][k2_]; s_ = B["sp"][k2_]; sb_ = B["spb"][k2_]
                t_ = B["tmp"][k2_]; a_ = B["A"][k2_]; am_ = B["Am"][k2_]; m_ = B["msk"][k2_]
                K.op(K.pe, lambda e: e.matmul(z[:, :], lhsT=kT[:, kb * 128:(kb + 1) * 128], rhs=q_,
                                              start=True, stop=True), reads=[kT, qT], writes=[z])
                K.op(K.act, lambda e: e.activation(out=ee[:, :], in_=z[:, :], func=AF.Exp), reads=[z], writes=[ee])
                K.op(K.act, lambda e: e.activation(out=s_[:, :], in_=ee[:, :], func=AF.Ln, bias=C["one"][:, 0:1]),
                     reads=[ee, C["one"]], writes=[s_])
                if masked:
                    K.op(K.pool, lambda e: e.tensor_scalar(out=m_[:, :], in0=qpos[:, i * 512:(i + 1) * 512],
                                                           scalar1=kpos[:, kb:kb + 1], scalar2=None,
                                                           op0=ALU.is_gt), reads=[qpos, kpos], writes=[m_])
                    K.op(K.pool, lambda e: e.tensor_tensor(out=sb_[:, :], in0=s_[:, :], in1=m_[:, :], op=ALU.mult),
                         reads=[s_, m_], writes=[sb_])
                else:
                    K.op(K.pool, lambda e: e.tensor_copy(out=sb_[:, :], in_=s_[:, :]), reads=[s_], writes=[sb_])
                K.op(K.dve, lambda e: e.tensor_tensor(out=t_[:, :], in0=z[:, :], in1=s_[:, :], op=ALU.subtract),
                     reads=[z, s_], writes=[t_])
                K.op(K.pe, lambda e: e.matmul(R[:, :], lhsT=C["Ustrict"][:, :], rhs=sb_[:, :],
                                              start=(bi == 0), stop=False, skip_group_check=True),
                     reads=[C["Ustrict"], sb_], writes=[R])
                K.op(K.dve, lambda e: e.tensor_tensor(out=t_[:, :], in0=t_[:, :], in1=R[:, :], op=ALU.subtract),
                     reads=[t_, R], writes=[t_])
                K.op(K.pe, lambda e: e.matmul(R[:, :], lhsT=C["Lincl"][:, :], rhs=sb_[:, :],
                                              start=False, stop=(kb == 0), skip_group_check=True),
                     reads=[C["Lincl"], sb_], writes=[R])
                K.op(K.act, lambda e: e.activation(out=a_[:, :], in_=t_[:, :], func=AF.Exp), reads=[t_], writes=[a_])
                if masked:
                    K.op(K.pool, lambda e: e.tensor_tensor(out=am_[:, :], in0=a_[:, :], in1=m_[:, :], op=ALU.mult),
                         reads=[a_, m_], writes=[am_])
                    ause = am_
                else:
                    ause = a_
                K.op(K.pe, lambda e: e.matmul(Op[:, :], lhsT=v[:, kb, :], rhs=ause[:, :],
                                              start=(bi == 0), stop=(kb == 0), skip_group_check=True),
                     reads=[v, ause], writes=[Op])
                yield
            o_ = B["os"][0]
            K.op(K.act, lambda e: e.activation(out=o_[:, :], in_=Op[:, :], func=AF.Copy), reads=[Op], writes=[o_])
            K.dma(K.sp, PAY2, PAY2[(8 + i) * 128:(9 + i) * 128, 0:512], o_, o_[:, :], partial=True, owner=o_)
            yield

        CB = [chain_bufs(0), chain_bufs(1)]
        queue = list(range(QTL - 1, -1, -1))
        active = [None, None]
        while queue or any(a is not None for a in active):
            for c_ in range(2):
                if active[c_] is None and queue:
                    active[c_] = tile_steps(queue.pop(0), CB[c_])
                if active[c_] is not None:
                    try:
                        next(active[c_])
                    except StopIteration:
                        active[c_] = None


def mlstm_stage_f(K, C, L, GI, PAY2):
    with Phase(K) as ph:
        qT = ph.sbuf("mqT", [128, S], BF16)
        kT = ph.sbuf("mkT", [128, S], BF16)
        kt = ph.sbuf("mk", [128, NCH, 128], F32)
        va = ph.sbuf("mva", [128, NCH, 129], BF16)
        gi = ph.sbuf("mgi", [128, NCH], F32)
        gf = ph.sbuf("mgf", [128, NCH], F32)
        bi_ = ph.sbuf("mbi", [128, 1], F32)
        bf_ = ph.sbuf("mbf", [128, 1], F32)
        hg = ph.sbuf("mhg", [128, 1], F32)
        ogf = ph.sbuf("mogf", [128, S], F32)
        GI.cols(qT, B_MQ)
        GI.cols(kT, B_MKT)
        GI.cols(ogf, B_MO)
        K.op(K.dve, lambda e: e.memset(va[:, :, 128:129], 1.0), writes=[va])
        GI.tm(kt, B_MK)
        GI.tm(va, B_MV)
        GI.gate(gi, 0)
        GI.gate(gf, 4)
        K.dma(K.sp, bi_, bi_[:, :], L["mbi"], L["mbi"][:, :])
        K.dma(K.sp, bf_, bf_[:, :], L["mbf"], L["mbf"][:, :])
        K.dma(K.sp, hg, hg[:, :], L["mhg"], L["mhg"][:, :])
        lf = ph.sbuf("mlf", [128, NCH], F32)
        nbf = ph.sbuf("mnbf", [128, 1], F32)
        K.op(K.dve, lambda e: e.tensor_scalar(out=nbf[:, :], in0=bf_[:, :], scalar1=-1.0, scalar2=None,
                                              op0=ALU.mult), reads=[bf_], writes=[nbf])
        K.op(K.act, lambda e: e.activation(out=lf[:, :], in_=gf[:, :], func=AF.Exp, scale=-1.0,
                                           bias=nbf[:, 0:1]), reads=[gf, nbf], writes=[lf])
        K.op(K.act, lambda e: e.activation(out=lf[:, :], in_=lf[:, :], func=AF.Ln, bias=C["one"][:, 0:1]),
             reads=[lf, C["one"]], writes=[lf])
        K.op(K.dve, lambda e: e.tensor_scalar(out=lf[:, :], in0=lf[:, :], scalar1=-1.0, scalar2=None,
                                              op0=ALU.mult), reads=[lf], writes=[lf])
        gps = ph.psum("mgps", [128, 2, NCH])
        K.op(K.pe, lambda e: e.matmul(gps[:, 0, :], lhsT=C["TriF"][:, :], rhs=lf[:, :], start=True, stop=True),
             reads=[C["TriF"], lf], writes=[gps])
        K.op(K.pe, lambda e: e.matmul(gps[:, 1, :], lhsT=C["onesF"][:, :], rhs=lf[:, :], start=True, stop=True),
             reads=[C["onesF"], lf], writes=[gps])
        ig = ph.sbuf("mig", [128, NCH], F32)
        K.op(K.dve, lambda e: e.tensor_scalar(out=ig[:, :], in0=gi[:, :], scalar1=bi_[:, 0:1], scalar2=None,
                                              op0=ALU.add), reads=[gi, bi_], writes=[ig])
        imb = ph.sbuf("mimb", [128, NCH], F32)
        K.op(K.dve, lambda e: e.tensor_tensor(out=imb[:, :], in0=ig[:, :], in1=gps[:, 0, :], op=ALU.subtract),
             reads=[ig, gps], writes=[imb])
        ek = ph.sbuf("mek", [128, NCH], F32)
        K.op(K.act, lambda e: e.activation(out=ek[:, :], in_=imb[:, :], func=AF.Exp, bias=C["lnsc"][:, 0:1]),
             reads=[imb, C["lnsc"]], writes=[ek])
        wk = ph.sbuf("mwk", [128, NCH], F32)
        K.op(K.dve, lambda e: e.tensor_tensor(out=wk[:, :], in0=imb[:, :], in1=gps[:, 1, :], op=ALU.add),
             reads=[imb, gps], writes=[wk])
        K.op(K.act, lambda e: e.activation(out=wk[:, :], in_=wk[:, :], func=AF.Exp, bias=C["lnsc"][:, 0:1]),
             reads=[wk, C["lnsc"]], writes=[wk])
        dec = ph.sbuf("mdec", [128, NCH], F32)
        K.op(K.act, lambda e: e.activation(out=dec[:, :], in_=gps[:, 1, :], func=AF.Exp), reads=[gps], writes=[dec])
        Cst = ph.sbuf("mC", [128, 129], F32)
        Cbf = ph.sbuf("mCbf", [128, 128], BF16)
        nbc = ph.sbuf("mnbc", [128, 128], BF16)
        K.op(K.dve, lambda e: e.memset(Cst[:, :], 0.0), writes=[Cst])
        K.op(K.dve, lambda e: e.memset(Cbf[:, :], 0.0), writes=[Cbf])
        K.op(K.dve, lambda e: e.memset(nbc[:, :], 0.0), writes=[nbc])
        sps = ph.psum("msps", [128, 128])
        bps = ph.psum("mbps", [128, 128])
        nps = ph.psum("mnps", [128, 128])
        dps = ph.psum("mdps", [128, 128])
        kvps = ph.psum("mkvps", [128, 129])
        mps = ph.psum("mmps", [128, 128])
        lfb = ph.sbuf("mlfb", [128, 128], F32)
        embt = ph.sbuf("membt", [128, 128], F32)
        PT = ph.sbuf("mPT", [128, 128], BF16)
        k2 = ph.sbuf("mk2", [128, 128], BF16)
        dm = ph.sbuf("mdm", [128, 128], F32)
        hh = ph.sbuf("mhh", [128, 128], F32)
        sq = ph.sbuf("msq", [128, 128], BF16)
        rt = ph.sbuf("mrt", [128, 128], F32)
        og = ph.sbuf("mog", [128, 128], F32)
        osg = ph.sbuf("mosg", [128, 128], F32)
        yo = [ph.sbuf(f"myo{i}", [128, 128], F32) for i in range(2)]
        for k in range(NCH):
            ts_ = slice(k * 128, (k + 1) * 128)
            K.op(K.pe, lambda e: e.matmul(sps[:, :], lhsT=kT[:, ts_], rhs=qT[:, ts_], start=True, stop=True),
                 reads=[kT, qT], writes=[sps])
            K.op(K.dve, lambda e: e.scalar_tensor_tensor(out=PT[:, :], in0=sps[:, :], scalar=ek[:, k:k + 1],
                                                         in1=C["causal"][:, :], op0=ALU.mult, op1=ALU.mult),
                 reads=[sps, ek, C["causal"]], writes=[PT])
            K.op(K.dve, lambda e: e.tensor_scalar(out=lfb[:, :], in0=C["onesF"][:, :], scalar1=lf[:, k:k + 1],
                                                  scalar2=None, op0=ALU.mult), reads=[C["onesF"], lf], writes=[lfb])
            K.op(K.pe, lambda e: e.matmul(bps[:, :], lhsT=lfb[:, :], rhs=C["TriF"][:, :], start=True, stop=True),
                 reads=[lfb, C["TriF"]], writes=[bps])
            K.op(K.act, lambda e: e.activation(out=embt[:, :], in_=bps[:, :], func=AF.Exp, scale=-1.0),
                 reads=[bps], writes=[embt])
            K.op(K.pe, lambda e: e.matmul(nps[:, :], lhsT=va[:, k, 0:128], rhs=PT[:, :], start=True, stop=False),
                 reads=[va, PT], writes=[nps])
            K.op(K.pe, lambda e: e.matmul(nps[:, :], lhsT=Cbf[:, :], rhs=qT[:, ts_], start=False, stop=True),
                 reads=[Cbf, qT], writes=[nps])
            K.op(K.pe, lambda e: e.matmul(dps[:, :], lhsT=C["ones"][:, :], rhs=PT[:, :], start=True, stop=False),
                 reads=[C["ones"], PT], writes=[dps])
            K.op(K.pe, lambda e: e.matmul(dps[:, :], lhsT=nbc[:, :], rhs=qT[:, ts_], start=False, stop=True),
                 reads=[nbc, qT], writes=[dps])
            K.op(K.act, lambda e: e.activation(out=dm[:, :], in_=dps[:, :], func=AF.Abs), reads=[dps], writes=[dm])
            K.op(K.dve, lambda e: e.tensor_tensor(out=dm[:, :], in0=dm[:, :], in1=embt[:, :], op=ALU.max),
                 reads=[dm, embt], writes=[dm])
            K.op(K.dve, lambda e: e.reciprocal(out=dm[:, :], in_=dm[:, :]), reads=[dm], writes=[dm])
            K.op(K.dve, lambda e: e.tensor_tensor(out=hh[:, :], in0=nps[:, :], in1=dm[:, :], op=ALU.mult),
                 reads=[nps, dm], writes=[hh])
            K.op(K.dve, lambda e: e.tensor_scalar(out=k2[:, :], in0=kt[:, k, :], scalar1=wk[:, k:k + 1],
                                                  scalar2=None, op0=ALU.mult), reads=[kt, wk], writes=[k2])
            K.op(K.pe, lambda e: e.matmul(kvps[:, :], lhsT=k2[:, :], rhs=va[:, k, :], start=True, stop=True),
                 reads=[k2, va], writes=[kvps])
            K.op(K.dve, lambda e: e.scalar_tensor_tensor(out=Cst[:, :], in0=Cst[:, :], scalar=dec[:, k:k + 1],
                                                         in1=kvps[:, :], op0=ALU.mult, op1=ALU.add),
                 reads=[Cst, dec, kvps], writes=[Cst])
            K.op(K.act, lambda e: e.activation(out=Cbf[:, :], in_=Cst[:, 0:128], func=AF.Copy),
                 reads=[Cst], writes=[Cbf])
            K.op(K.pool, lambda e: e.tensor_scalar(out=nbc[:, :], in0=C["onesF"][:, :], scalar1=Cst[:, 128:129],
                                                   scalar2=None, op0=ALU.mult), reads=[C["onesF"], Cst], writes=[nbc])
            K.op(K.act, lambda e: e.activation(out=sq[:, :], in_=hh[:, :], func=AF.Square), reads=[hh], writes=[sq])
            K.op(K.pe, lambda e: e.matmul(mps[:, :], lhsT=C["ones"][:, :], rhs=sq[:, :], start=True, stop=True),
                 reads=[C["ones"], sq], writes=[mps])
            K.op(K.act, lambda e: e.activation(out=rt[:, :], in_=mps[:, :], func=AF.Sqrt, scale=1.0 / 128,
                                               bias=C["eps"][:, 0:1]), reads=[mps, C["eps"]], writes=[rt])
            K.op(K.dve, lambda e: e.reciprocal(out=rt[:, :], in_=rt[:, :]), reads=[rt], writes=[rt])
            K.op(K.dve, lambda e: e.scalar_tensor_tensor(out=hh[:, :], in0=hh[:, :], scalar=hg[:, 0:1],
                                                         in1=rt[:, :], op0=ALU.mult, op1=ALU.mult),
                 reads=[hh, hg, rt], writes=[hh])
            K.op(K.act, lambda e: e.activation(out=osg[:, :], in_=ogf[:, ts_], func=AF.Sigmoid), reads=[ogf], writes=[osg])
            y_ = yo[k % 2]
            K.op(K.dve, lambda e: e.tensor_tensor(out=y_[:, :], in0=hh[:, :], in1=osg[:, :], op=ALU.mult),
                 reads=[hh, osg], writes=[y_])
            K.dma(K.sp, PAY2, PAY2[(k // 8) * 128:(k // 8 + 1) * 128, (k % 8) * 128:(k % 8 + 1) * 128],
                  y_, y_[:, :], partial=True, owner=y_)


def outproj_stage_f(K, C, x_src, ymixL, GAT2, wout_l, yT, x_dst, gi):
    gains = C["gains"]
    with Phase(K) as p1:
        yb = p1.sbuf("o_yb", [128, DC, T], BF16)
        rstd = p1.sbuf("o_rstd", [128, T], F32)
        for c in range(4):
            K.dma(K.pool, yb, yb[:, c, :], ymixL, ymixL[c], partial=(c > 0))
            K.dma(K.pool, yb, yb[:, 8 + c, :], ymixL, ymixL[4 + c], partial=True)
        for hh in range(4):
            K.dma_sel(yb, GAT2, lambda v: [(yb[:, 4 + hh, :], blk(GAT2, hh, v, NBLK2))], partial=True)
            K.dma_sel(yb, GAT2, lambda v: [
                (yb[:, 12 + hh, 0:512], blk(GAT2, hh, 8 + v, NBLK2)[:, 0:512]),
                (yb[:, 12 + hh, 512:1024], blk(GAT2, hh + 4, 8 + v, NBLK2)[:, 0:512]),
            ], partial=True)
        down_phase(K, C, p1, yb, DC, wout_l, yT, rstd)
        epilogue_phase(K, C, x_src, yT, rstd, lambda c: gains[:, gi * DC + c:gi * DC + c + 1], x_dst, half=False)


FUSED_IN = dict(
    x_in=[DC, 128, T], gains=[128, None], wgu1=[None, FC, 128, 2, DC, 128], wd1=[None, DC, 128, FC, 128],
    wgu2=[None, FC, 128, 2, DC, 128], wd2=[None, DC, 128, FC, 128], win=[None, NPJ, 128, DC, 128],
    wout=[None, DC, 128, DC, 128], conv_w=[None, 128, 4, 3], pool_w=[None, 128, 4, 128],
    pool_scale=[None, 128, 4], mbi=[None, 128, 1], mbf=[None, 128, 1], mhg=[None, 128, 1],
    invcnt=[4, 128, T], qpos=[128, QTL * 512], kpos=[128, NCH], zeros=[128, 16],
    k_Ustrict=[128, 128], k_Lincl=[128, 128], k_causal=[128, 128], k_TriF=[128, 128], k_onesF=[128, 128])


def build_fused(depth, stages=None):
    nc = bass.Bass("TRN2", target_bir_lowering=False)
    with ExitStack() as es:
        K = Kern(nc, es)
        I = {}
        for k, shp in FUSED_IN.items():
            shp = [(depth * 6 * DC if k == "gains" else depth) if d is None else d for d in shp]
            I[k] = K.dram(k, shp, F32, "ExternalInput")
        x_out = K.dram("x_out", [DC, 128, T], F32, "ExternalOutput")
        X = [K.dram(f"xs{i}", [DC, 128, T], F32) for i in range(3)]
        yT = K.dram("yT", [DC, 128, T], F32)
        projT = K.dram("projT", [NPJ, 128, T], F32)
        PAY = K.dram("pay", [NBLK * 128, T], F32)
        GAT = K.dram("gat", [NBLK, NCORES * 128, T], F32)
        ymixL = K.dram("ymixl", [8, 128, T], F32)
        PAY2 = K.dram("pay2", [NBLK2 * 128, T], F32)
        GAT2 = K.dram("gat2", [NBLK2, NCORES * 128, T], F32)
        C = load_consts(K, es, I["gains"])
        load_mixer_consts(K, es, C, I)
        GI = GatIn(K, GAT)
        lay = lambda name, l: Buf(I[name].t[l], name)
        for l in range(depth):
            src = I["x_in"] if l == 0 else X[0]
            dst = x_out if l == depth - 1 else X[0]
            on = lambda st: stages is None or st in stages
            if on("ffn1"):
                ffn_stage(K, C, src, X[1], yT, lay("wgu1", l), lay("wd1", l), l * 6 + 0)
            if on("inproj"):
                inproj_stage_f(K, C, X[1] if on("ffn1") else src, projT, PAY, lay("win", l), l * 6 + 2)
            if on("ag1"):
                for b in range(NBLK):
                    K.collective("AllGather", PAY, PAY[b * 128:(b + 1) * 128, :], GAT, GAT[b], first=(b == 0))
            L = dict(conv_w=lay("conv_w", l), pool_w=lay("pool_w", l), pool_scale=lay("pool_scale", l),
                     mbi=lay("mbi", l), mbf=lay("mbf", l), mhg=lay("mhg", l),
                     invcnt=I["invcnt"], qpos=I["qpos"], kpos=I["kpos"])
            if on("cp"):
                conv_pool_stage_f(K, C, L, projT, GAT, ymixL, I["zeros"])
            if on("ml"):
                mlstm_stage_f(K, C, L, GI, PAY2)
            if on("sb"):
                sb_stage_f(K, C, L, GI, PAY2)
            if on("ag2"):
                for b in range(NBLK2):
                    K.collective("AllGather", PAY2, PAY2[b * 128:(b + 1) * 128, :], GAT2, GAT2[b], first=(b == 0))
            if on("outproj"):
                outproj_stage_f(K, C, X[1] if on("ffn1") else src, ymixL, GAT2, lay("wout", l), yT, X[2], l * 6 + 3)
            if on("ffn2"):
                ffn_stage(K, C, X[2], dst, yT, lay("wgu2", l), lay("wd2", l), l * 6 + 4)
        if stages is not None and "ffn2" not in stages:
            K.sync_bufs([projT, PAY, GAT, ymixL, PAY2, GAT2, X[2]], engs=[K.sp, K.pool])
        finish(K, [x_out])
    return nc


def permute_win(w):
    out = np.zeros((D, NPJ * 128), np.float32)
    out[:, 0:3584] = w[:, 0:3584]
    out[:, 3584:5632] = w[:, 3592:5640]
    out[:, 5632:5640] = w[:, 3584:3592]
    return out


def fused_inputs(inp, depth):
    f = lambda a: np.asarray(a, dtype=np.float32)
    sh = {}
    sh["gains"] = np.ascontiguousarray(np.concatenate([gains_cols(f(inp["norm_gains"][l])) for l in range(depth)], axis=1))
    sh["wgu1"] = np.stack([tile_gu(f(inp["ffn1_w_gate"][l]), f(inp["ffn1_w_up"][l])) for l in range(depth)])
    sh["wd1"] = np.stack([tile_down(f(inp["ffn1_w_down"][l])) for l in range(depth)])
    sh["wgu2"] = np.stack([tile_gu(f(inp["ffn2_w_gate"][l]), f(inp["ffn2_w_up"][l])) for l in range(depth)])
    sh["wd2"] = np.stack([tile_down(f(inp["ffn2_w_down"][l])) for l in range(depth)])
    sh["win"] = np.stack([tile_kn(permute_win(f(inp["w_in"][l])), DC, NPJ) for l in range(depth)])
    sh["wout"] = np.stack([tile_kn(f(inp["w_out"][l]), DC, DC) for l in range(depth)])
    sh["conv_w"] = np.stack([np.ascontiguousarray(f(inp["conv_w"][l]).reshape(3, 4, 128).transpose(2, 1, 0)) for l in range(depth)])
    sh["pool_w"] = np.stack([np.ascontiguousarray(f(inp["pool_w"][l]).transpose(1, 0, 2)) for l in range(depth)])
    sh["pool_scale"] = np.stack([np.ascontiguousarray(f(inp["pool_scale"][l]).reshape(4, 128).T) for l in range(depth)])
    sh["kpos"] = np.ascontiguousarray((np.arange(NCH)[None, :] * 128 + np.arange(128)[:, None]).astype(np.float32))
    sh["zeros"] = np.zeros((128, 16), np.float32)
    sh.update(mixer_consts_host())
    x = f(inp["x"])
    maps = []
    for c in range(NCORES):
        h = c % 4
        m = dict(sh)
        m["x_in"] = to_fm(x[0, c * T:(c + 1) * T])
        m["mbi"] = np.stack([np.full((128, 1), f(inp["mlstm_i_bias"][l])[h], np.float32) for l in range(depth)])
        m["mbf"] = np.stack([np.full((128, 1), f(inp["mlstm_f_bias"][l])[h], np.float32) for l in range(depth)])
        m["mhg"] = np.stack([np.ascontiguousarray(f(inp["mlstm_head_gain"][l])[h * 128:(h + 1) * 128].reshape(128, 1))
                             for l in range(depth)])
        t = np.arange(c * T, (c + 1) * T)
        cnt = np.stack([np.minimum(t + 1, w) for w in (2, 4, 8, 16)], axis=0).astype(np.float32)
        m["invcnt"] = np.ascontiguousarray(np.broadcast_to((1.0 / cnt)[:, None, :], (4, 128, T))).astype(np.float32)
        qp = np.concatenate([np.arange(ti * 512, (ti + 1) * 512) for ti in sb_tiles(c)]).astype(np.float32)
        m["qpos"] = np.ascontiguousarray(np.broadcast_to(qp[None, :], (128, QTL * 512)))
        maps.append(m)
    return maps


_PROGS = {}


def kernel(**inp):
    if "fused" not in _PROGS:
        _PROGS["fused"] = build_fused(DEPTH)
    maps = fused_inputs(inp, DEPTH)
    res = run_bass_kernel_spmd(_PROGS["fused"], maps, core_ids=list(range(NCORES)))
    out = np.concatenate([from_fm(r["x_out"]) for r in res.results], axis=0)[None]
    return np.ascontiguousarray(out.astype(np.float32))
```

```python
import os
import numpy as np
import ml_dtypes
from contextlib import ExitStack
import concourse.bass as bass
import concourse.mybir as mybir
from concourse.bass_utils import run_bass_kernel_spmd

F32 = mybir.dt.float32
BF16 = mybir.dt.bfloat16
AF = mybir.ActivationFunctionType
ALU = mybir.AluOpType

NCORES = 8
D = 2048
DC = D // 128
S = 8192
T = S // NCORES
DFF = 5632
FC = DFF // 128
DEPTH = 4
EPS = 1e-6
G = 512
DIN = 5640


class Buf:
    def __init__(self, t, name=""):
        self.t = t
        self.name = name
        self.w = {}
        self.r = {}
        self.dsem = None
        self.dcount = 0

    def __getitem__(self, idx):
        return self.t[idx]


class Eng:
    def __init__(self, K, name, h):
        self.K = K
        self.name = name
        self.h = h
        self.sem = K.es.enter_context(K.nc.semaphore("e_" + name))
        self.n = 0
        self.seen = {}

    def wait(self, tok):
        if tok is None:
            return
        sem, val = tok
        if sem is self.sem and self.name == "pe":
            return
        k = id(sem)
        if self.seen.get(k, 0) >= val:
            return
        self.h.wait_ge(sem, val)
        self.seen[k] = val


class Kern:
    def __init__(self, nc, es):
        self.nc = nc
        self.es = es
        self.pe = Eng(self, "pe", nc.tensor)
        self.act = Eng(self, "act", nc.scalar)
        self.dve = Eng(self, "dve", nc.vector)
        self.pool = Eng(self, "pool", nc.gpsimd)
        self.sp = Eng(self, "sp", nc.sync)
        self.engs = [self.pe, self.act, self.dve, self.pool, self.sp]
        self.free_sems = []
        self.semcnt = {}

    def op(self, eng, fn, reads=(), writes=()):
        for b in reads:
            for tok in b.w.values():
                eng.wait(tok)
        for b in writes:
            for tok in b.w.values():
                eng.wait(tok)
            for tok in b.r.values():
                eng.wait(tok)
        ins = fn(eng.h)
        eng.n += 1
        ins.then_inc(eng.sem, 1)
        tok = (eng.sem, eng.n)
        for b in reads:
            b.r[id(eng.sem)] = tok
        for b in writes:
            b.w = {id(eng.sem): tok}
            b.r = {}
        return ins

    def get_dsem(self, name):
        if self.free_sems:
            return self.free_sems.pop()
        return self.es.enter_context(self.nc.semaphore(self.uniq("d_" + name)))

    def dma(self, q, ob, out_ap, ib, in_ap, partial=False, owner=None, **kw):
        own = owner or ob
        for tok in ib.w.values():
            q.wait(tok)
        if not partial:
            for tok in ob.w.values():
                q.wait(tok)
        for tok in ob.r.values():
            q.wait(tok)
        if own.dsem is None:
            own.dsem = self.get_dsem(own.name)
        cnt = self.semcnt.get(id(own.dsem), 0) + 1
        self.semcnt[id(own.dsem)] = cnt
        ins = q.h.dma_start(out=out_ap, in_=in_ap, **kw)
        ins.then_inc(own.dsem, 16)
        tok = (own.dsem, 16 * cnt)
        ib.r[id(own.dsem)] = tok
        if partial:
            ob.w[id(own.dsem)] = tok
        else:
            ob.w = {id(own.dsem): tok}
        ob.r = {}
        return ins

    def collective(self, kind, ib, in_ap, ob, out_ap, first=True):
        q = self.pool
        for tok in ib.w.values():
            q.wait(tok)
        if first:
            for tok in ob.w.values():
                q.wait(tok)
            for tok in ob.r.values():
                q.wait(tok)
        if not hasattr(self, "ccsem"):
            self.ccsem = self.es.enter_context(self.nc.semaphore("ccsem"))
            self.cccnt = 0
        self.cccnt += 1
        ins = q.h.collective_compute(kind, ALU.bypass, replica_groups=[list(range(NCORES))],
                                     ins=[in_ap], outs=[out_ap])
        ins.then_inc(self.ccsem, 1)
        tok = (self.ccsem, self.cccnt)
        ib.r[id(self.ccsem)] = tok
        if first:
            ob.w = {id(self.ccsem): tok}
        else:
            ob.w[id(self.ccsem)] = tok
        ob.r = {}

    def dma_sel(self, ob, ib, variants_fn, owner=None, partial=False, nvar=NCORES):
        q = self.pool
        own = owner or ob
        for tok in ib.w.values():
            q.wait(tok)
        if not partial:
            for tok in ob.w.values():
                q.wait(tok)
        for tok in ob.r.values():
            q.wait(tok)
        if own.dsem is None:
            own.dsem = self.get_dsem(own.name)
        lists = [variants_fn(v) for v in range(nvar)]
        n = len(lists[0])
        assert all(len(l) == n for l in lists)
        if not hasattr(self, "pid"):
            self.pid = q.h.partition_id()
        for v in range(nvar):
            with q.h.If(self.pid == v):
                for (oa, ia) in lists[v]:
                    q.h.dma_start(out=oa, in_=ia).then_inc(own.dsem, 16)
        cnt = self.semcnt.get(id(own.dsem), 0) + n
        self.semcnt[id(own.dsem)] = cnt
        tok = (own.dsem, 16 * cnt)
        ib.r[id(own.dsem)] = tok
        if partial:
            ob.w[id(own.dsem)] = tok
        else:
            ob.w = {id(own.dsem): tok}
        ob.r = {}

    def sync_bufs(self, bufs, engs=None):
        for e in (engs or self.engs):
            for b in bufs:
                for tok in b.w.values():
                    e.wait(tok)
                for tok in b.r.values():
                    e.wait(tok)

    def uniq(self, name):
        self.uid = getattr(self, "uid", 0) + 1
        return f"{name}_{self.uid}"

    def sbuf(self, st, name, shape, dt):
        t = st.enter_context(self.nc.sbuf_tensor(self.uniq(name), shape, dt))
        return Buf(t, name)

    def psum(self, st, name, shape, dt=F32):
        t = st.enter_context(self.nc.psum_tensor(self.uniq(name), shape, dt))
        return Buf(t, name)

    def dram(self, name, shape, dt, kind="Internal"):
        return Buf(self.nc.dram_tensor(name, shape, dt, kind=kind).ap(), name)


class Phase:
    def __init__(self, K):
        self.K = K
        self.st = ExitStack()
        self.bufs = []

    def __enter__(self):
        self.st.__enter__()
        return self

    def sbuf(self, name, shape, dt):
        b = self.K.sbuf(self.st, name, shape, dt)
        self.bufs.append(b)
        return b

    def psum(self, name, shape, dt=F32):
        b = self.K.psum(self.st, name, shape, dt)
        self.bufs.append(b)
        return b

    def __exit__(self, *a):
        self.K.sync_bufs(self.bufs)
        for b in self.bufs:
            if b.dsem is not None:
                self.K.free_sems.append(b.dsem)
        return self.st.__exit__(*a)


def norm_in_phase(K, C, x_src, gcol, hT):
    with Phase(K) as ph:
        xs = ph.sbuf("n_xs", [128, DC, T], F32)
        sq = [ph.sbuf(f"n_sq{i}", [128, T], BF16) for i in range(2)]
        ss = ph.psum("n_ss", [128, T])
        rstd = ph.sbuf("n_rstd", [128, T], F32)
        for c in range(DC):
            K.dma(K.sp, xs, xs[:, c, :], x_src, x_src[c], partial=True)
        for c in range(DC):
            s = sq[c % 2]
            K.op(K.act, lambda e: e.activation(out=s[:, :], in_=xs[:, c, :], func=AF.Square),
                 reads=[xs], writes=[s])
            for h in range(T // 512):
                K.op(K.pe, lambda e: e.matmul(ss[:, h * 512:(h + 1) * 512], lhsT=C["ones"][:, :],
                                              rhs=s[:, h * 512:(h + 1) * 512],
                                              start=(c == 0), stop=(c == DC - 1)),
                     reads=[C["ones"], s], writes=[ss])
        rstd_from_ss(K, C, ph, ss, rstd)
        for c in range(DC):
            K.op(K.dve, lambda e: e.scalar_tensor_tensor(out=hT[:, c, :], in0=xs[:, c, :],
                                                         scalar=gcol(c), in1=rstd[:, :],
                                                         op0=ALU.mult, op1=ALU.mult),
                 reads=[xs, rstd, C["gains"]], writes=[hT])


def rstd_from_ss(K, C, ph, ss, rstd):
    rt = ph.sbuf("r_rt", [128, T], F32)
    K.op(K.act, lambda e: e.activation(out=rt[:, :], in_=ss[:, :], func=AF.Sqrt,
                                       scale=1.0 / D, bias=C["eps"][:, 0:1]),
         reads=[ss, C["eps"]], writes=[rt])
    K.op(K.dve, lambda e: e.reciprocal(out=rstd[:, :], in_=rt[:, :]), reads=[rt], writes=[rstd])


def epilogue_phase(K, C, x_src, y_src, rstd, gcol, x_dst, half):
    with Phase(K) as ph:
        xb = [ph.sbuf(f"e_x{i}", [128, T], F32) for i in range(2)]
        yb = [ph.sbuf(f"e_y{i}", [128, T], F32) for i in range(2)]
        for c in range(DC):
            x_, y_ = xb[c % 2], yb[c % 2]
            K.dma(K.sp, x_, x_[:, :], x_src, x_src[c])
            K.dma(K.sp, y_, y_[:, :], y_src, y_src[c])
            K.op(K.dve, lambda e: e.scalar_tensor_tensor(out=y_[:, :], in0=y_[:, :], scalar=gcol(c),
                                                         in1=rstd[:, :], op0=ALU.mult, op1=ALU.mult),
                 reads=[y_, rstd, C["gains"]], writes=[y_])
            if half:
                K.op(K.dve, lambda e: e.scalar_tensor_tensor(out=x_[:, :], in0=y_[:, :], scalar=0.5,
                                                             in1=x_[:, :], op0=ALU.mult, op1=ALU.add),
                     reads=[y_, x_], writes=[x_])
            else:
                K.op(K.dve, lambda e: e.tensor_tensor(out=x_[:, :], in0=y_[:, :], in1=x_[:, :], op=ALU.add),
                     reads=[y_, x_], writes=[x_])
            K.dma(K.sp, x_dst, x_dst[c], x_, x_[:, :], partial=True, owner=x_)


def down_phase(K, C, ph_outer, rhsT, nk, wd, yT, rstd):
    with Phase(K) as p4:
        ss = p4.psum("f_ss", [128, T])
        db = [p4.sbuf(f"f_db{i}", [128, nk, 128], BF16) for i in range(2)]
        po = [p4.psum(f"f_po{i}", [128, T]) for i in range(2)]
        ysb = [p4.sbuf(f"f_y{i}", [128, T], F32) for i in range(2)]
        sq = [p4.sbuf(f"f_sq{i}", [128, T], BF16) for i in range(2)]
        for i in range(DC):
            w = db[i % 2]
            p = po[i % 2]
            y_ = ysb[i % 2]
            s_ = sq[i % 2]
            for f0 in range(0, nk, 16):
                f1 = min(nk, f0 + 16)
                K.dma(K.pool, w, w[:, f0:f1, :], wd, wd[i][:, f0:f1, :], partial=(f0 > 0))
            for fc in range(nk):
                for h in range(T // 512):
                    K.op(K.pe, lambda e: e.matmul(p[:, h * 512:(h + 1) * 512], lhsT=w[:, fc, :],
                                                  rhs=rhsT[:, fc, h * 512:(h + 1) * 512],
                                                  start=(fc == 0), stop=(fc == nk - 1)),
                         reads=[w, rhsT], writes=[p])
            K.op(K.act, lambda e: e.activation(out=s_[:, :], in_=p[:, :], func=AF.Square),
                 reads=[p], writes=[s_])
            K.op(K.act, lambda e: e.activation(out=y_[:, :], in_=p[:, :], func=AF.Copy), reads=[p], writes=[y_])
            K.dma(K.sp, yT, yT[i], y_, y_[:, :], partial=True, owner=y_)
            for h in range(T // 512):
                K.op(K.pe, lambda e: e.matmul(ss[:, h * 512:(h + 1) * 512], lhsT=C["ones"][:, :],
                                              rhs=s_[:, h * 512:(h + 1) * 512],
                                              start=(i == 0), stop=(i == DC - 1)),
                     reads=[C["ones"], s_], writes=[ss])
        rstd_from_ss(K, C, p4, ss, rstd)


def ffn_stage(K, C, x_src, x_dst, yT, wgu, wd, gi):
    gains = C["gains"]
    with Phase(K) as p1:
        hT = p1.sbuf("f_hT", [128, DC, T], BF16)
        rstd2 = p1.sbuf("f_rstd2", [128, T], F32)
        norm_in_phase(K, C, x_src, lambda c: gains[:, gi * DC + c:gi * DC + c + 1], hT)
        with Phase(K) as big:
            hid = big.sbuf("f_hid", [128, FC, T], BF16)
            with Phase(K) as p2:
                wb = [p2.sbuf(f"f_wb{i}", [128, 2, DC, 128], BF16) for i in range(3)]
                ps = [p2.psum(f"f_ps{i}", [128, 2, T]) for i in range(2)]
                sg = [p2.sbuf(f"f_sg{i}", [128, T], BF16) for i in range(2)]
                for j in range(FC):
                    w = wb[j % 3]
                    p = ps[j % 2]
                    g_ = sg[j % 2]
                    for m in range(2):
                        K.dma(K.pool, w, w[:, m, :, :], wgu, wgu[j][:, m, :, :], partial=(m > 0))
                    for m in range(2):
                        for kc in range(DC):
                            for h in range(T // 512):
                                K.op(K.pe, lambda e: e.matmul(p[:, m, h * 512:(h + 1) * 512],
                                                              lhsT=w[:, m, kc, :],
                                                              rhs=hT[:, kc, h * 512:(h + 1) * 512],
                                                              start=(kc == 0), stop=(kc == DC - 1)),
                                     reads=[w, hT], writes=[p])
                    K.op(K.act, lambda e: e.activation(out=g_[:, :], in_=p[:, 0, :], func=AF.Silu),
                         reads=[p], writes=[g_])
                    K.op(K.dve, lambda e: e.tensor_tensor(out=hid[:, j, :], in0=p[:, 1, :], in1=g_[:, :],
                                                          op=ALU.mult),
                         reads=[p, g_], writes=[hid])
            down_phase(K, C, big, hid, FC, wd, yT, rstd2)
        epilogue_phase(K, C, x_src, yT, rstd2,
                       lambda c: gains[:, (gi + 1) * DC + c:(gi + 1) * DC + c + 1], x_dst, half=True)


def load_consts(K, st, gains_dram):
    C = {}
    C["ones"] = K.sbuf(st, "c_ones", [128, 128], BF16)
    C["eps"] = K.sbuf(st, "c_eps", [128, 1], F32)
    C["gains"] = K.sbuf(st, "c_gains", [128, int(gains_dram.t.shape[1])], F32)
    K.op(K.dve, lambda e: e.memset(C["ones"][:, :], 1.0), writes=[C["ones"]])
    K.op(K.dve, lambda e: e.memset(C["eps"][:, :], EPS), writes=[C["eps"]])
    K.dma(K.sp, C["gains"], C["gains"][:, :], gains_dram, gains_dram[:, :])
    return C


def finish(K, out_bufs):
    K.sync_bufs(out_bufs, engs=[K.sp])


def tile_gu(wg, wu):
    a = wg.reshape(DC, 128, FC, 128).transpose(2, 1, 0, 3)
    b = wu.reshape(DC, 128, FC, 128).transpose(2, 1, 0, 3)
    return np.ascontiguousarray(np.stack([a, b], axis=2))


def tile_down(wd):
    return np.ascontiguousarray(wd.reshape(FC, 128, DC, 128).transpose(2, 1, 0, 3))


def gains_cols(g):
    return np.ascontiguousarray(g.reshape(6, DC, 128).transpose(2, 0, 1).reshape(128, 6 * DC))


def to_fm(xc):
    return np.ascontiguousarray(xc.T.reshape(DC, 128, T))


def from_fm(xt):
    return np.ascontiguousarray(xt.reshape(D, T).T)


NCH = S // 128
QTL = 8
LNSC = float(np.log(128.0 ** -0.5))


def cast_load_cols(K, buf, src, ncols, lead=None):
    for c0 in range(0, ncols, 2048):
        c1 = min(ncols, c0 + 2048)
        K.dma(K.pool, buf, buf[:, c0:c1], src, src[:, c0:c1], partial=(c0 > 0))


def cast_load_3d(K, buf, src, n, inner):
    step = max(1, 2048 // inner)
    for c0 in range(0, n, step):
        c1 = min(n, c0 + step)
        K.dma(K.pool, buf, buf[:, c0:c1, :], src, src[:, c0:c1, :], partial=(c0 > 0))


def conv_pool_stage(K, C, I, O):
    with Phase(K) as ph:
        cw = ph.sbuf("cw", [128, 4, 3], F32)
        psc = ph.sbuf("psc", [128, 4], F32)
        pw = ph.sbuf("pw", [128, 4, 128], BF16)
        K.dma(K.sp, cw, cw[:, :, :], I["conv_w"], I["conv_w"][:, :, :])
        K.dma(K.sp, psc, psc[:, :], I["pool_scale"], I["pool_scale"][:, :])
        K.dma(K.pool, pw, pw[:, :, :], I["pool_w"], I["pool_w"][:, :, :])
        for ch in range(4):
            cc = ph.sbuf("cc", [128, T + 2], F32)
            cu = ph.sbuf("cu", [128, T + 2], F32)
            cb = ph.sbuf("cb", [128, T], F32)
            acc = ph.sbuf("acc", [128, T], F32)
            K.dma(K.sp, cc, cc[:, :], I["cc"], I["cc"][ch])
            K.dma(K.sp, cu, cu[:, :], I["cu"], I["cu"][ch])
            K.dma(K.sp, cb, cb[:, :], I["cb"], I["cb"][ch])
            K.op(K.dve, lambda e: e.tensor_tensor(out=cc[:, :], in0=cc[:, :], in1=cu[:, :], op=ALU.mult),
                 reads=[cu, cc], writes=[cc])
            K.op(K.dve, lambda e: e.tensor_scalar(out=acc[:, :], in0=cc[:, 2:T + 2], scalar1=cw[:, ch, 2:3],
                                                  scalar2=None, op0=ALU.mult), reads=[cc, cw], writes=[acc])
            for j in (1, 0):
                K.op(K.dve, lambda e: e.scalar_tensor_tensor(out=acc[:, :], in0=cc[:, j:T + j],
                                                             scalar=cw[:, ch, j:j + 1], in1=acc[:, :],
                                                             op0=ALU.mult, op1=ALU.add),
                     reads=[cc, cw, acc], writes=[acc])
            K.op(K.dve, lambda e: e.tensor_tensor(out=acc[:, :], in0=acc[:, :], in1=cb[:, :], op=ALU.mult),
                 reads=[acc, cb], writes=[acc])
            K.dma(K.sp, O["y_conv"], O["y_conv"][ch], acc, acc[:, :], partial=True, owner=acc)
            u = ph.sbuf("pu", [128, T + 15], F32)
            sa = ph.sbuf("psa", [128, T + 15], F32)
            sb = ph.sbuf("psb", [128, T + 15], F32)
            ic = ph.sbuf("pic", [128, T], F32)
            pl = ph.sbuf("ppl", [128, T], BF16)
            yo = ph.sbuf("pyo", [128, T], F32)
            pps = ph.psum("pps", [128, T])
            K.dma(K.sp, u, u[:, :], I["pu"], I["pu"][ch])
            K.dma(K.sp, ic, ic[:, :], I["invcnt"], I["invcnt"][ch])
            cur, nxt = u, sa
            sh = 1
            for lvl in range(ch + 1):
                c_, n_ = cur, nxt
                K.op(K.dve, lambda e: e.tensor_tensor(out=n_[:, sh:T + 15], in0=c_[:, sh:T + 15],
                                                      in1=c_[:, 0:T + 15 - sh], op=ALU.add),
                     reads=[c_], writes=[n_])
                cur = nxt
                nxt = sb if cur is sa else sa
                sh *= 2
            ws = cur
            K.op(K.dve, lambda e: e.tensor_tensor(out=ws[:, 15:T + 15], in0=ws[:, 15:T + 15], in1=ic[:, :],
                                                  op=ALU.mult), reads=[ws, ic], writes=[ws])
            K.op(K.dve, lambda e: e.tensor_tensor(out=pl[:, :], in0=ws[:, 15:T + 15], in1=u[:, 15:T + 15],
                                                  op=ALU.subtract), reads=[ws, u], writes=[pl])
            for h in range(T // 512):
                K.op(K.pe, lambda e: e.matmul(pps[:, h * 512:(h + 1) * 512], lhsT=pw[:, ch, :],
                                              rhs=pl[:, h * 512:(h + 1) * 512], start=True, stop=True),
                     reads=[pw, pl], writes=[pps])
            K.op(K.act, lambda e: e.activation(out=yo[:, :], in_=pps[:, :], func=AF.Copy,
                                               scale=psc[:, ch:ch + 1]), reads=[pps, psc], writes=[yo])
            K.dma(K.sp, O["y_pool"], O["y_pool"][ch], yo, yo[:, :], partial=True, owner=yo)


def sb_stage(K, C, I, O):
    with Phase(K) as ph:
        qT = ph.sbuf("sqT", [128, QTL * 512], BF16)
        kT = ph.sbuf("skT", [128, S], BF16)
        v = ph.sbuf("sv", [128, NCH, 128], BF16)
        qpos = ph.sbuf("qpos", [128, QTL * 512], F32)
        kpos = ph.sbuf("kpos", [128, NCH], F32)
        qf = ph.sbuf("sqf", [128, QTL * 512], F32)
        K.dma(K.sp, qf, qf[:, :], I["sq"], I["sq"][:, :])
        K.op(K.dve, lambda e: e.tensor_scalar(out=qT[:, :], in0=qf[:, :], scalar1=float(128.0 ** -0.5),
                                              scalar2=None, op0=ALU.mult), reads=[qf], writes=[qT])
        cast_load_cols(K, kT, I["skT"], S)
        cast_load_3d(K, v, I["sv"], NCH, 128)
        K.dma(K.sp, qpos, qpos[:, :], I["qpos"], I["qpos"][:, :])
        K.dma(K.sp, kpos, kpos[:, :], I["kpos"], I["kpos"][:, :])
        zp = [ph.psum(f"szp{i}", [128, 512]) for i in range(2)]
        R = ph.psum("sR", [128, 512])
        Op = ph.psum("sO", [128, 512])
        e_ = [ph.sbuf(f"se{i}", [128, 512], F32) for i in range(2)]
        sp = [ph.sbuf(f"ssp{i}", [128, 512], F32) for i in range(2)]
        spb = [ph.sbuf(f"sspb{i}", [128, 512], BF16) for i in range(2)]
        tmp = [ph.sbuf(f"stmp{i}", [128, 512], F32) for i in range(2)]
        A = [ph.sbuf(f"sA{i}", [128, 512], BF16) for i in range(2)]
        Am = [ph.sbuf(f"sAm{i}", [128, 512], BF16) for i in range(2)]
        msk = [ph.sbuf(f"smk{i}", [128, 512], F32) for i in range(2)]
        osb = [ph.sbuf(f"sos{i}", [128, 512], F32) for i in range(2)]
        step = 0
        for i in range(QTL):
            nb = 8 * i + 8
            q_ = qT[:, i * 512:(i + 1) * 512]
            for bi, kb in enumerate(range(nb - 1, -1, -1)):
                masked = bi < 8
                z = zp[step % 2]; ee = e_[step % 2]; s_ = sp[step % 2]; sb_ = spb[step % 2]
                t_ = tmp[step % 2]; a_ = A[step % 2]; am_ = Am[step % 2]; m_ = msk[step % 2]
                step += 1
                K.op(K.pe, lambda e: e.matmul(z[:, :], lhsT=kT[:, kb * 128:(kb + 1) * 128], rhs=q_,
                                              start=True, stop=True), reads=[kT, qT], writes=[z])
                K.op(K.act, lambda e: e.activation(out=ee[:, :], in_=z[:, :], func=AF.Exp), reads=[z], writes=[ee])
                K.op(K.act, lambda e: e.activation(out=s_[:, :], in_=ee[:, :], func=AF.Ln, bias=C["one"][:, 0:1]),
                     reads=[ee, C["one"]], writes=[s_])
                if masked:
                    K.op(K.pool, lambda e: e.tensor_scalar(out=m_[:, :], in0=qpos[:, i * 512:(i + 1) * 512],
                                                           scalar1=kpos[:, kb:kb + 1], scalar2=None,
                                                           op0=ALU.is_gt), reads=[qpos, kpos], writes=[m_])
                    K.op(K.pool, lambda e: e.tensor_tensor(out=sb_[:, :], in0=s_[:, :], in1=m_[:, :], op=ALU.mult),
                         reads=[s_, m_], writes=[sb_])
                else:
                    K.op(K.pool, lambda e: e.tensor_copy(out=sb_[:, :], in_=s_[:, :]), reads=[s_], writes=[sb_])
                K.op(K.dve, lambda e: e.tensor_tensor(out=t_[:, :], in0=z[:, :], in1=s_[:, :], op=ALU.subtract),
                     reads=[z, s_], writes=[t_])
                K.op(K.pe, lambda e: e.matmul(R[:, :], lhsT=C["Ustrict"][:, :], rhs=sb_[:, :],
                                              start=(bi == 0), stop=False, skip_group_check=True),
                     reads=[C["Ustrict"], sb_], writes=[R])
                K.op(K.dve, lambda e: e.tensor_tensor(out=t_[:, :], in0=t_[:, :], in1=R[:, :], op=ALU.subtract),
                     reads=[t_, R], writes=[t_])
                K.op(K.pe, lambda e: e.matmul(R[:, :], lhsT=C["Lincl"][:, :], rhs=sb_[:, :],
                                              start=False, stop=(kb == 0), skip_group_check=True),
                     reads=[C["Lincl"], sb_], writes=[R])
                K.op(K.act, lambda e: e.activation(out=a_[:, :], in_=t_[:, :], func=AF.Exp), reads=[t_], writes=[a_])
                if masked:
                    K.op(K.pool, lambda e: e.tensor_tensor(out=am_[:, :], in0=a_[:, :], in1=m_[:, :], op=ALU.mult),
                         reads=[a_, m_], writes=[am_])
                    ause = am_
                else:
                    ause = a_
                K.op(K.pe, lambda e: e.matmul(Op[:, :], lhsT=v[:, kb, :], rhs=ause[:, :],
                                              start=(bi == 0), stop=(kb == 0), skip_group_check=True),
                     reads=[v, ause], writes=[Op])
            o_ = osb[i % 2]
            K.op(K.act, lambda e: e.activation(out=o_[:, :], in_=Op[:, :], func=AF.Copy), reads=[Op], writes=[o_])
            K.dma(K.sp, O["y_sb"], O["y_sb"][:, i * 512:(i + 1) * 512], o_, o_[:, :], partial=True, owner=o_)


def mlstm_stage(K, C, I, O):
    with Phase(K) as ph:
        qT = ph.sbuf("mqT", [128, S], BF16)
        kT = ph.sbuf("mkT", [128, S], BF16)
        kt = ph.sbuf("mk", [128, NCH, 128], F32)
        va = ph.sbuf("mva", [128, NCH, 129], BF16)
        gi = ph.sbuf("mgi", [128, NCH], F32)
        gf = ph.sbuf("mgf", [128, NCH], F32)
        bi_ = ph.sbuf("mbi", [128, 1], F32)
        bf_ = ph.sbuf("mbf", [128, 1], F32)
        hg = ph.sbuf("mhg", [128, 1], F32)
        cast_load_cols(K, qT, I["mqT"], S)
        cast_load_cols(K, kT, I["mkT"], S)
        for c0 in range(0, NCH, 16):
            K.dma(K.sp, kt, kt[:, c0:c0 + 16, :], I["mk"], I["mk"][:, c0:c0 + 16, :], partial=(c0 > 0))
        cast_load_3d(K, va, I["mva"], NCH, 129)
        K.dma(K.sp, gi, gi[:, :], I["mgi"], I["mgi"][:, :])
        K.dma(K.sp, gf, gf[:, :], I["mgf"], I["mgf"][:, :])
        K.dma(K.sp, bi_, bi_[:, :], I["mbi"], I["mbi"][:, :])
        K.dma(K.sp, bf_, bf_[:, :], I["mbf"], I["mbf"][:, :])
        K.dma(K.sp, hg, hg[:, :], I["mhg"], I["mhg"][:, :])
        lf = ph.sbuf("mlf", [128, NCH], F32)
        nbf = ph.sbuf("mnbf", [128, 1], F32)
        K.op(K.dve, lambda e: e.tensor_scalar(out=nbf[:, :], in0=bf_[:, :], scalar1=-1.0, scalar2=None,
                                              op0=ALU.mult), reads=[bf_], writes=[nbf])
        K.op(K.act, lambda e: e.activation(out=lf[:, :], in_=gf[:, :], func=AF.Exp, scale=-1.0,
                                           bias=nbf[:, 0:1]), reads=[gf, nbf], writes=[lf])
        K.op(K.act, lambda e: e.activation(out=lf[:, :], in_=lf[:, :], func=AF.Ln, bias=C["one"][:, 0:1]),
             reads=[lf, C["one"]], writes=[lf])
        K.op(K.dve, lambda e: e.tensor_scalar(out=lf[:, :], in0=lf[:, :], scalar1=-1.0, scalar2=None,
                                              op0=ALU.mult), reads=[lf], writes=[lf])
        gps = ph.psum("mgps", [128, 2, NCH])
        K.op(K.pe, lambda e: e.matmul(gps[:, 0, :], lhsT=C["TriF"][:, :], rhs=lf[:, :], start=True, stop=True),
             reads=[C["TriF"], lf], writes=[gps])
        K.op(K.pe, lambda e: e.matmul(gps[:, 1, :], lhsT=C["onesF"][:, :], rhs=lf[:, :], start=True, stop=True),
             reads=[C["onesF"], lf], writes=[gps])
        ig = ph.sbuf("mig", [128, NCH], F32)
        K.op(K.dve, lambda e: e.tensor_scalar(out=ig[:, :], in0=gi[:, :], scalar1=bi_[:, 0:1], scalar2=None,
                                              op0=ALU.add), reads=[gi, bi_], writes=[ig])
        imb = ph.sbuf("mimb", [128, NCH], F32)
        K.op(K.dve, lambda e: e.tensor_tensor(out=imb[:, :], in0=ig[:, :], in1=gps[:, 0, :], op=ALU.subtract),
             reads=[ig, gps], writes=[imb])
        ek = ph.sbuf("mek", [128, NCH], F32)
        K.op(K.act, lambda e: e.activation(out=ek[:, :], in_=imb[:, :], func=AF.Exp, bias=C["lnsc"][:, 0:1]),
             reads=[imb, C["lnsc"]], writes=[ek])
        wk = ph.sbuf("mwk", [128, NCH], F32)
        K.op(K.dve, lambda e: e.tensor_tensor(out=wk[:, :], in0=imb[:, :], in1=gps[:, 1, :], op=ALU.add),
             reads=[imb, gps], writes=[wk])
        K.op(K.act, lambda e: e.activation(out=wk[:, :], in_=wk[:, :], func=AF.Exp, bias=C["lnsc"][:, 0:1]),
             reads=[wk, C["lnsc"]], writes=[wk])
        dec = ph.sbuf("mdec", [128, NCH], F32)
        K.op(K.act, lambda e: e.activation(out=dec[:, :], in_=gps[:, 1, :], func=AF.Exp), reads=[gps], writes=[dec])
        Cst = ph.sbuf("mC", [128, 129], F32)
        Cbf = ph.sbuf("mCbf", [128, 128], BF16)
        nbc = ph.sbuf("mnbc", [128, 128], BF16)
        K.op(K.dve, lambda e: e.memset(Cst[:, :], 0.0), writes=[Cst])
        K.op(K.dve, lambda e: e.memset(Cbf[:, :], 0.0), writes=[Cbf])
        K.op(K.dve, lambda e: e.memset(nbc[:, :], 0.0), writes=[nbc])
        sps = ph.psum("msps", [128, 128])
        bps = ph.psum("mbps", [128, 128])
        nps = ph.psum("mnps", [128, 128])
        dps = ph.psum("mdps", [128, 128])
        kvps = ph.psum("mkvps", [128, 129])
        mps = ph.psum("mmps", [128, 128])
        lfb = ph.sbuf("mlfb", [128, 128], F32)
        embt = ph.sbuf("membt", [128, 128], F32)
        PT = ph.sbuf("mPT", [128, 128], BF16)
        k2 = ph.sbuf("mk2", [128, 128], BF16)
        dm = ph.sbuf("mdm", [128, 128], F32)
        hh = ph.sbuf("mhh", [128, 128], F32)
        sq = ph.sbuf("msq", [128, 128], BF16)
        rt = ph.sbuf("mrt", [128, 128], F32)
        og = ph.sbuf("mog", [128, 128], F32)
        osg = ph.sbuf("mosg", [128, 128], F32)
        yo = [ph.sbuf(f"myo{i}", [128, 128], F32) for i in range(2)]
        for k in range(NCH):
            ts_ = slice(k * 128, (k + 1) * 128)
            K.dma(K.sp, og, og[:, :], I["moT"], I["moT"][:, ts_])
            K.op(K.pe, lambda e: e.matmul(sps[:, :], lhsT=kT[:, ts_], rhs=qT[:, ts_], start=True, stop=True),
                 reads=[kT, qT], writes=[sps])
            K.op(K.dve, lambda e: e.scalar_tensor_tensor(out=PT[:, :], in0=sps[:, :], scalar=ek[:, k:k + 1],
                                                         in1=C["causal"][:, :], op0=ALU.mult, op1=ALU.mult),
                 reads=[sps, ek, C["causal"]], writes=[PT])
            K.op(K.dve, lambda e: e.tensor_scalar(out=lfb[:, :], in0=C["onesF"][:, :], scalar1=lf[:, k:k + 1],
                                                  scalar2=None, op0=ALU.mult), reads=[C["onesF"], lf], writes=[lfb])
            K.op(K.pe, lambda e: e.matmul(bps[:, :], lhsT=lfb[:, :], rhs=C["TriF"][:, :], start=True, stop=True),
                 reads=[lfb, C["TriF"]], writes=[bps])
            K.op(K.act, lambda e: e.activation(out=embt[:, :], in_=bps[:, :], func=AF.Exp, scale=-1.0),
                 reads=[bps], writes=[embt])
            K.op(K.pe, lambda e: e.matmul(nps[:, :], lhsT=va[:, k, 0:128], rhs=PT[:, :], start=True, stop=False),
                 reads=[va, PT], writes=[nps])
            K.op(K.pe, lambda e: e.matmul(nps[:, :], lhsT=Cbf[:, :], rhs=qT[:, ts_], start=False, stop=True),
                 reads=[Cbf, qT], writes=[nps])
            K.op(K.pe, lambda e: e.matmul(dps[:, :], lhsT=C["ones"][:, :], rhs=PT[:, :], start=True, stop=False),
                 reads=[C["ones"], PT], writes=[dps])
            K.op(K.pe, lambda e: e.matmul(dps[:, :], lhsT=nbc[:, :], rhs=qT[:, ts_], start=False, stop=True),
                 reads=[nbc, qT], writes=[dps])
            K.op(K.act, lambda e: e.activation(out=dm[:, :], in_=dps[:, :], func=AF.Abs), reads=[dps], writes=[dm])
            K.op(K.dve, lambda e: e.tensor_tensor(out=dm[:, :], in0=dm[:, :], in1=embt[:, :], op=ALU.max),
                 reads=[dm, embt], writes=[dm])
            K.op(K.dve, lambda e: e.reciprocal(out=dm[:, :], in_=dm[:, :]), reads=[dm], writes=[dm])
            K.op(K.dve, lambda e: e.tensor_tensor(out=hh[:, :], in0=nps[:, :], in1=dm[:, :], op=ALU.mult),
                 reads=[nps, dm], writes=[hh])
            K.op(K.dve, lambda e: e.tensor_scalar(out=k2[:, :], in0=kt[:, k, :], scalar1=wk[:, k:k + 1],
                                                  scalar2=None, op0=ALU.mult), reads=[kt, wk], writes=[k2])
            K.op(K.pe, lambda e: e.matmul(kvps[:, :], lhsT=k2[:, :], rhs=va[:, k, :], start=True, stop=True),
                 reads=[k2, va], writes=[kvps])
            K.op(K.dve, lambda e: e.scalar_tensor_tensor(out=Cst[:, :], in0=Cst[:, :], scalar=dec[:, k:k + 1],
                                                         in1=kvps[:, :], op0=ALU.mult, op1=ALU.add),
                 reads=[Cst, dec, kvps], writes=[Cst])
            K.op(K.act, lambda e: e.activation(out=Cbf[:, :], in_=Cst[:, 0:128], func=AF.Copy),
                 reads=[Cst], writes=[Cbf])
            K.op(K.pool, lambda e: e.tensor_scalar(out=nbc[:, :], in0=C["onesF"][:, :], scalar1=Cst[:, 128:129],
                                                   scalar2=None, op0=ALU.mult), reads=[C["onesF"], Cst], writes=[nbc])
            K.op(K.act, lambda e: e.activation(out=sq[:, :], in_=hh[:, :], func=AF.Square), reads=[hh], writes=[sq])
            K.op(K.pe, lambda e: e.matmul(mps[:, :], lhsT=C["ones"][:, :], rhs=sq[:, :], start=True, stop=True),
                 reads=[C["ones"], sq], writes=[mps])
            K.op(K.act, lambda e: e.activation(out=rt[:, :], in_=mps[:, :], func=AF.Sqrt, scale=1.0 / 128,
                                               bias=C["eps"][:, 0:1]), reads=[mps, C["eps"]], writes=[rt])
            K.op(K.dve, lambda e: e.reciprocal(out=rt[:, :], in_=rt[:, :]), reads=[rt], writes=[rt])
            K.op(K.dve, lambda e: e.scalar_tensor_tensor(out=hh[:, :], in0=hh[:, :], scalar=hg[:, 0:1],
                                                         in1=rt[:, :], op0=ALU.mult, op1=ALU.mult),
                 reads=[hh, hg, rt], writes=[hh])
            K.op(K.act, lambda e: e.activation(out=osg[:, :], in_=og[:, :], func=AF.Sigmoid), reads=[og], writes=[osg])
            y_ = yo[k % 2]
            K.op(K.dve, lambda e: e.tensor_tensor(out=y_[:, :], in0=hh[:, :], in1=osg[:, :], op=ALU.mult),
                 reads=[hh, osg], writes=[y_])
            K.dma(K.sp, O["y_ml"], O["y_ml"][:, ts_], y_, y_[:, :], partial=True, owner=y_)


def load_mixer_consts(K, st, C, I):
    for name, dt in (("Ustrict", BF16), ("Lincl", BF16), ("causal", F32), ("TriF", F32), ("onesF", F32)):
        C[name] = K.sbuf(st, "c_" + name, [128, 128], dt)
        q = K.pool if dt == BF16 else K.sp
        K.dma(q, C[name], C[name][:, :], I["k_" + name], I["k_" + name][:, :])
    C["one"] = K.sbuf(st, "c_one", [128, 1], F32)
    C["lnsc"] = K.sbuf(st, "c_lnsc", [128, 1], F32)
    K.op(K.dve, lambda e: e.memset(C["one"][:, :], 1.0), writes=[C["one"]])
    K.op(K.dve, lambda e: e.memset(C["lnsc"][:, :], LNSC), writes=[C["lnsc"]])


def mixer_consts_host():
    j = np.arange(128)[:, None]
    s = np.arange(128)[None, :]
    return {
        "k_Ustrict": (j > s).astype(np.float32),
        "k_Lincl": (j <= s).astype(np.float32),
        "k_causal": (j <= s).astype(np.float32),
        "k_TriF": (j <= s).astype(np.float32),
        "k_onesF": np.ones((128, 128), np.float32),
    }


OFF = dict(cb=0, cc=512, cu=1024, mq=1536, mk=2048, mv=2560, mo=3072, mi=3584, mf=3588, pu=3592,
           sq=4104, sk=4616, sv=5128)


def _fm_halo(a, c, halo):
    lo = c * T - halo
    if lo < 0:
        blk = np.concatenate([np.zeros((-lo, a.shape[1]), a.dtype), a[0:(c + 1) * T]], axis=0)
    else:
        blk = a[lo:(c + 1) * T]
    return np.ascontiguousarray(blk.T.reshape(4, 128, halo + T))


def sb_tiles(c):
    r = c // 4
    return [2 * i + r for i in range(QTL)]


def mixer_inputs(proj, conv_w, pool_w, pool_scale, i_bias, f_bias, head_gain, c):
    h = c % 4
    hs = slice(h * 128, (h + 1) * 128)
    g = lambda name: proj[:, OFF[name]:OFF[name] + 512]
    I = {}
    I["cb"] = _fm_halo(g("cb"), c, 0)
    I["cc"] = _fm_halo(g("cc"), c, 2)
    I["cu"] = _fm_halo(g("cu"), c, 2)
    I["pu"] = _fm_halo(g("pu"), c, 15)
    I["conv_w"] = np.ascontiguousarray(conv_w.reshape(3, 4, 128).transpose(2, 1, 0))
    I["pool_w"] = np.ascontiguousarray(pool_w.transpose(1, 0, 2))
    I["pool_scale"] = np.ascontiguousarray(pool_scale.reshape(4, 128).T)
    t = np.arange(c * T, (c + 1) * T)
    cnt = np.stack([np.minimum(t + 1, w) for w in (2, 4, 8, 16)], axis=0).astype(np.float32)
    I["invcnt"] = np.ascontiguousarray(np.broadcast_to((1.0 / cnt)[:, None, :], (4, 128, T))).astype(np.float32)
    mq, mk, mv, mo = g("mq")[:, hs], g("mk")[:, hs], g("mv")[:, hs], g("mo")[:, hs]
    I["mqT"] = np.ascontiguousarray(mq.T)
    I["mkT"] = np.ascontiguousarray(mk.T)
    I["mk"] = np.ascontiguousarray(mk.reshape(NCH, 128, 128).transpose(1, 0, 2))
    va = np.concatenate([mv, np.ones((S, 1), np.float32)], axis=1)
    I["mva"] = np.ascontiguousarray(va.reshape(NCH, 128, 129).transpose(1, 0, 2))
    I["moT"] = np.ascontiguousarray(mo.T)
    I["mgi"] = np.ascontiguousarray(proj[:, OFF["mi"] + h].reshape(NCH, 128).T)
    I["mgf"] = np.ascontiguousarray(proj[:, OFF["mf"] + h].reshape(NCH, 128).T)
    I["mbi"] = np.full((128, 1), i_bias[h], np.float32)
    I["mbf"] = np.full((128, 1), f_bias[h], np.float32)
    I["mhg"] = np.ascontiguousarray(head_gain[hs].reshape(128, 1))
    sq, sk, sv = g("sq")[:, hs], g("sk")[:, hs], g("sv")[:, hs]
    tiles = sb_tiles(c)
    qsel = np.concatenate([sq[ti * 512:(ti + 1) * 512] for ti in tiles], axis=0)
    I["sq"] = np.ascontiguousarray(qsel.T)
    I["skT"] = np.ascontiguousarray(sk.T)
    I["sv"] = np.ascontiguousarray(sv.reshape(NCH, 128, 128).transpose(1, 0, 2))
    qp = np.concatenate([np.arange(ti * 512, (ti + 1) * 512) for ti in tiles]).astype(np.float32)
    I["qpos"] = np.ascontiguousarray(np.broadcast_to(qp[None, :], (128, QTL * 512)))
    I["kpos"] = np.ascontiguousarray((np.arange(NCH)[None, :] * 128 + np.arange(128)[:, None]).astype(np.float32))
    I.update(mixer_consts_host())
    return I


MIX_IN_SHAPES = dict(cb=[4, 128, T], cc=[4, 128, T + 2], cu=[4, 128, T + 2], pu=[4, 128, T + 15],
                     conv_w=[128, 4, 3], pool_w=[128, 4, 128], pool_scale=[128, 4], invcnt=[4, 128, T],
                     mqT=[128, S], mkT=[128, S], mk=[128, NCH, 128], mva=[128, NCH, 129], moT=[128, S],
                     mgi=[128, NCH], mgf=[128, NCH], mbi=[128, 1], mbf=[128, 1], mhg=[128, 1],
                     sq=[128, QTL * 512], skT=[128, S], sv=[128, NCH, 128], qpos=[128, QTL * 512],
                     kpos=[128, NCH], k_Ustrict=[128, 128], k_Lincl=[128, 128], k_causal=[128, 128],
                     k_TriF=[128, 128], k_onesF=[128, 128])
MIX_OUT_SHAPES = dict(y_conv=[4, 128, T], y_pool=[4, 128, T], y_ml=[128, S], y_sb=[128, QTL * 512])


def build_mixer_prog(parts=("cp", "ml", "sb")):
    nc = bass.Bass("TRN2", target_bir_lowering=False)
    with ExitStack() as es:
        K = Kern(nc, es)
        I = {k: K.dram(k, v, F32, "ExternalInput") for k, v in MIX_IN_SHAPES.items()}
        O = {k: K.dram(k, v, F32, "ExternalOutput") for k, v in MIX_OUT_SHAPES.items()}
        C = {}
        C["ones"] = K.sbuf(es, "c_ones", [128, 128], BF16)
        C["eps"] = K.sbuf(es, "c_eps", [128, 1], F32)
        K.op(K.dve, lambda e: e.memset(C["ones"][:, :], 1.0), writes=[C["ones"]])
        K.op(K.dve, lambda e: e.memset(C["eps"][:, :], EPS), writes=[C["eps"]])
        load_mixer_consts(K, es, C, I)
        if "cp" in parts:
            conv_pool_stage(K, C, I, O)
        if "ml" in parts:
            mlstm_stage(K, C, I, O)
        if "sb" in parts:
            sb_stage(K, C, I, O)
        finish(K, list(O.values()))
    return nc


def assemble_mix(results):
    y = np.zeros((S, D), np.float32)
    for c, r in enumerate(results):
        ts = slice(c * T, (c + 1) * T)
        y[ts, 0:512] = r["y_conv"].reshape(512, T).T
        y[ts, 1024:1536] = r["y_pool"].reshape(512, T).T
        h = c % 4
        if c < 4:
            y[:, 512 + h * 128:512 + (h + 1) * 128] = r["y_ml"].T
        for i, ti in enumerate(sb_tiles(c)):
            y[ti * 512:(ti + 1) * 512, 1536 + h * 128:1536 + (h + 1) * 128] = r["y_sb"][:, i * 512:(i + 1) * 512].T
    return y


NPJ = 45


def inproj_stage(K, C, x_src, projT, win, gi):
    gains = C["gains"]
    with Phase(K) as p1:
        hT = p1.sbuf("i_hT", [128, DC, T], BF16)
        norm_in_phase(K, C, x_src, lambda c: gains[:, gi * DC + c:gi * DC + c + 1], hT)
        with Phase(K) as p2:
            wb = [p2.sbuf(f"i_wb{i}", [128, DC, 128], BF16) for i in range(3)]
            ps = [p2.psum(f"i_ps{i}", [128, T]) for i in range(2)]
            ob = [p2.sbuf(f"i_ob{i}", [128, T], F32) for i in range(2)]
            for j in range(NPJ):
                w = wb[j % 3]; p = ps[j % 2]; o_ = ob[j % 2]
                K.dma(K.pool, w, w[:, :, :], win, win[j])
                for kc in range(DC):
                    for h in range(T // 512):
                        K.op(K.pe, lambda e: e.matmul(p[:, h * 512:(h + 1) * 512], lhsT=w[:, kc, :],
                                                      rhs=hT[:, kc, h * 512:(h + 1) * 512],
                                                      start=(kc == 0), stop=(kc == DC - 1)),
                             reads=[w, hT], writes=[p])
                K.op(K.act, lambda e: e.activation(out=o_[:, :], in_=p[:, :], func=AF.Copy), reads=[p], writes=[o_])
                K.dma(K.sp, projT, projT[j], o_, o_[:, :], partial=True, owner=o_)


def outproj_stage(K, C, x_src, ymix, wout, yT, x_dst, gi):
    gains = C["gains"]
    with Phase(K) as p1:
        yb = p1.sbuf("o_yb", [128, DC, T], BF16)
        rstd = p1.sbuf("o_rstd", [128, T], F32)
        for c in range(DC):
            K.dma(K.pool, yb, yb[:, c, :], ymix, ymix[c], partial=(c > 0))
        down_phase(K, C, p1, yb, DC, wout, yT, rstd)
        epilogue_phase(K, C, x_src, yT, rstd, lambda c: gains[:, gi * DC + c:gi * DC + c + 1], x_dst, half=False)


def build_pa():
    nc = bass.Bass("TRN2", target_bir_lowering=False)
    with ExitStack() as es:
        K = Kern(nc, es)
        x_in = K.dram("x_in", [DC, 128, T], F32, "ExternalInput")
        wgu = K.dram("wgu", [FC, 128, 2, DC, 128], F32, "ExternalInput")
        wd = K.dram("wd", [DC, 128, FC, 128], F32, "ExternalInput")
        win = K.dram("win", [NPJ, 128, DC, 128], F32, "ExternalInput")
        gains = K.dram("gains", [128, 6 * DC], F32, "ExternalInput")
        x_out = K.dram("x_out", [DC, 128, T], F32, "ExternalOutput")
        projT = K.dram("projT", [NPJ, 128, T], F32, "ExternalOutput")
        yT = K.dram("yT", [DC, 128, T], F32, "Internal")
        C = load_consts(K, es, gains)
        ffn_stage(K, C, x_in, x_out, yT, wgu, wd, 0)
        inproj_stage(K, C, x_out, projT, win, 2)
        finish(K, [x_out, projT])
    return nc


def build_pc():
    nc = bass.Bass("TRN2", target_bir_lowering=False)
    with ExitStack() as es:
        K = Kern(nc, es)
        x_in = K.dram("x_in", [DC, 128, T], F32, "ExternalInput")
        ymix = K.dram("ymix", [DC, 128, T], F32, "ExternalInput")
        wout = K.dram("wout", [DC, 128, DC, 128], F32, "ExternalInput")
        wgu = K.dram("wgu", [FC, 128, 2, DC, 128], F32, "ExternalInput")
        wd = K.dram("wd", [DC, 128, FC, 128], F32, "ExternalInput")
        gains = K.dram("gains", [128, 6 * DC], F32, "ExternalInput")
        x_out = K.dram("x_out", [DC, 128, T], F32, "ExternalOutput")
        x_mid = K.dram("x_mid", [DC, 128, T], F32, "Internal")
        yT = K.dram("yT", [DC, 128, T], F32, "Internal")
        C = load_consts(K, es, gains)
        outproj_stage(K, C, x_in, ymix, wout, yT, x_mid, 3)
        ffn_stage(K, C, x_mid, x_out, yT, wgu, wd, 4)
        finish(K, [x_out])
    return nc


def tile_kn(w, nk, nn):
    return np.ascontiguousarray(w.reshape(nk, 128, nn, 128).transpose(2, 1, 0, 3))


CH = dict(cb=0, cc=4, cu=8, mq=12, mk=16, mv=20, mo=24, pu=28, sq=32, sk=36, sv=40, gates=44)
NBLK = 5
B_MO, B_GATES, B_HALO, HALO_C0 = 0, 4, 4, 64
NBLKH = 32
B_MQ, B_MKT, B_SQ, B_SQ1, B_SKT, B_MK, B_MV, B_SV = 0, 4, 8, 12, 16, 20, 24, 28
NBLK2 = 16


def blkh(buf, r, b):
    o = r * 256 + (b % 2) * 128
    return buf[b // 2][o:o + 128, :]


def blk(buf, r, b, nb):
    return buf[b][r * 128:(r + 1) * 128, :]


def inproj_stage_f(K, C, x_src, projT, PAY, PAYH, win_l, gi):
    gains = C["gains"]
    with Phase(K) as p1:
        hT = p1.sbuf("i_hT", [128, DC, T], BF16)
        norm_in_phase(K, C, x_src, lambda c: gains[:, gi * DC + c:gi * DC + c + 1], hT)
        with Phase(K) as p2:
            wb = [p2.sbuf(f"i_wb{i}", [128, DC, 128], BF16) for i in range(3)]
            ps = [p2.psum(f"i_ps{i}", [128, T]) for i in range(2)]
            ob = [p2.sbuf(f"i_ob{i}", [128, T], F32) for i in range(2)]
            pt = [p2.psum(f"i_pt{i}", [128, T]) for i in range(2)]
            ot = [p2.sbuf(f"i_ot{i}", [128, T], BF16) for i in range(2)]
            gsb = p2.sbuf("i_gsb", [128, 8, 8], F32)
            ntm = 0
            for j in range(NPJ):
                w = wb[j % 3]; p = ps[j % 2]; o_ = ob[j % 2]
                K.dma(K.pool, w, w[:, :, :], win_l, win_l[j])
                if j < NPJ - 1:
                    for kc in range(DC):
                        for h in range(T // 512):
                            K.op(K.pe, lambda e: e.matmul(p[:, h * 512:(h + 1) * 512], lhsT=w[:, kc, :],
                                                          rhs=hT[:, kc, h * 512:(h + 1) * 512],
                                                          start=(kc == 0), stop=(kc == DC - 1)),
                                 reads=[w, hT], writes=[p])
                    K.op(K.act, lambda e: e.activation(out=o_[:, :], in_=p[:, :], func=AF.Copy),
                         reads=[p], writes=[o_])
                    K.dma(K.sp, projT, projT[j], o_, o_[:, :], partial=True, owner=o_)
                tmb = None
                for nm, b0 in (("mk", B_MK), ("mv", B_MV), ("sv", B_SV)):
                    if CH[nm] <= j < CH[nm] + 4:
                        tmb = b0 + (j - CH[nm])
                if tmb is not None:
                    q_ = pt[ntm % 2]; t_ = ot[ntm % 2]; ntm += 1
                    for tb in range(T // 128):
                        for kc in range(DC):
                            K.op(K.pe, lambda e: e.matmul(q_[:, tb * 128:(tb + 1) * 128],
                                                          lhsT=hT[:, kc, tb * 128:(tb + 1) * 128], rhs=w[:, kc, :],
                                                          start=(kc == 0), stop=(kc == DC - 1)),
                                 reads=[w, hT], writes=[q_])
                    K.op(K.act, lambda e: e.activation(out=t_[:, :], in_=q_[:, :], func=AF.Copy),
                         reads=[q_], writes=[t_])
                    K.dma(K.sp, PAYH, PAYH[tmb * 128:(tmb + 1) * 128, :], t_, t_[:, :], partial=True, owner=t_)
                if j == CH["gates"]:
                    q_ = pt[ntm % 2]; ntm += 1
                    for tb in range(T // 128):
                        for kc in range(DC):
                            K.op(K.pe, lambda e: e.matmul(q_[:, tb * 8:(tb + 1) * 8],
                                                          lhsT=hT[:, kc, tb * 128:(tb + 1) * 128], rhs=w[:, kc, 0:8],
                                                          start=(kc == 0), stop=(kc == DC - 1)),
                                 reads=[w, hT], writes=[q_])
                    K.op(K.act, lambda e: e.activation(out=gsb[:, :, :],
                                                       in_=q_[:, 0:64].rearrange("p (tb g) -> p g tb", g=8),
                                                       func=AF.Copy), reads=[q_], writes=[gsb])
                    K.dma(K.sp, PAY, PAY[B_GATES * 128:(B_GATES + 1) * 128, 0:64],
                          gsb, gsb[:, :, :].rearrange("p g tb -> p (g tb)"), partial=True, owner=gsb)
    for h in range(4):
        K.dma(K.sp, PAY, PAY[(B_MO + h) * 128:(B_MO + h + 1) * 128, :], projT, projT[CH["mo"] + h], partial=True)
    for nm, b0 in (("mq", B_MQ), ("mk", B_MKT), ("sk", B_SKT)):
        for h in range(4):
            K.dma(K.pool, PAYH, PAYH[(b0 + h) * 128:(b0 + h + 1) * 128, :], projT, projT[CH[nm] + h], partial=True)
    for h in range(4):
        for half, b0 in ((0, B_SQ), (1, B_SQ1)):
            K.dma(K.pool, PAYH, PAYH[(b0 + h) * 128:(b0 + h + 1) * 128, 0:512], projT,
                  projT[CH["sq"] + h][:, half * 512:(half + 1) * 512], partial=True)
    hb = PAY[B_HALO * 128:(B_HALO + 1) * 128, :]
    for ch in range(4):
        K.dma(K.sp, PAY, hb[:, HALO_C0 + ch * 2:HALO_C0 + ch * 2 + 2], projT, projT[CH["cc"] + ch][:, T - 2:T],
              partial=True)
        K.dma(K.sp, PAY, hb[:, HALO_C0 + 8 + ch * 2:HALO_C0 + 8 + ch * 2 + 2], projT,
              projT[CH["cu"] + ch][:, T - 2:T], partial=True)
        K.dma(K.sp, PAY, hb[:, HALO_C0 + 16 + ch * 15:HALO_C0 + 16 + ch * 15 + 15], projT,
              projT[CH["pu"] + ch][:, T - 15:T], partial=True)


def conv_pool_stage_f(K, C, L, projT, GAT, ymixL, zeros):
    def halo_var(dst_ap, c0, n):
        def f(v):
            if v == 0:
                return [(dst_ap, zeros[:, 0:n])]
            return [(dst_ap, blk(GAT, v - 1, B_HALO, NBLK)[:, HALO_C0 + c0:HALO_C0 + c0 + n])]
        return f
    with Phase(K) as ph:
        cw = ph.sbuf("cw", [128, 4, 3], F32)
        psc = ph.sbuf("psc", [128, 4], F32)
        pw = ph.sbuf("pw", [128, 4, 128], BF16)
        K.dma(K.sp, cw, cw[:, :, :], L["conv_w"], L["conv_w"][:, :, :])
        K.dma(K.sp, psc, psc[:, :], L["pool_scale"], L["pool_scale"][:, :])
        K.dma(K.pool, pw, pw[:, :, :], L["pool_w"], L["pool_w"][:, :, :])
        for ch in range(4):
            cc = ph.sbuf("cc", [128, T + 2], F32)
            cu = ph.sbuf("cu", [128, T + 2], F32)
            cb = ph.sbuf("cb", [128, T], F32)
            acc = ph.sbuf("acc", [128, T], F32)
            K.dma(K.sp, cc, cc[:, 2:T + 2], projT, projT[CH["cc"] + ch])
            K.dma_sel(cc, GAT, halo_var(cc[:, 0:2], ch * 2, 2), partial=True)
            K.dma(K.sp, cu, cu[:, 2:T + 2], projT, projT[CH["cu"] + ch])
            K.dma_sel(cu, GAT, halo_var(cu[:, 0:2], 8 + ch * 2, 2), partial=True)
            K.dma(K.sp, cb, cb[:, :], projT, projT[CH["cb"] + ch])
            K.op(K.dve, lambda e: e.tensor_tensor(out=cc[:, :], in0=cc[:, :], in1=cu[:, :], op=ALU.mult),
                 reads=[cu, cc], writes=[cc])
            K.op(K.dve, lambda e: e.tensor_scalar(out=acc[:, :], in0=cc[:, 2:T + 2], scalar1=cw[:, ch, 2:3],
                                                  scalar2=None, op0=ALU.mult), reads=[cc, cw], writes=[acc])
            for j in (1, 0):
                K.op(K.dve, lambda e: e.scalar_tensor_tensor(out=acc[:, :], in0=cc[:, j:T + j],
                                                             scalar=cw[:, ch, j:j + 1], in1=acc[:, :],
                                                             op0=ALU.mult, op1=ALU.add),
                     reads=[cc, cw, acc], writes=[acc])
            K.op(K.dve, lambda e: e.tensor_tensor(out=acc[:, :], in0=acc[:, :], in1=cb[:, :], op=ALU.mult),
                 reads=[acc, cb], writes=[acc])
            K.dma(K.sp, ymixL, ymixL[ch], acc, acc[:, :], partial=True, owner=acc)
            u = ph.sbuf("pu", [128, T + 15], F32)
            sa = ph.sbuf("psa", [128, T + 15], F32)
            sb = ph.sbuf("psb", [128, T + 15], F32)
            ic = ph.sbuf("pic", [128, T], F32)
            pl = ph.sbuf("ppl", [128, T], BF16)
            yo = ph.sbuf("pyo", [128, T], F32)
            pps = ph.psum("pps", [128, T])
            K.dma(K.sp, u, u[:, 15:T + 15], projT, projT[CH["pu"] + ch])
            K.dma_sel(u, GAT, halo_var(u[:, 0:15], 16 + ch * 15, 15), partial=True)
            K.dma(K.sp, ic, ic[:, :], L["invcnt"], L["invcnt"][ch])
            cur, nxt = u, sa
            sh = 1
            for lvl in range(ch + 1):
                c_, n_ = cur, nxt
                K.op(K.dve, lambda e: e.tensor_tensor(out=n_[:, sh:T + 15], in0=c_[:, sh:T + 15],
                                                      in1=c_[:, 0:T + 15 - sh], op=ALU.add),
                     reads=[c_], writes=[n_])
                cur = nxt
                nxt = sb if cur is sa else sa
                sh *= 2
            ws = cur
            K.op(K.dve, lambda e: e.tensor_tensor(out=ws[:, 15:T + 15], in0=ws[:, 15:T + 15], in1=ic[:, :],
                                                  op=ALU.mult), reads=[ws, ic], writes=[ws])
            K.op(K.dve, lambda e: e.tensor_tensor(out=pl[:, :], in0=ws[:, 15:T + 15], in1=u[:, 15:T + 15],
                                                  op=ALU.subtract), reads=[ws, u], writes=[pl])
            for h in range(T // 512):
                K.op(K.pe, lambda e: e.matmul(pps[:, h * 512:(h + 1) * 512], lhsT=pw[:, ch, :],
                                              rhs=pl[:, h * 512:(h + 1) * 512], start=True, stop=True),
                     reads=[pw, pl], writes=[pps])
            K.op(K.act, lambda e: e.activation(out=yo[:, :], in_=pps[:, :], func=AF.Copy,
                                               scale=psc[:, ch:ch + 1]), reads=[pps, psc], writes=[yo])
            K.dma(K.sp, ymixL, ymixL[4 + ch], yo, yo[:, :], partial=True, owner=yo)


class GatIn:
    def __init__(self, K, GAT, GATH):
        self.K = K
        self.GAT = GAT
        self.GATH = GATH

    def cols(self, buf, b0, per_rank_cols=T):
        n = per_rank_cols
        self.K.dma_sel(buf, self.GATH, lambda v: [(buf[:, r * n:(r + 1) * n], blkh(self.GATH, r, b0 + v % 4)[:, 0:n])
                                                  for r in range(NCORES)])

    def cols32(self, buf, b0):
        self.K.dma_sel(buf, self.GAT, lambda v: [(buf[:, r * T:(r + 1) * T], blk(self.GAT, r, b0 + v % 4, NBLK))
                                                 for r in range(NCORES)])

    def tm(self, buf, b0, width=128):
        self.K.dma_sel(buf, self.GATH, lambda v: [(buf[:, 8 * r:8 * r + 8, 0:128],
                                                   blkh(self.GATH, r, b0 + v % 4).rearrange("p (a b) -> p a b", b=128))
                                                  for r in range(NCORES)], partial=True)

    def gate(self, buf, g0):
        self.K.dma_sel(buf, self.GAT, lambda v: [(buf[:, 8 * r:8 * r + 8],
                                                  blk(self.GAT, r, B_GATES, NBLK)[:, (g0 + v % 4) * 8:(g0 + v % 4) * 8 + 8])
                                                 for r in range(NCORES)])


def sb_stage_f(K, C, L, GI, PAY2):
    with Phase(K) as ph:
        qT = ph.sbuf("sqT", [128, QTL * 512], BF16)
        kT = ph.sbuf("skT", [128, S], BF16)
        v = ph.sbuf("sv", [128, NCH, 128], BF16)
        qpos = ph.sbuf("qpos", [128, QTL * 512], F32)
        kpos = ph.sbuf("kpos", [128, NCH], F32)
        qf = ph.sbuf("sqf", [128, QTL * 512], F32)
        K.dma_sel(qf, GI.GATH, lambda c_: [(qf[:, r * 512:(r + 1) * 512],
                                            blkh(GI.GATH, r, (B_SQ1 if c_ // 4 else B_SQ) + c_ % 4)[:, 0:512])
                                           for r in range(NCORES)])
        K.op(K.dve, lambda e: e.tensor_scalar(out=qT[:, :], in0=qf[:, :], scalar1=float(128.0 ** -0.5),
                                              scalar2=None, op0=ALU.mult), reads=[qf], writes=[qT])
        GI.cols(kT, B_SKT)
        GI.tm(v, B_SV)
        K.dma(K.sp, qpos, qpos[:, :], L["qpos"], L["qpos"][:, :])
        K.dma(K.sp, kpos, kpos[:, :], L["kpos"], L["kpos"][:, :])
        def chain_bufs(cid):
            B = {}
            B["zp"] = [ph.psum(f"szp{cid}{i}", [128, 512]) for i in range(2)]
            B["R"] = ph.psum(f"sR{cid}", [128, 512])
            B["O"] = ph.psum(f"sO{cid}", [128, 512])
            for nm, dt in (("e", F32), ("sp", F32), ("spb", BF16), ("tmp", F32), ("A", BF16), ("Am", BF16),
                           ("msk", F32), ("os", BF16)):
                B[nm] = [ph.sbuf(f"s{nm}{cid}{i}", [128, 512], dt) for i in range(2)]
            return B

        def tile_steps(i, B):
            R = B["R"]; Op = B["O"]
            nb = 8 * i + 8
            q_ = qT[:, i * 512:(i + 1) * 512]
            for bi, kb in enumerate(range(nb - 1, -1, -1)):
                masked = bi < 8
                k2_ = bi % 2
                z = B["zp"][k2_]; ee = B["e"][k2_]; s_ = B["sp"][k2_]; sb_ = B["spb"][k2_]
                t_ = B["tmp"][k2_]; a_ = B["A"][k2_]; am_ = B["Am"][k2_]; m_ = B["msk"][k2_]
                K.op(K.pe, lambda e: e.matmul(z[:, :], lhsT=kT[:, kb * 128:(kb + 1) * 128], rhs=q_,
                                              start=True, stop=True), reads=[kT, qT], writes=[z])
                K.op(K.act, lambda e: e.activation(out=ee[:, :], in_=z[:, :], func=AF.Exp), reads=[z], writes=[ee])
                K.op(K.act, lambda e: e.activation(out=s_[:, :], in_=ee[:, :], func=AF.Ln, bias=C["one"][:, 0:1]),
                     reads=[ee, C["one"]], writes=[s_])
                if masked:
                    K.op(K.pool, lambda e: e.tensor_scalar(out=m_[:, :], in0=qpos[:, i * 512:(i + 1) * 512],
                                                           scalar1=kpos[:, kb:kb + 1], scalar2=None,
                                                           op0=ALU.is_gt), reads=[qpos, kpos], writes=[m_])
                    K.op(K.pool, lambda e: e.tensor_tensor(out=sb_[:, :], in0=s_[:, :], in1=m_[:, :], op=ALU.mult),
                         reads=[s_, m_], writes=[sb_])
                else:
                    K.op(K.pool, lambda e: e.tensor_copy(out=sb_[:, :], in_=s_[:, :]), reads=[s_], writes=[sb_])
                K.op(K.dve, lambda e: e.tensor_tensor(out=t_[:, :], in0=z[:, :], in1=s_[:, :], op=ALU.subtract),
                     reads=[z, s_], writes=[t_])
                K.op(K.pe, lambda e: e.matmul(R[:, :], lhsT=C["Ustrict"][:, :], rhs=sb_[:, :],
                                              start=(bi == 0), stop=False, skip_group_check=True),
                     reads=[C["Ustrict"], sb_], writes=[R])
                yield
                K.op(K.dve, lambda e: e.tensor_tensor(out=t_[:, :], in0=t_[:, :], in1=R[:, :], op=ALU.subtract),
                     reads=[t_, R], writes=[t_])
                K.op(K.pe, lambda e: e.matmul(R[:, :], lhsT=C["Lincl"][:, :], rhs=sb_[:, :],
                                              start=False, stop=(kb == 0), skip_group_check=True),
                     reads=[C["Lincl"], sb_], writes=[R])
                K.op(K.act, lambda e: e.activation(out=a_[:, :], in_=t_[:, :], func=AF.Exp), reads=[t_], writes=[a_])
                if masked:
                    K.op(K.pool, lambda e: e.tensor_tensor(out=am_[:, :], in0=a_[:, :], in1=m_[:, :], op=ALU.mult),
                         reads=[a_, m_], writes=[am_])
                    ause = am_
                else:
                    ause = a_
                K.op(K.pe, lambda e: e.matmul(Op[:, :], lhsT=v[:, kb, :], rhs=ause[:, :],
                                              start=(bi == 0), stop=(kb == 0), skip_group_check=True),
                     reads=[v, ause], writes=[Op])
                yield
            o_ = B["os"][0]
            K.op(K.act, lambda e: e.activation(out=o_[:, :], in_=Op[:, :], func=AF.Copy), reads=[Op], writes=[o_])
            K.dma(K.sp, PAY2, PAY2[(8 + i) * 128:(9 + i) * 128, 0:512], o_, o_[:, :], partial=True, owner=o_)
            yield

        CB = [chain_bufs(0), chain_bufs(1)]
        queue = list(range(QTL - 1, -1, -1))
        active = [None, None]
        while queue or any(a is not None for a in active):
            for c_ in range(2):
                if active[c_] is None and queue:
                    active[c_] = tile_steps(queue.pop(0), CB[c_])
                if active[c_] is not None:
                    try:
                        next(active[c_])
                    except StopIteration:
                        active[c_] = None


def mlstm_stage_f(K, C, L, GI, PAY2):
    with Phase(K) as ph:
        qT = ph.sbuf("mqT", [128, S], BF16)
        kT = ph.sbuf("mkT", [128, S], BF16)
        kt = ph.sbuf("mk", [128, NCH, 128], F32)
        va = ph.sbuf("mva", [128, NCH, 129], BF16)
        gi = ph.sbuf("mgi", [128, NCH], F32)
        gf = ph.sbuf("mgf", [128, NCH], F32)
        bi_ = ph.sbuf("mbi", [128, 1], F32)
        bf_ = ph.sbuf("mbf", [128, 1], F32)
        hg = ph.sbuf("mhg", [128, 1], F32)
        ogf = ph.sbuf("mogf", [128, S], F32)
        GI.cols(qT, B_MQ)
        GI.cols(kT, B_MKT)
        GI.cols32(ogf, B_MO)
        K.op(K.dve, lambda e: e.memset(va[:, :, 128:129], 1.0), writes=[va])
        GI.tm(kt, B_MK)
        GI.tm(va, B_MV)
        GI.gate(gi, 0)
        GI.gate(gf, 4)
        K.dma(K.sp, bi_, bi_[:, :], L["mbi"], L["mbi"][:, :])
        K.dma(K.sp, bf_, bf_[:, :], L["mbf"], L["mbf"][:, :])
        K.dma(K.sp, hg, hg[:, :], L["mhg"], L["mhg"][:, :])
        lf = ph.sbuf("mlf", [128, NCH], F32)
        nbf = ph.sbuf("mnbf", [128, 1], F32)
        K.op(K.dve, lambda e: e.tensor_scalar(out=nbf[:, :], in0=bf_[:, :], scalar1=-1.0, scalar2=None,
                                              op0=ALU.mult), reads=[bf_], writes=[nbf])
        K.op(K.act, lambda e: e.activation(out=lf[:, :], in_=gf[:, :], func=AF.Exp, scale=-1.0,
                                           bias=nbf[:, 0:1]), reads=[gf, nbf], writes=[lf])
        K.op(K.act, lambda e: e.activation(out=lf[:, :], in_=lf[:, :], func=AF.Ln, bias=C["one"][:, 0:1]),
             reads=[lf, C["one"]], writes=[lf])
        K.op(K.dve, lambda e: e.tensor_scalar(out=lf[:, :], in0=lf[:, :], scalar1=-1.0, scalar2=None,
                                              op0=ALU.mult), reads=[lf], writes=[lf])
        gps = ph.psum("mgps", [128, 2, NCH])
        K.op(K.pe, lambda e: e.matmul(gps[:, 0, :], lhsT=C["TriF"][:, :], rhs=lf[:, :], start=True, stop=True),
             reads=[C["TriF"], lf], writes=[gps])
        K.op(K.pe, lambda e: e.matmul(gps[:, 1, :], lhsT=C["onesF"][:, :], rhs=lf[:, :], start=True, stop=True),
             reads=[C["onesF"], lf], writes=[gps])
        ig = ph.sbuf("mig", [128, NCH], F32)
        K.op(K.dve, lambda e: e.tensor_scalar(out=ig[:, :], in0=gi[:, :], scalar1=bi_[:, 0:1], scalar2=None,
                                              op0=ALU.add), reads=[gi, bi_], writes=[ig])
        imb = ph.sbuf("mimb", [128, NCH], F32)
        K.op(K.dve, lambda e: e.tensor_tensor(out=imb[:, :], in0=ig[:, :], in1=gps[:, 0, :], op=ALU.subtract),
             reads=[ig, gps], writes=[imb])
        ek = ph.sbuf("mek", [128, NCH], F32)
        K.op(K.act, lambda e: e.activation(out=ek[:, :], in_=imb[:, :], func=AF.Exp, bias=C["lnsc"][:, 0:1]),
             reads=[imb, C["lnsc"]], writes=[ek])
        wk = ph.sbuf("mwk", [128, NCH], F32)
        K.op(K.dve, lambda e: e.tensor_tensor(out=wk[:, :], in0=imb[:, :], in1=gps[:, 1, :], op=ALU.add),
             reads=[imb, gps], writes=[wk])
        K.op(K.act, lambda e: e.activation(out=wk[:, :], in_=wk[:, :], func=AF.Exp, bias=C["lnsc"][:, 0:1]),
             reads=[wk, C["lnsc"]], writes=[wk])
        dec = ph.sbuf("mdec", [128, NCH], F32)
        K.op(K.act, lambda e: e.activation(out=dec[:, :], in_=gps[:, 1, :], func=AF.Exp), reads=[gps], writes=[dec])
        Cst = ph.sbuf("mC", [128, 129], F32)
        Cbf = ph.sbuf("mCbf", [128, 128], BF16)
        nbc = ph.sbuf("mnbc", [128, 128], BF16)
        K.op(K.dve, lambda e: e.memset(Cst[:, :], 0.0), writes=[Cst])
        K.op(K.dve, lambda e: e.memset(Cbf[:, :], 0.0), writes=[Cbf])
        K.op(K.dve, lambda e: e.memset(nbc[:, :], 0.0), writes=[nbc])
        sps = ph.psum("msps", [128, 128])
        bps = ph.psum("mbps", [128, 128])
        nps = ph.psum("mnps", [128, 128])
        dps = ph.psum("mdps", [128, 128])
        kvps = ph.psum("mkvps", [128, 129])
        mps = ph.psum("mmps", [128, 128])
        lfb = ph.sbuf("mlfb", [128, 128], F32)
        embt = ph.sbuf("membt", [128, 128], F32)
        PT = ph.sbuf("mPT", [128, 128], BF16)
        k2 = ph.sbuf("mk2", [128, 128], BF16)
        dm = ph.sbuf("mdm", [128, 128], F32)
        hh = ph.sbuf("mhh", [128, 128], F32)
        sq = ph.sbuf("msq", [128, 128], BF16)
        rt = ph.sbuf("mrt", [128, 128], F32)
        og = ph.sbuf("mog", [128, 128], F32)
        osg = ph.sbuf("mosg", [128, 128], F32)
        yo = [ph.sbuf(f"myo{i}", [128, 128], BF16) for i in range(2)]
        for k in range(NCH):
            ts_ = slice(k * 128, (k + 1) * 128)
            K.op(K.pe, lambda e: e.matmul(sps[:, :], lhsT=kT[:, ts_], rhs=qT[:, ts_], start=True, stop=True),
                 reads=[kT, qT], writes=[sps])
            K.op(K.dve, lambda e: e.scalar_tensor_tensor(out=PT[:, :], in0=sps[:, :], scalar=ek[:, k:k + 1],
                                                         in1=C["causal"][:, :], op0=ALU.mult, op1=ALU.mult),
                 reads=[sps, ek, C["causal"]], writes=[PT])
            K.op(K.dve, lambda e: e.tensor_scalar(out=lfb[:, :], in0=C["onesF"][:, :], scalar1=lf[:, k:k + 1],
                                                  scalar2=None, op0=ALU.mult), reads=[C["onesF"], lf], writes=[lfb])
            K.op(K.pe, lambda e: e.matmul(bps[:, :], lhsT=lfb[:, :], rhs=C["TriF"][:, :], start=True, stop=True),
                 reads=[lfb, C["TriF"]], writes=[bps])
            K.op(K.act, lambda e: e.activation(out=embt[:, :], in_=bps[:, :], func=AF.Exp, scale=-1.0),
                 reads=[bps], writes=[embt])
            K.op(K.pe, lambda e: e.matmul(nps[:, :], lhsT=va[:, k, 0:128], rhs=PT[:, :], start=True, stop=False),
                 reads=[va, PT], writes=[nps])
            K.op(K.pe, lambda e: e.matmul(nps[:, :], lhsT=Cbf[:, :], rhs=qT[:, ts_], start=False, stop=True),
                 reads=[Cbf, qT], writes=[nps])
            K.op(K.pe, lambda e: e.matmul(dps[:, :], lhsT=C["ones"][:, :], rhs=PT[:, :], start=True, stop=False),
                 reads=[C["ones"], PT], writes=[dps])
            K.op(K.pe, lambda e: e.matmul(dps[:, :], lhsT=nbc[:, :], rhs=qT[:, ts_], start=False, stop=True),
                 reads=[nbc, qT], writes=[dps])
            K.op(K.act, lambda e: e.activation(out=dm[:, :], in_=dps[:, :], func=AF.Abs), reads=[dps], writes=[dm])
            K.op(K.dve, lambda e: e.tensor_tensor(out=dm[:, :], in0=dm[:, :], in1=embt[:, :], op=ALU.max),
                 reads=[dm, embt], writes=[dm])
            K.op(K.dve, lambda e: e.reciprocal(out=dm[:, :], in_=dm[:, :]), reads=[dm], writes=[dm])
            K.op(K.dve, lambda e: e.tensor_tensor(out=hh[:, :], in0=nps[:, :], in1=dm[:, :], op=ALU.mult),
                 reads=[nps, dm], writes=[hh])
            K.op(K.dve, lambda e: e.tensor_scalar(out=k2[:, :], in0=kt[:, k, :], scalar1=wk[:, k:k + 1],
                                                  scalar2=None, op0=ALU.mult), reads=[kt, wk], writes=[k2])
            K.op(K.pe, lambda e: e.matmul(kvps[:, :], lhsT=k2[:, :], rhs=va[:, k, :], start=True, stop=True),
                 reads=[k2, va], writes=[kvps])
            K.op(K.dve, lambda e: e.scalar_tensor_tensor(out=Cst[:, :], in0=Cst[:, :], scalar=dec[:, k:k + 1],
                                                         in1=kvps[:, :], op0=ALU.mult, op1=ALU.add),
                 reads=[Cst, dec, kvps], writes=[Cst])
            K.op(K.act, lambda e: e.activation(out=Cbf[:, :], in_=Cst[:, 0:128], func=AF.Copy),
                 reads=[Cst], writes=[Cbf])
            K.op(K.pool, lambda e: e.tensor_scalar(out=nbc[:, :], in0=C["onesF"][:, :], scalar1=Cst[:, 128:129],
                                                   scalar2=None, op0=ALU.mult), reads=[C["onesF"], Cst], writes=[nbc])
            K.op(K.act, lambda e: e.activation(out=sq[:, :], in_=hh[:, :], func=AF.Square), reads=[hh], writes=[sq])
            K.op(K.pe, lambda e: e.matmul(mps[:, :], lhsT=C["ones"][:, :], rhs=sq[:, :], start=True, stop=True),
                 reads=[C["ones"], sq], writes=[mps])
            K.op(K.act, lambda e: e.activation(out=rt[:, :], in_=mps[:, :], func=AF.Sqrt, scale=1.0 / 128,
                                               bias=C["eps"][:, 0:1]), reads=[mps, C["eps"]], writes=[rt])
            K.op(K.dve, lambda e: e.reciprocal(out=rt[:, :], in_=rt[:, :]), reads=[rt], writes=[rt])
            K.op(K.dve, lambda e: e.scalar_tensor_tensor(out=hh[:, :], in0=hh[:, :], scalar=hg[:, 0:1],
                                                         in1=rt[:, :], op0=ALU.mult, op1=ALU.mult),
                 reads=[hh, hg, rt], writes=[hh])
            K.op(K.act, lambda e: e.activation(out=osg[:, :], in_=ogf[:, ts_], func=AF.Sigmoid), reads=[ogf], writes=[osg])
            y_ = yo[k % 2]
            K.op(K.dve, lambda e: e.tensor_tensor(out=y_[:, :], in0=hh[:, :], in1=osg[:, :], op=ALU.mult),
                 reads=[hh, osg], writes=[y_])
            K.dma(K.sp, PAY2, PAY2[(k // 8) * 128:(k // 8 + 1) * 128, (k % 8) * 128:(k % 8 + 1) * 128],
                  y_, y_[:, :], partial=True, owner=y_)


def outproj_stage_f(K, C, x_src, ymixL, GAT2, wout_l, yT, x_dst, gi):
    gains = C["gains"]
    with Phase(K) as p1:
        yb = p1.sbuf("o_yb", [128, DC, T], BF16)
        rstd = p1.sbuf("o_rstd", [128, T], F32)
        for c in range(4):
            K.dma(K.pool, yb, yb[:, c, :], ymixL, ymixL[c], partial=(c > 0))
            K.dma(K.pool, yb, yb[:, 8 + c, :], ymixL, ymixL[4 + c], partial=True)
        for hh in range(4):
            K.dma_sel(yb, GAT2, lambda v: [(yb[:, 4 + hh, :], blkh(GAT2, hh, v))], partial=True)
            K.dma_sel(yb, GAT2, lambda v: [
                (yb[:, 12 + hh, 0:512], blkh(GAT2, hh, 8 + v)[:, 0:512]),
                (yb[:, 12 + hh, 512:1024], blkh(GAT2, hh + 4, 8 + v)[:, 0:512]),
            ], partial=True)
        down_phase(K, C, p1, yb, DC, wout_l, yT, rstd)
        epilogue_phase(K, C, x_src, yT, rstd, lambda c: gains[:, gi * DC + c:gi * DC + c + 1], x_dst, half=False)


FUSED_IN = dict(
    x_in=[DC, 128, T], gains=[128, None], wgu1=[None, FC, 128, 2, DC, 128], wd1=[None, DC, 128, FC, 128],
    wgu2=[None, FC, 128, 2, DC, 128], wd2=[None, DC, 128, FC, 128], win=[None, NPJ, 128, DC, 128],
    wout=[None, DC, 128, DC, 128], conv_w=[None, 128, 4, 3], pool_w=[None, 128, 4, 128],
    pool_scale=[None, 128, 4], mbi=[None, 128, 1], mbf=[None, 128, 1], mhg=[None, 128, 1],
    invcnt=[4, 128, T], qpos=[128, QTL * 512], kpos=[128, NCH], zeros=[128, 16],
    k_Ustrict=[128, 128], k_Lincl=[128, 128], k_causal=[128, 128], k_TriF=[128, 128], k_onesF=[128, 128])


def build_fused(depth, stages=None):
    nc = bass.Bass("TRN2", target_bir_lowering=False)
    with ExitStack() as es:
        K = Kern(nc, es)
        I = {}
        for k, shp in FUSED_IN.items():
            shp = [(depth * 6 * DC if k == "gains" else depth) if d is None else d for d in shp]
            I[k] = K.dram(k, shp, F32, "ExternalInput")
        x_out = K.dram("x_out", [DC, 128, T], F32, "ExternalOutput")
        X = [K.dram(f"xs{i}", [DC, 128, T], F32) for i in range(3)]
        yT = K.dram("yT", [DC, 128, T], F32)
        projT = K.dram("projT", [NPJ, 128, T], F32)
        PAY = K.dram("pay", [NBLK * 128, T], F32)
        GAT = K.dram("gat", [NBLK, NCORES * 128, T], F32)
        PAYH = K.dram("payh", [NBLKH * 128, T], BF16)
        GATH = K.dram("gath", [NBLKH // 2, NCORES * 256, T], BF16)
        ymixL = K.dram("ymixl", [8, 128, T], F32)
        PAY2 = K.dram("pay2", [NBLK2 * 128, T], BF16)
        GAT2 = K.dram("gat2", [NBLK2 // 2, NCORES * 256, T], BF16)
        C = load_consts(K, es, I["gains"])
        load_mixer_consts(K, es, C, I)
        GI = GatIn(K, GAT, GATH)
        lay = lambda name, l: Buf(I[name].t[l], name)
        for l in range(depth):
            src = I["x_in"] if l == 0 else X[0]
            dst = x_out if l == depth - 1 else X[0]
            on = lambda st: stages is None or st in stages
            if on("ffn1"):
                ffn_stage(K, C, src, X[1], yT, lay("wgu1", l), lay("wd1", l), l * 6 + 0)
            if on("inproj"):
                inproj_stage_f(K, C, X[1] if on("ffn1") else src, projT, PAY, PAYH, lay("win", l), l * 6 + 2)
            if on("ag1"):
                for b in range(NBLK):
                    K.collective("AllGather", PAY, PAY[b * 128:(b + 1) * 128, :], GAT, GAT[b], first=(b == 0))
                for b in range(NBLKH // 2):
                    K.collective("AllGather", PAYH, PAYH[b * 256:(b + 1) * 256, :], GATH, GATH[b], first=(b == 0))
            L = dict(conv_w=lay("conv_w", l), pool_w=lay("pool_w", l), pool_scale=lay("pool_scale", l),
                     mbi=lay("mbi", l), mbf=lay("mbf", l), mhg=lay("mhg", l),
                     invcnt=I["invcnt"], qpos=I["qpos"], kpos=I["kpos"])
            if on("cp"):
                conv_pool_stage_f(K, C, L, projT, GAT, ymixL, I["zeros"])
            if on("ml"):
                mlstm_stage_f(K, C, L, GI, PAY2)
            if on("sb"):
                sb_stage_f(K, C, L, GI, PAY2)
            if on("ag2"):
                for b in range(NBLK2 // 2):
                    K.collective("AllGather", PAY2, PAY2[b * 256:(b + 1) * 256, :], GAT2, GAT2[b], first=(b == 0))
            if on("outproj"):
                outproj_stage_f(K, C, X[1] if on("ffn1") else src, ymixL, GAT2, lay("wout", l), yT, X[2], l * 6 + 3)
            if on("ffn2"):
                ffn_stage(K, C, X[2], dst, yT, lay("wgu2", l), lay("wd2", l), l * 6 + 4)
        if stages is not None and "ffn2" not in stages:
            K.sync_bufs([projT, PAY, GAT, PAYH, GATH, ymixL, PAY2, GAT2, X[2]], engs=[K.sp, K.pool])
        finish(K, [x_out])
    return nc


def permute_win(w):
    out = np.zeros((D, NPJ * 128), np.float32)
    out[:, 0:3584] = w[:, 0:3584]
    out[:, 3584:5632] = w[:, 3592:5640]
    out[:, 5632:5640] = w[:, 3584:3592]
    return out


def fused_inputs(inp, depth):
    f = lambda a: np.asarray(a, dtype=np.float32)
    sh = {}
    sh["gains"] = np.ascontiguousarray(np.concatenate([gains_cols(f(inp["norm_gains"][l])) for l in range(depth)], axis=1))
    sh["wgu1"] = np.stack([tile_gu(f(inp["ffn1_w_gate"][l]), f(inp["ffn1_w_up"][l])) for l in range(depth)])
    sh["wd1"] = np.stack([tile_down(f(inp["ffn1_w_down"][l])) for l in range(depth)])
    sh["wgu2"] = np.stack([tile_gu(f(inp["ffn2_w_gate"][l]), f(inp["ffn2_w_up"][l])) for l in range(depth)])
    sh["wd2"] = np.stack([tile_down(f(inp["ffn2_w_down"][l])) for l in range(depth)])
    sh["win"] = np.stack([tile_kn(permute_win(f(inp["w_in"][l])), DC, NPJ) for l in range(depth)])
    sh["wout"] = np.stack([tile_kn(f(inp["w_out"][l]), DC, DC) for l in range(depth)])
    sh["conv_w"] = np.stack([np.ascontiguousarray(f(inp["conv_w"][l]).reshape(3, 4, 128).transpose(2, 1, 0)) for l in range(depth)])
    sh["pool_w"] = np.stack([np.ascontiguousarray(f(inp["pool_w"][l]).transpose(1, 0, 2)) for l in range(depth)])
    sh["pool_scale"] = np.stack([np.ascontiguousarray(f(inp["pool_scale"][l]).reshape(4, 128).T) for l in range(depth)])
    sh["kpos"] = np.ascontiguousarray((np.arange(NCH)[None, :] * 128 + np.arange(128)[:, None]).astype(np.float32))
    sh["zeros"] = np.zeros((128, 16), np.float32)
    sh.update(mixer_consts_host())
    x = f(inp["x"])
    maps = []
    for c in range(NCORES):
        h = c % 4
        m = dict(sh)
        m["x_in"] = to_fm(x[0, c * T:(c + 1) * T])
        m["mbi"] = np.stack([np.full((128, 1), f(inp["mlstm_i_bias"][l])[h], np.float32) for l in range(depth)])
        m["mbf"] = np.stack([np.full((128, 1), f(inp["mlstm_f_bias"][l])[h], np.float32) for l in range(depth)])
        m["mhg"] = np.stack([np.ascontiguousarray(f(inp["mlstm_head_gain"][l])[h * 128:(h + 1) * 128].reshape(128, 1))
                             for l in range(depth)])
        t = np.arange(c * T, (c + 1) * T)
        cnt = np.stack([np.minimum(t + 1, w) for w in (2, 4, 8, 16)], axis=0).astype(np.float32)
        m["invcnt"] = np.ascontiguousarray(np.broadcast_to((1.0 / cnt)[:, None, :], (4, 128, T))).astype(np.float32)
        qp = np.concatenate([np.arange(ti * 512, (ti + 1) * 512) for ti in sb_tiles(c)]).astype(np.float32)
        m["qpos"] = np.ascontiguousarray(np.broadcast_to(qp[None, :], (128, QTL * 512)))
        maps.append(m)
    return maps


_PROGS = {}


def kernel(**inp):
    if "fused" not in _PROGS:
        _PROGS["fused"] = build_fused(DEPTH)
    maps = fused_inputs(inp, DEPTH)
    res = run_bass_kernel_spmd(_PROGS["fused"], maps, core_ids=list(range(NCORES)))
    out = np.concatenate([from_fm(r["x_out"]) for r in res.results], axis=0)[None]
    return np.ascontiguousarray(out.astype(np.float32))
```
